# Optimizing a Trainium2 kernel written in Bass

```python
import jax, jax.numpy as jnp
from jax import lax
import numpy as np

D_MODEL = 1024
BATCH = 8
SEQ = 2048
DEPTH = 2

GRID_W = 64
CTX_LEN = 256
N_MIXERS = 2
POOL_WINDOWS = (2, 4, 8, 16)
N_POOL_GROUPS = 4
POOL_GROUP = D_MODEL // N_POOL_GROUPS
N_HEADS = D_MODEL // 128
Q_LORA = D_MODEL // 2
KV_LORA = D_MODEL // 4
QK_NOPE = 128
QK_ROPE = 64
V_HEAD = 128
ROPE_AXIS = QK_ROPE // 2
ROPE_BASE = 10000.0
ATTN_SCALE = (QK_NOPE + QK_ROPE) ** -0.5
Q_BLOCK = 128
N_EXPERTS = 32
TOP_K = 4
D_FF = D_MODEL
SWIGLU_LIMIT = 7.0
SWIGLU_ALPHA = 1.702
EXPERT_BLOCK = 256
EPS = 1e-6
N_POOL_LAYERS = (DEPTH + 1) // 2
N_MLA_LAYERS = DEPTH // 2

kernel_name = "hybrid_pool_mla_moe_prefix_trunk"


def rmsnorm(x, g):
    xf = x.astype(jnp.float32)
    y = xf * lax.rsqrt(jnp.mean(xf * xf, axis=-1, keepdims=True) + EPS)
    return (y * g.astype(jnp.float32)).astype(x.dtype)


def modulate(h, shift, scale):
    return h * (1 + scale) + shift


def centred_mean(h, w):
    L = h.shape[1]
    cs = jnp.cumsum(h.astype(jnp.float32), axis=1)
    cs = jnp.pad(cs, ((0, 0), (1, 0), (0, 0)))
    t = jnp.arange(L)
    lo = jnp.clip(t - w // 2, 0, L)
    hi = jnp.clip(t + w - w // 2, 0, L)
    s = cs[:, hi] - cs[:, lo]
    cnt = (hi - lo).astype(jnp.float32)
    return (s / cnt[None, :, None]).astype(h.dtype)


def pool_mixer(h, w, b, scale):
    B, L, D = h.shape
    hg = h.reshape(B, L, N_POOL_GROUPS, POOL_GROUP)
    d = jnp.stack([centred_mean(hg[:, :, g], POOL_WINDOWS[g]) - hg[:, :, g]
                   for g in range(N_POOL_GROUPS)], axis=2)
    y = jnp.einsum('blgc,gcd->blgd', d, w).reshape(B, L, D) + b
    return y * scale


def rotate(x, ang):
    x1, x2 = jnp.split(x, 2, axis=-1)
    cos = jnp.cos(ang)[None, :, None, :].astype(x.dtype)
    sin = jnp.sin(ang)[None, :, None, :].astype(x.dtype)
    return jnp.concatenate([x1 * cos - x2 * sin, x2 * cos + x1 * sin], axis=-1)


def axial_rope(x, ang_row, ang_col):
    xr, xc = jnp.split(x, 2, axis=-1)
    return jnp.concatenate([rotate(xr, ang_row), rotate(xc, ang_col)], axis=-1)


def mla_q(h, w_dq, q_norm_g, w_uq, ang):
    B, L, _ = h.shape
    q = (rmsnorm(h @ w_dq, q_norm_g) @ w_uq).reshape(B, L, N_HEADS, QK_NOPE + QK_ROPE)
    q_nope, q_pe = q[..., :QK_NOPE], q[..., QK_NOPE:]
    if ang is not None:
        q_pe = axial_rope(q_pe, *ang)
    return jnp.concatenate([q_nope, q_pe], axis=-1)


def mla_kv(h, w_dkv, kv_norm_g, w_ukv, ang):
    B, L, _ = h.shape
    kv_a = h @ w_dkv
    c_kv, k_pe = kv_a[..., :KV_LORA], kv_a[..., KV_LORA:][:, :, None, :]
    kv = (rmsnorm(c_kv, kv_norm_g) @ w_ukv).reshape(B, L, N_HEADS, QK_NOPE + V_HEAD)
    k_nope, v = kv[..., :QK_NOPE], kv[..., QK_NOPE:]
    if ang is not None:
        k_pe = axial_rope(k_pe, *ang)
    k = jnp.concatenate([k_nope, jnp.broadcast_to(k_pe, (B, L, N_HEADS, QK_ROPE))], axis=-1)
    return k, v


def attend(q, k, v):
    s = jnp.einsum('bqhd,bkhd->bhqk', q, k).astype(jnp.float32) * ATTN_SCALE
    p = jax.nn.softmax(s, axis=-1).astype(v.dtype)
    return jnp.einsum('bhqk,bkhd->bqhd', p, v)


def blocked_attention(q, k, v):
    B, L, H, Dk = q.shape
    nb = L // Q_BLOCK
    qb = jnp.moveaxis(q.reshape(B, nb, Q_BLOCK, H, Dk), 1, 0)
    ob = lax.map(lambda qq: attend(qq, k, v), qb)
    return jnp.moveaxis(ob, 0, 1).reshape(B, L, H * V_HEAD)


def moe_ffn(h, router_w, router_b, w_gu, b_gu, w_down, b_down):
    T, D = h.shape
    logits = (h @ router_w + router_b).astype(jnp.float32)
    top_logits, top_e = lax.top_k(logits, TOP_K)
    gates = jax.nn.softmax(top_logits, axis=-1).astype(h.dtype)
    n_assign = T * TOP_K
    flat_e = top_e.reshape(-1)
    order = jnp.argsort(flat_e)
    sorted_e = flat_e[order]
    counts = jnp.bincount(flat_e, length=N_EXPERTS)
    padded = (counts + EXPERT_BLOCK - 1) // EXPERT_BLOCK * EXPERT_BLOCK
    start = jnp.cumsum(counts) - counts
    pends = jnp.cumsum(padded)
    pstart = pends - padded
    slot = (pstart[sorted_e] + jnp.arange(n_assign) - start[sorted_e]).astype(jnp.int32)
    n_blocks = -(-n_assign // EXPERT_BLOCK) + N_EXPERTS
    n_slots = n_blocks * EXPERT_BLOCK
    slot_token = jnp.full((n_slots,), T, jnp.int32).at[slot].set((order // TOP_K).astype(jnp.int32))
    block_expert = jnp.clip(jnp.searchsorted(pends, jnp.arange(n_blocks) * EXPERT_BLOCK, side='right'),
                            0, N_EXPERTS - 1)
    h_pad = jnp.concatenate([h, jnp.zeros((1, D), h.dtype)], axis=0)
    xs = h_pad[slot_token].reshape(n_blocks, EXPERT_BLOCK, D)

    def expert_block(args):
        xb, e = args
        gu = xb @ w_gu[e] + b_gu[e]
        gate = jnp.minimum(gu[:, :D_FF], SWIGLU_LIMIT)
        up = jnp.clip(gu[:, D_FF:], -SWIGLU_LIMIT, SWIGLU_LIMIT)
        act = (up + 1) * (gate * jax.nn.sigmoid(SWIGLU_ALPHA * gate))
        return act @ w_down[e] + b_down[e]

    ys = lax.map(expert_block, (xs, block_expert)).reshape(n_slots, D)
    assign_slot = jnp.zeros((n_assign,), jnp.int32).at[order].set(slot)
    y = ys[assign_slot].reshape(T, TOP_K, D)
    return jnp.einsum('tk,tkd->td', gates, y)


def setup_inputs(seed: int = 0) -> dict:
    key = jax.random.key(seed)
    ks = jax.random.split(key, 32)
    D, f32 = D_MODEL, jnp.float32
    nrm = lambda k, shape, s: jax.random.normal(k, shape, f32) * s
    gain = lambda k, shape: 1.0 + 0.02 * jax.random.normal(k, shape, f32)
    return {
        "x": nrm(ks[0], (BATCH, SEQ, D), 1.0),
        "c": nrm(ks[1], (BATCH, D), 1.0),
        "ctx": nrm(ks[2], (BATCH, CTX_LEN, D), 1.0),
        "c_ctx": nrm(ks[3], (D,), 1.0),
        "ada_w": nrm(ks[4], (DEPTH, D, 6 * D), 0.5 * D ** -0.5),
        "ada_b": nrm(ks[5], (DEPTH, 6 * D), 0.02),
        "norm1_g": gain(ks[6], (DEPTH, D)),
        "norm2_g": gain(ks[7], (DEPTH, D)),
        "pool_w": nrm(ks[8], (N_POOL_LAYERS, N_POOL_GROUPS, POOL_GROUP, POOL_GROUP), POOL_GROUP ** -0.5),
        "pool_b": nrm(ks[9], (N_POOL_LAYERS, D), 0.02),
        "pool_scale": gain(ks[10], (N_POOL_LAYERS, D)),
        "w_dq": nrm(ks[11], (N_MLA_LAYERS, D, Q_LORA), D ** -0.5),
        "q_norm_g": gain(ks[12], (N_MLA_LAYERS, Q_LORA)),
        "w_uq": nrm(ks[13], (N_MLA_LAYERS, Q_LORA, N_HEADS * (QK_NOPE + QK_ROPE)), Q_LORA ** -0.5),
        "w_dkv": nrm(ks[14], (N_MLA_LAYERS, D, KV_LORA + QK_ROPE), D ** -0.5),
        "kv_norm_g": gain(ks[15], (N_MLA_LAYERS, KV_LORA)),
        "w_ukv": nrm(ks[16], (N_MLA_LAYERS, KV_LORA, N_HEADS * (QK_NOPE + V_HEAD)), KV_LORA ** -0.5),
        "w_o": nrm(ks[17], (N_MLA_LAYERS, N_HEADS * V_HEAD, D), (N_HEADS * V_HEAD) ** -0.5),
        "router_w": nrm(ks[18], (DEPTH, D, N_EXPERTS), D ** -0.5),
        "router_b": nrm(ks[19], (DEPTH, N_EXPERTS), 0.01),
        "w_gu": nrm(ks[20], (DEPTH, N_EXPERTS, D, 2 * D_FF), D ** -0.5),
        "b_gu": nrm(ks[21], (DEPTH, N_EXPERTS, 2 * D_FF), 0.02),
        "w_down": nrm(ks[22], (DEPTH, N_EXPERTS, D_FF, D), D_FF ** -0.5),
        "b_down": nrm(ks[23], (DEPTH, N_EXPERTS, D), 0.02),
        "final_g": gain(ks[24], (D,)),
    }


def reference(x, c, ctx, c_ctx, ada_w, ada_b, norm1_g, norm2_g, pool_w, pool_b, pool_scale,
              w_dq, q_norm_g, w_uq, w_dkv, kv_norm_g, w_ukv, w_o,
              router_w, router_b, w_gu, b_gu, w_down, b_down, final_g):
    B, L, D = x.shape
    C = ctx.shape[1]
    rows = L // GRID_W
    row_pos = jnp.repeat(jnp.arange(rows), GRID_W).astype(jnp.float32)
    col_pos = jnp.tile(jnp.arange(GRID_W), rows).astype(jnp.float32)
    inv_freq = ROPE_BASE ** (-jnp.arange(0, ROPE_AXIS, 2, dtype=jnp.float32) / ROPE_AXIS)
    ang = (row_pos[:, None] * inv_freq, col_pos[:, None] * inv_freq)

    xc = ctx
    sc_lat = jax.nn.silu(c)
    sc_ctx = jax.nn.silu(c_ctx)
    for i in range(DEPTH):
        is_last = i == DEPTH - 1
        mixer = i % N_MIXERS
        j = i // N_MIXERS
        mod = sc_lat @ ada_w[i] + ada_b[i]
        sh1, s1, g1, sh2, s2, g2 = jnp.split(mod[:, None, :], 6, axis=-1)
        ctx_needed = (not is_last) or mixer == 1
        if ctx_needed:
            modc = sc_ctx @ ada_w[i] + ada_b[i]
            sh1c, s1c, g1c, sh2c, s2c, g2c = jnp.split(modc, 6, axis=-1)
            hc = modulate(rmsnorm(xc, norm1_g[i]), sh1c, s1c)
        h = modulate(rmsnorm(x, norm1_g[i]), sh1, s1)

        if mixer == 0:
            x = x + g1 * pool_mixer(h, pool_w[j], pool_b[j], pool_scale[j])
            if not is_last:
                xc = xc + g1c * pool_mixer(hc, pool_w[j], pool_b[j], pool_scale[j])
        else:
            kc, vc = mla_kv(hc, w_dkv[j], kv_norm_g[j], w_ukv[j], None)
            kl, vl = mla_kv(h, w_dkv[j], kv_norm_g[j], w_ukv[j], ang)
            ql = mla_q(h, w_dq[j], q_norm_g[j], w_uq[j], ang)
            k_all = jnp.concatenate([kc, kl], axis=1)
            v_all = jnp.concatenate([vc, vl], axis=1)
            x = x + g1 * (blocked_attention(ql, k_all, v_all) @ w_o[j])
            if not is_last:
                qc = mla_q(hc, w_dq[j], q_norm_g[j], w_uq[j], None)
                yc = attend(qc, kc, vc).reshape(B, C, N_HEADS * V_HEAD)
                xc = xc + g1c * (yc @ w_o[j])

        h2 = modulate(rmsnorm(x, norm2_g[i]), sh2, s2)
        if is_last:
            y = moe_ffn(h2.reshape(B * L, D), router_w[i], router_b[i], w_gu[i], b_gu[i],
                        w_down[i], b_down[i])
            x = x + g2 * y.reshape(B, L, D)
        else:
            h2c = modulate(rmsnorm(xc, norm2_g[i]), sh2c, s2c)
            tokens = jnp.concatenate([h2.reshape(B * L, D), h2c.reshape(B * C, D)], axis=0)
            y = moe_ffn(tokens, router_w[i], router_b[i], w_gu[i], b_gu[i], w_down[i], b_down[i])
            x = x + g2 * y[:B * L].reshape(B, L, D)
            xc = xc + g2c * y[B * L:].reshape(B, C, D)

    return rmsnorm(x, final_g)
```

```python
import numpy as np
from contextlib import ExitStack
import concourse.bass as bass
import concourse.mybir as mybir
from concourse.bass_utils import run_bass_kernel_spmd

F32 = mybir.dt.float32
BF16 = mybir.dt.bfloat16
I32 = mybir.dt.int32
U32 = mybir.dt.uint32
ALU = mybir.AluOpType
AF = mybir.ActivationFunctionType
AX = mybir.AxisListType

ENGINES = ("pe", "act", "dve", "pool", "sp")
EPOCH = 30000

D = 1024
L = 2048
C = 256
T = L + C
NE = 32
CAP = 1024
NSLOT = NE * CAP
EPS = 1e-6
ATTN_SCALE = 192 ** -0.5

STAGE = 99
NCORES = 8
DEBUG_TAGS = False
NO_ACT_DMA = False
CUT = 0


class SemGrp:
    __slots__ = ("name", "ndma", "sem", "bg")

    def __init__(self, name):
        self.name = name
        self.ndma = 0
        self.sem = None
        self.bg = False


class Buf:
    __slots__ = ("name", "last_w", "readers", "semgrp", "dram", "psum")

    def __init__(self, name, grp=None, dram=False, psum=False):
        self.name = name
        self.dram = dram
        self.psum = psum
        self.last_w = None
        self.readers = []
        self.semgrp = grp if grp is not None else SemGrp(name)


class TV:
    __slots__ = ("ap", "buf")

    def __init__(self, ap, buf):
        self.ap = ap
        self.buf = buf

    def __getitem__(self, k):
        return TV(self.ap[k], self.buf)

    def bitcast(self, dt):
        return TV(self.ap.bitcast(dt), self.buf)

    def rearrange(self, s, **kw):
        return TV(self.ap.rearrange(s, **kw), self.buf)

    def bc(self, shape):
        return TV(self.ap.to_broadcast(list(shape)), self.buf)

    def sub(self, name):
        return TV(self.ap, Buf(name))


class Op:
    __slots__ = ("eng", "fn", "deps", "is_dma", "grp", "signal", "epoch", "sigval", "tag", "cond", "dval")


def _ap(x):
    return x.ap if isinstance(x, TV) else x


class Prog:
    def __init__(self, nc, stack):
        self.nc = nc
        self.stack = stack
        self.ops = {e: [] for e in ENGINES}
        self.grps = {}
        self.n = 0
        self.cur_cond = None
        self.creg = {}
        self.nregion = 0

    def op(self, eng, fn, reads=(), writes=(), dma=None):
        o = Op()
        o.eng = eng
        o.fn = fn
        o.tag = None
        if DEBUG_TAGS:
            import sys as _s
            f = _s._getframe(1)
            while f.f_code.co_name in ("mm", "tr", "act", "tt", "ts", "stt", "copy", "memset", "red", "dma", "op"):
                f = f.f_back
            o.tag = f.f_lineno
        o.is_dma = dma is not None
        o.signal = False
        o.grp = None
        o.cond = self.cur_cond
        o.dval = 0
        deps = []
        seen = set()

        def add(d):
            if d is None or id(d) in seen:
                return
            seen.add(id(d))
            if d.is_dma:
                deps.append(("d", d.grp, 16 * d.grp.ndma))
            else:
                if d.eng == eng and eng == "pe" and not o.is_dma:
                    return
                deps.append(("c", d))

        rb = [r.buf if isinstance(r, TV) else r for r in reads if r is not None and not isinstance(r, (int, float))]
        wb = [w.buf if isinstance(w, TV) else w for w in writes]
        def latest(readers):
            last = {}
            for rd in readers:
                last[("d", id(rd.grp)) if rd.is_dma else ("c", rd.eng)] = rd
            return last.values()

        for r in rb:
            add(r.last_w)
            if r.psum:
                for rd in latest(r.readers):
                    if rd.eng != eng:
                        add(rd)
        for w in wb:
            add(w.last_w)
            for rd in latest(w.readers):
                add(rd)
        o.deps = deps
        if o.is_dma:
            g = (dma.buf if isinstance(dma, TV) else dma).semgrp
            o.dval = 16 * g.ndma
            g.ndma += 1
            o.grp = g
            self.grps[id(g)] = g
        for r in rb:
            r.readers.append(o)
        for w in wb:
            w.last_w = o
            w.readers = []
        self.ops[eng].append(o)
        self.n += 1
        return o

    def barrier(self, skip_bg=True):
        lasts = []
        for e in ENGINES:
            for o in reversed(self.ops[e]):
                if not o.is_dma and o.fn is not None:
                    lasts.append(o)
                    break
        dm = [("d", g, 16 * g.ndma) for g in self.grps.values() if g.ndma > 0 and not (skip_bg and g.bg)]
        for e in ENGINES:
            o = Op()
            o.eng = e
            o.fn = None
            o.is_dma = False
            o.signal = False
            o.grp = None
            o.cond = None
            o.dval = 0
            o.deps = [("c", x) for x in lasts if not (x.eng == e)] + list(dm)
            self.ops[e].append(o)

    def begin_region(self, flag, engines=("pe", "act", "dve", "sp")):
        assert self.cur_cond is None
        for e in engines:
            def ld(eng, e=e):
                if e not in self.creg:
                    self.creg[e] = eng.alloc_register(f"cflag_{e}")
                return eng.reg_load(self.creg[e], flag.ap)
            self.op(e, ld, reads=[flag])
        self.nregion += 1
        self.cur_cond = self.nregion

    def end_region(self):
        self.cur_cond = None

    def mm(self, out, lhsT, rhs, start=True, stop=True):
        return self.op("pe", lambda e: e.matmul(out.ap, lhsT=lhsT.ap, rhs=rhs.ap, start=start, stop=stop),
                       reads=[lhsT, rhs], writes=[out])

    def tr(self, out, in_, ident):
        return self.op("pe", lambda e: e.transpose(out=out.ap, in_=in_.ap, identity=ident.ap),
                       reads=[in_, ident], writes=[out])

    def act(self, out, in_, func, bias=0.0, scale=1.0, accum_out=None, eng="act"):
        kw = {}
        if accum_out is not None:
            kw["accum_out"] = accum_out.ap
        return self.op(eng, lambda e: e.activation(out=out.ap, in_=in_.ap, func=func, bias=_ap(bias), scale=_ap(scale), **kw),
                       reads=[in_, bias, scale], writes=[out] + ([accum_out] if accum_out is not None else []))

    def tt(self, eng, out, in0, in1, op):
        return self.op(eng, lambda e: e.tensor_tensor(out=out.ap, in0=in0.ap, in1=in1.ap, op=op),
                       reads=[in0, in1], writes=[out])

    def ts(self, eng, out, in0, s1, op0, s2=None, op1=None, accum_out=None):
        kw = {}
        if op1 is not None:
            kw["op1"] = op1
        if accum_out is not None:
            kw["accum_out"] = accum_out.ap
        return self.op(eng, lambda e: e.tensor_scalar(out=out.ap, in0=in0.ap, scalar1=_ap(s1), scalar2=_ap(s2), op0=op0, **kw),
                       reads=[in0, s1, s2], writes=[out] + ([accum_out] if accum_out is not None else []))

    def stt(self, eng, out, in0, scalar, in1, op0, op1):
        return self.op(eng, lambda e: e.scalar_tensor_tensor(out=out.ap, in0=in0.ap, scalar=_ap(scalar), in1=in1.ap, op0=op0, op1=op1),
                       reads=[in0, scalar, in1], writes=[out])

    def copy(self, eng, out, in_):
        if eng == "act":
            return self.op(eng, lambda e: e.activation(out=out.ap, in_=in_.ap, func=AF.Copy), reads=[in_], writes=[out])
        return self.op(eng, lambda e: e.tensor_copy(out=out.ap, in_=in_.ap), reads=[in_], writes=[out])

    def memset(self, eng, out, val):
        return self.op(eng, lambda e: e.memset(out.ap, val), writes=[out])

    def red(self, eng, out, in_, op, axis=AX.X):
        return self.op(eng, lambda e: e.tensor_reduce(out=out.ap, in_=in_.ap, axis=axis, op=op), reads=[in_], writes=[out])

    def dma(self, q, out, in_, owner=None, **kw):
        if q == "act" and NO_ACT_DMA:
            q = "sp"
        if owner is None:
            owner = in_ if out.buf.dram else out
        return self.op(q, lambda e: e.dma_start(out=out.ap, in_=in_.ap, **kw), reads=[in_], writes=[out], dma=owner)

    def emit(self):
        nc = self.nc
        for e in ENGINES:
            for o in self.ops[e]:
                for d in o.deps:
                    if d[0] == "c":
                        d[1].signal = True
        self.esems = {}
        for e in ENGINES:
            cnt = 0
            for o in self.ops[e]:
                if not o.is_dma and o.signal:
                    o.epoch = cnt // EPOCH
                    o.sigval = cnt % EPOCH + 1
                    cnt += 1
            nep = max(1, (cnt + EPOCH - 1) // EPOCH)
            self.esems[e] = [self.stack.enter_context(nc.semaphore(f"s_{e}{k}")) for k in range(nep)]
        for g in self.grps.values():
            g.sem = self.stack.enter_context(nc.semaphore(f"d_{g.name}"))
        self.nsem = sum(len(v) for v in self.esems.values()) + len(self.grps)
        block = self.stack.enter_context(nc.Block())
        reg = {"pe": block.tensor, "act": block.scalar, "dve": block.vector, "pool": block.gpsimd, "sp": block.sync}
        nwaits = {e: 0 for e in ENGINES}

        def make(e):
            def body(eng):
                def do_waits(o, waited):
                    for d in o.deps:
                        if d[0] == "d":
                            sem, val, key = d[1].sem, d[2], ("d", id(d[1]))
                        else:
                            x = d[1]
                            sem, val, key = self.esems[x.eng][x.epoch], x.sigval, ("c", x.eng, x.epoch)
                        if waited.get(key, 0) >= val:
                            continue
                        waited[key] = val
                        eng.wait_ge(sem, val)
                        nwaits[e] += 1

                def real(o, waited):
                    do_waits(o, waited)
                    if o.fn is None:
                        assert not o.signal
                        return
                    ins = o.fn(eng)
                    if o.is_dma:
                        ins.then_inc(o.grp.sem, 16)
                    elif o.signal:
                        ins.then_inc(self.esems[e][o.epoch], 1)

                pend = {}

                def flush():
                    for sem, n in pend.values():
                        eng.sem_inc(sem, n)
                    pend.clear()

                def ghost(o, waited, first):
                    if o.is_dma:
                        if o.dval > 0:
                            flush()
                            eng.wait_ge(o.grp.sem, o.dval)
                        k_ = id(o.grp.sem)
                        pend[k_] = (o.grp.sem, pend.get(k_, (None, 0))[1] + 16)
                        flush()
                    elif o.signal:
                        sm = self.esems[e][o.epoch]
                        k_ = id(sm)
                        pend[k_] = (sm, pend.get(k_, (None, 0))[1] + 1)

                waited = {}
                ops = self.ops[e]
                i = 0
                while i < len(ops):
                    o = ops[i]
                    if o.cond is None:
                        real(o, waited)
                        i += 1
                        continue
                    j = i
                    while j < len(ops) and ops[j].cond == o.cond:
                        j += 1
                    w1 = dict(waited)
                    with eng.If_ne(self.creg[e], 0):
                        for k in range(i, j):
                            real(ops[k], w1)
                    w2 = dict(waited)
                    with eng.Else():
                        eng.drain()
                        for k in range(i, j):
                            ghost(ops[k], w2, k == i)
                        flush()
                    i = j
            return body

        for e in ENGINES:
            if self.ops[e]:
                reg[e](make(e))
        self.nwaits = nwaits


def _shape(ap, shape):
    if len(shape) <= 1:
        return ap
    names = "abcdefg"[:len(shape)]
    kw = {names[i]: int(shape[i]) for i in range(len(shape) - 1)}
    return ap.rearrange(f"p ({' '.join(names)}) -> p {' '.join(names)}", **kw)


class Arena:
    def __init__(self, P, cols):
        self.P = P
        self.t = P.stack.enter_context(P.nc.sbuf_tensor("arena", [128, cols], F32))
        self.cols = cols
        self.top = 0
        self.n = 0

    def mark(self):
        return self.top

    def reset(self, m):
        self.top = m

    def f32(self, name, shape):
        n = int(np.prod(shape))
        off = self.top
        self.top += n
        assert self.top <= self.cols, f"arena overflow {name}: {self.top} > {self.cols}"
        ap = _shape(self.t[:, off:off + n], shape)
        self.n += 1
        return TV(ap, Buf(f"{name}{self.n}"))

    def b16(self, name, shape, dt=BF16):
        n = int(np.prod(shape))
        nf = (n + 1) // 2
        off = self.top
        self.top += nf
        assert self.top <= self.cols, f"arena overflow {name}: {self.top} > {self.cols}"
        ap = _shape(self.t[:, off:off + nf].bitcast(dt)[:, 0:n], shape)
        self.n += 1
        return TV(ap, Buf(f"{name}{self.n}"))

    def i32(self, name, shape):
        tv = self.f32(name, shape)
        return TV(tv.ap.bitcast(I32), tv.buf)


R_N1G, R_N2G, R_PB, R_PS, R_FG, R_C, R_CC, R_ADAB, R_QG, R_KG, R_BD = 0, 2, 4, 5, 6, 7, 8, 9, 21, 22, 32
NV1 = 96


def build_program(stage=99):
    nc = bass.Bass("TRN2", target_bir_lowering=False)
    dr = {}

    def din(name, shape, dt=F32):
        dr[name] = nc.dram_tensor(name, list(shape), dt, kind="ExternalInput").ap()
        return dr[name]

    x_d = din("x", [L, D]); ctx_d = din("ctx", [C, D])
    v1_d = din("v1", [NV1, D]); v2_d = din("v2", [128, D])
    adaw_d = din("ada_w", [2, D, 6 * D])
    poolw_d = din("pool_w", [4, 256, 256])
    if stage >= 2:
        rw_d = din("router_w", [2, D, NE]); rb_d = din("router_b", [2, NE])
        wgu_d = din("w_gu", [2, NE, D, 2 * D]); wdn_d = din("w_down", [2, NE, D, D]); bdn_d = din("b_down", [2, NE, D])
    if stage >= 3:
        wdq_d = din("w_dq", [D, 512]); wuqn_d = din("w_uq_n", [512, 1024]); wuqr_d = din("w_uq_r", [512, 1024])
        wdkv_d = din("w_dkv", [D, 384]); wuk_d = din("w_uk", [256, 1024]); wuv_d = din("w_uv", [256, 1024])
        wo_d = din("w_o", [D, D])
    out_d = nc.dram_tensor("out", [L, D], F32, kind="ExternalOutput").ap()
    dbg_d = nc.dram_tensor("dbg", [128, 8, T], F32, kind="ExternalOutput").ap() if stage < 99 else None
    if stage >= 2:
        xs_d = nc.dram_tensor("xs_scr", [NSLOT + 1, D], BF16, kind="Internal").ap()
        ys_d = nc.dram_tensor("ys_scr", [NSLOT + 1, D], F32, kind="Internal").ap()

    st = ExitStack()
    P = Prog(nc, st)
    A = Arena(P, 52736)
    dram = lambda ap, name: TV(ap, Buf(name, dram=True))

    PS = []
    for k in range(8):
        t = st.enter_context(nc.psum_tensor(f"ps{k}", [128, 512], F32))
        PS.append(TV(t[:, :], Buf(f"ps{k}", psum=True)))

    def ps_b16(k):
        return TV(PS[k].ap.bitcast(BF16), PS[k].buf)

    xT = A.f32("xT", [8, T])
    xTc = [TV(xT.ap[:, j, :], Buf(f"xT{j}")) for j in range(8)]
    VT1 = A.f32("VT1", [8, NV1])
    VT2 = A.f32("VT2", [8, 128])
    MOD = A.f32("MOD", [2, 48, 2])
    DER = A.f32("DER", [2, 6, 8, 2])
    identf = A.f32("identf", [128])
    onesf = A.f32("onesf", [128])
    ident = A.b16("ident", [128])
    onesb = A.b16("onesb", [128])
    tri = A.b16("tri", [128])
    zero1 = A.f32("zero1", [1])
    pmark = A.mark()

    P.memset("pool", identf, 0.0)
    P.op("pool", lambda e: e.affine_select(out=identf.ap, in_=identf.ap, pattern=[[-1, 128]], compare_op=ALU.not_equal,
                                           fill=1.0, base=0, channel_multiplier=1), reads=[identf], writes=[identf])
    P.copy("dve", ident, identf)
    P.memset("pool", onesf, 1.0)
    P.memset("pool", onesb, 1.0)
    P.memset("pool", zero1, 0.0)
    P.memset("pool", tri, 1.0)
    P.op("pool", lambda e: e.affine_select(out=tri.ap, in_=tri.ap, pattern=[[1, 128]], compare_op=ALU.is_gt,
                                           fill=0.0, base=0, channel_multiplier=-1), reads=[tri], writes=[tri])

    m0 = A.mark()
    v1s = A.f32("v1s", [D]); v2s = A.f32("v2s", [D])
    P.dma("sp", v1s[0:NV1], dram(v1_d, "v1d"))
    P.dma("sp", v2s, dram(v2_d, "v2d"))
    for (src, dst, nr) in ((v1s, VT1, NV1), (v2s, VT2, 128)):
        for j in range(8):
            pk = PS[j % 2]
            P.mm(pk[:, 0:nr], src[0:nr, j * 128:(j + 1) * 128], identf[0:nr, 0:nr])
            P.copy("dve", dst[:, j, :], pk[:, 0:nr])
    def cut():
        P.dma("sp", dram(dbg_d[:, 0, :], "dbgd"), xTc[0])
        P.barrier()
        P.emit()
        return nc, st, P
    if CUT == 1:
        return cut()
    scv = A.f32("scv", [8, 2])
    P.act(scv[:, :, 0], VT1[:, :, R_C], AF.Silu)
    P.act(scv[:, :, 1], VT1[:, :, R_CC], AF.Silu)
    aw = [A.f32(f"aw{k}", [8, 768]) for k in range(2)]
    q = 0
    for i in range(2):
        for cc in range(8):
            w = aw[q % 2]
            P.dma("sp" if q % 2 == 0 else "act", w, dram(adaw_d[i, :, cc * 768:(cc + 1) * 768].rearrange("(kc p) n -> p kc n", p=128), "adawd"))
            for mloc in range(6):
                mch = cc * 6 + mloc
                pk = PS[2 + (mch % 2)]
                for k in range(8):
                    P.mm(pk[:, 0:2], w[:, k, mloc * 128:(mloc + 1) * 128], scv[:, k, :], start=(k == 0), stop=(k == 7))
                P.ts("dve", MOD[:, i, mch, :], pk[:, 0:2], VT1[:, mch % 8, R_ADAB + i * 6 + mch // 8: R_ADAB + i * 6 + mch // 8 + 1], ALU.add)
            q += 1
    if CUT == 2:
        return cut()
    for i in range(2):
        for s in range(2):
            P.stt("dve", DER[:, i, 0, :, s], MOD[:, i, 8:16, s], 1.0, VT1[:, :, R_N1G + i], ALU.add, ALU.mult)
            P.stt("dve", DER[:, i, 1, :, s], MOD[:, i, 32:40, s], 1.0, VT1[:, :, R_N2G + i], ALU.add, ALU.mult)
        if i == 0:
            for s in range(2):
                P.tt("dve", DER[:, 0, 2, :, s], MOD[:, 0, 16:24, s], VT1[:, :, R_PS], ALU.mult)
                P.tt("dve", DER[:, 0, 3, :, s], DER[:, 0, 2, :, s], VT1[:, :, R_PB], ALU.mult)

    if CUT == 3:
        return cut()
    xin = [A.f32(f"xin{k}", [D]) for k in range(3)]
    for tt in range(18):
        xi = xin[tt % 3]
        src = x_d[tt * 128:(tt + 1) * 128, :] if tt < 16 else ctx_d[(tt - 16) * 128:(tt - 15) * 128, :]
        P.dma("sp" if tt % 2 == 0 else "act", xi, dram(src, "xd"))
        for half in range(2):
            pk = PS[4 + (2 * tt + half) % 4]
            for jj in range(4):
                j = half * 4 + jj
                P.mm(pk[:, jj * 128:(jj + 1) * 128], xi[:, j * 128:(j + 1) * 128], identf)
            dst = TV(xT.ap[:, half * 4:(half + 1) * 4, tt * 128:(tt + 1) * 128], None)
            srcv = pk.rearrange("p (a b) -> p a b", a=4)
            if half == 0:
                P.op("dve", lambda e, d=dst, s_=srcv: e.tensor_copy(out=d.ap, in_=s_.ap), reads=[pk], writes=xTc[0:4])
            else:
                P.op("act", lambda e, d=dst, s_=srcv: e.activation(out=d.ap, in_=s_.ap, func=AF.Copy), reads=[pk], writes=xTc[4:8])
    if stage >= 2:
        ZOFF = 51600
        zt = TV(A.t[:, ZOFF:ZOFF + 1024].bitcast(BF16), Buf("zt"))
        zt.buf.semgrp.bg = True
        xs_t = TV(xs_d, Buf("xs_scr", dram=True))
        ys_t = TV(ys_d, Buf("ys_scr", dram=True))
        P.memset("pool", zt, 0.0)
        for r in range(0, NSLOT, 256):
            P.dma("sp", TV(xs_d[r:r + 256, :].rearrange("(p a) n -> p (a n)", p=128), xs_t.buf), zt, owner=zt)
        ztf = TV(zt.ap.bitcast(F32), zt.buf)
        for r in range(0, NSLOT, 128):
            P.dma("sp", TV(ys_d[r:r + 128, :], ys_t.buf), ztf, owner=zt)
        P.dma("sp", TV(xs_d[NSLOT:NSLOT + 1, :], xs_t.buf), zt[0:1, 0:D], owner=zt)
        P.dma("sp", TV(ys_d[NSLOT:NSLOT + 1, :], ys_t.buf), ztf[0:1, 0:D], owner=zt)

    P.barrier()
    A.reset(m0)

    def dump_and_finish():
        for j in range(8):
            P.dma("sp", dram(dbg_d[:, j, :], "dbgd"), xTc[j])
        P.barrier()
        P.emit()
        return nc, st, P

    if stage == 0:
        return dump_and_finish()

    BLK = [(0, 512), (512, 512), (1024, 512), (1536, 512), (2048, 256)]

    def rstd_all(rstd, ntok_blocks, src_chunks, nfeat, sq_pool, pss):
        nch = len(src_chunks)
        for bi, (t0, n) in enumerate(ntok_blocks):
            pk = pss[bi % len(pss)]
            for j in range(nch):
                sq = sq_pool[(bi * nch + j) % len(sq_pool)]
                P.act(sq[:, 0:n], src_chunks[j][:, t0:t0 + n], AF.Square)
                P.mm(pk[:, 0:n], onesf, sq[:, 0:n], start=(j == 0), stop=(j == nch - 1))
            P.ts("dve", rstd[:, t0:t0 + n], pk[:, 0:n], 1.0 / nfeat, ALU.mult, EPS, ALU.add)
            P.act(rstd[:, t0:t0 + n], rstd[:, t0:t0 + n], AF.Sqrt)
            P.op("dve", lambda e, o=rstd[:, t0:t0 + n]: e.reciprocal(out=o.ap, in_=o.ap), reads=[rstd], writes=[rstd])

    m1 = A.mark()
    rstd = A.f32("rstd", [T])
    sqp = [A.f32(f"sq{k}", [512]) for k in range(3)]
    rstd_all(rstd, BLK, xTc, D, sqp, [PS[0], PS[1]])
    WIN = (2, 4, 8, 16)
    PADW = 16
    segs = ((0, L, 0), (L, C, 1))
    hp = [A.f32(f"hp{s}", [ln + 2 * PADW]) for (_, ln, s) in segs]
    sa = [A.f32(f"sa{s}", [ln + 2 * PADW]) for (_, ln, s) in segs]
    sb = [A.f32(f"sb{s}", [ln + 2 * PADW]) for (_, ln, s) in segs]
    icnt = [[A.f32(f"ic{s}_{g}", [ln]) for g in range(4)] for (_, ln, s) in segs]
    dT = A.b16("dT", [2, T])
    uu = A.f32("uu", [T])
    pw = A.b16("pw", [4, 2, 256])
    P.dma("pool", pw, dram(poolw_d.rearrange("g (kc p) n -> p g kc n", p=128), "pwd"))

    def window_sum(si, w, src, dst_final):
        ln = segs[si][1]
        W = ln + 2 * PADW
        cur = src
        bufs = [sa[si], sb[si]]
        step = 1
        k = 0
        ww = 2
        while ww <= w:
            dst = bufs[k % 2]
            if ww == 2:
                P.tt("dve", dst[:, 1:W], cur[:, 0:W - 1], cur[:, 1:W], ALU.add)
            else:
                sh = ww // 4
                P.tt("dve", dst[:, sh:W - sh], cur[:, 0:W - 2 * sh], cur[:, 2 * sh:W], ALU.add)
            cur = dst
            k += 1
            ww *= 2
        return cur

    for si, (t0, ln, s) in enumerate(segs):
        for bufz in (hp[si], sa[si], sb[si]):
            P.memset("pool", bufz, 0.0)
    for si, (t0, ln, s) in enumerate(segs):
        P.memset("pool", hp[si][:, PADW:PADW + ln], 1.0)
        for g in range(4):
            cur = window_sum(si, WIN[g], hp[si], None)
            P.op("dve", lambda e, o=icnt[si][g], c=cur, ln=ln: e.reciprocal(out=o.ap, in_=c.ap[:, PADW:PADW + ln]),
                 reads=[cur], writes=[icnt[si][g]])
    for g in range(4):
        for kc in range(2):
            j = 2 * g + kc
            P.tt("dve", uu, xTc[j], rstd, ALU.mult)
            for si, (t0, ln, s) in enumerate(segs):
                P.act(hp[si][:, PADW:PADW + ln], uu[:, t0:t0 + ln], AF.Identity,
                      bias=MOD[:, 0, 0 + j, s:s + 1], scale=DER[:, 0, 0, j, s:s + 1])
                cur = window_sum(si, WIN[g], hp[si], None)
                P.tt("pool", cur[:, PADW:PADW + ln], cur[:, PADW:PADW + ln], icnt[si][g], ALU.mult)
                P.tt("pool", dT[:, kc, t0:t0 + ln], cur[:, PADW:PADW + ln], hp[si][:, PADW:PADW + ln], ALU.subtract)
        for mc in range(2):
            j = 2 * g + mc
            for bi, (t0, n) in enumerate(BLK):
                s = 0 if t0 < L else 1
                pk = PS[2 + (bi % 4)]
                for kc in range(2):
                    P.mm(pk[:, 0:n], pw[:, g, kc, mc * 128:(mc + 1) * 128], dT[:, kc, t0:t0 + n], start=(kc == 0), stop=(kc == 1))
                P.stt("dve", xTc[j][:, t0:t0 + n], pk[:, 0:n], DER[:, 0, 2, j, s:s + 1], xTc[j][:, t0:t0 + n], ALU.mult, ALU.add)
                P.ts("dve", xTc[j][:, t0:t0 + n], xTc[j][:, t0:t0 + n], DER[:, 0, 3, j, s:s + 1], ALU.add)
    P.barrier()
    A.reset(m1)

    if stage == 1:
        return dump_and_finish()

    REG = {}

    def mkregs(e):
        for nm, val in (("sc", NSLOT - 1), ("ga", NSLOT)):
            r = e.alloc_register(f"bc_{nm}")
            e.reg_mov(r, val)
            REG[nm] = r
        return e.nop()
    P.op("pool", mkregs)
    def moe(layer, ntt):
        ntok = ntt * 128
        blks = [b for b in BLK if b[0] < ntok]
        mm0 = A.mark()
        slot_i = A.i32("slot_i", [18, 4])
        gates = A.f32("gates", [18, 4])
        G = A.f32("G", [18, NE])
        masks = A.b16("masks", [18, NE])
        ebase = A.f32("ebase", [NE])
        ebi = A.i32("ebi", [NE])
        rw = A.f32("rw", [8, NE])
        rb = A.f32("rb", [NE])
        bdT = A.f32("bdT", [D])
        flags_f = A.f32("flags_f", [4, NE]); flags_i = A.i32("flags_i", [4, NE])
        P.op("pool", lambda e: e.iota(ebi.ap, pattern=[[CAP, NE]], base=0, channel_multiplier=0), writes=[ebi])
        P.copy("dve", ebase, ebi)
        P.dma("sp", rw, dram(rw_d[layer].rearrange("(kc p) n -> p kc n", p=128), "rwd"))
        P.dma("sp", rb[0:1, :], dram(rb_d[layer:layer + 1, :], "rbd"))
        P.dma("sp", bdT[0:NE, :], dram(bdn_d[layer], "bdd"))
        ma = A.mark()
        rstd2 = A.f32("rstd2", [T])
        sq2 = [A.f32(f"sq2{k}", [512]) for k in range(3)]
        rstd_all(rstd2, blks, xTc, D, sq2, [PS[0], PS[1]])
        u2 = [A.f32(f"u2{k}", [128]) for k in range(2)]
        h2f = [A.f32(f"h2f{k}", [8, 128]) for k in range(2)]
        h2b = [A.b16(f"h2b{k}", [8, 128]) for k in range(2)]
        h2tok = [A.b16(f"h2tok{k}", [D]) for k in range(2)]
        lg = A.f32("lg", [NE]); top8 = A.f32("top8", [8]); maskf = A.f32("maskf", [NE])
        pos = A.f32("pos", [NE]); t1 = A.f32("t1", [NE]); oh = A.f32("oh", [NE]); sel = A.f32("sel", [NE])
        slotsf = A.f32("slotsf", [4]); negm = A.f32("negm", [1]); ex = A.f32("ex", [4]); sumex = A.f32("sumex", [1])
        for tt in range(ntt):
            s = 0 if tt < 16 else 1
            tok = slice(tt * 128, (tt + 1) * 128)
            hf = h2f[tt % 2]; hb = h2b[tt % 2]
            for j in range(8):
                u = u2[j % 2]
                P.tt("dve", u, xTc[j][:, tok], rstd2[:, tok], ALU.mult)
                P.act(hf[:, j, :], u, AF.Identity, bias=MOD[:, layer, 24 + j, s:s + 1], scale=DER[:, layer, 1, j, s:s + 1])
                P.act(hb[:, j, :], u, AF.Identity, bias=MOD[:, layer, 24 + j, s:s + 1], scale=DER[:, layer, 1, j, s:s + 1])
            pl = PS[2 + tt % 2]
            for j in range(8):
                P.mm(pl[:, 0:NE], hf[:, j, :], rw[:, j, :], start=(j == 0), stop=False)
            P.mm(pl[:, 0:NE], onesf[0:1, :], rb[0:1, :], start=False, stop=True)
            P.copy("dve", lg, pl[:, 0:NE])
            P.op("dve", lambda e: e.max(out=top8.ap, in_=lg.ap), reads=[lg], writes=[top8])
            P.ts("dve", maskf, lg, top8[:, 3:4], ALU.is_ge)
            P.copy("dve", masks[:, tt, :], maskf)
            pp = PS[4 + tt % 2]
            P.mm(pp[:, 0:NE], tri, masks[:, tt, :], start=True, stop=(tt == 0))
            for i in range(tt):
                P.mm(pp[:, 0:NE], onesb, masks[:, i, :], start=False, stop=(i == tt - 1))
            P.copy("dve", pos, pp[:, 0:NE])
            P.ts("dve", t1, pos, float(CAP), ALU.is_ge, 1.0e6, ALU.mult)
            P.tt("dve", pos, pos, ebase, ALU.add)
            P.tt("dve", pos, pos, t1, ALU.add)
            P.ts("dve", pos, pos, float(NSLOT), ALU.min)
            P.ts("dve", negm, top8[:, 0:1], -1.0, ALU.mult)
            P.act(ex, top8[:, 0:4], AF.Exp, bias=negm, accum_out=sumex)
            P.op("dve", lambda e: e.reciprocal(out=sumex.ap, in_=sumex.ap), reads=[sumex], writes=[sumex])
            P.ts("dve", gates[:, tt, :], ex, sumex, ALU.mult)
            for k in range(4):
                P.ts("dve", oh, lg, top8[:, k:k + 1], ALU.is_equal)
                P.tt("dve", sel, oh, pos, ALU.mult)
                P.red("dve", slotsf[:, k:k + 1], sel, ALU.add)
                if k == 0:
                    P.ts("dve", G[:, tt, :], oh, gates[:, tt, 0:1], ALU.mult)
                else:
                    P.stt("dve", G[:, tt, :], oh, gates[:, tt, k:k + 1], G[:, tt, :], ALU.mult, ALU.add)
            P.copy("dve", slot_i[:, tt, :], slotsf)
            pt_ = PS[6 + tt % 2]
            ptb = TV(pt_.ap.bitcast(BF16), pt_.buf)
            for j in range(8):
                P.tr(ptb[:, j * 128:(j + 1) * 128], hb[:, j, :], ident)
            ht = h2tok[tt % 2]
            P.copy("act", ht, ptb)
            for k in range(4):
                P.op("pool", lambda e, ht=ht, tt=tt, k=k: e.indirect_dma_start(
                    out=xs_d, out_offset=bass.IndirectOffsetOnAxis(ap=slot_i.ap[:, tt, k:k + 1], axis=0),
                    in_=ht.ap, in_offset=None, bounds_check=REG["sc"], oob_is_err=False),
                    reads=[ht, slot_i], writes=[xs_t], dma=ht)
        pc = PS[0]
        for tt in range(ntt):
            P.mm(pc[:, 0:NE], onesb, masks[:, tt, :], start=(tt == 0), stop=(tt == ntt - 1))
        for q in range(4):
            P.ts("dve", flags_f[:, q, :], pc[:, 0:NE], float(q * 256), ALU.is_gt)
        P.copy("dve", flags_i, flags_f)
        P.barrier()
        A.reset(ma)
        BW = 256
        NQ = CAP // BW
        NRING = 9
        ring = [A.b16(f"ring{k}", [8, 512]) for k in range(NRING)]
        XeT = [A.b16(f"XeT{k}", [8, BW]) for k in range(2)]
        actT = [A.b16(f"actT{k}", [8, BW]) for k in range(2)]
        xrow = [A.b16(f"xrow{k}", [D]) for k in range(2)]
        yout = [A.f32(f"yout{k}", [D]) for k in range(2)]
        gsb = [A.f32(f"gsb{k}", [BW]) for k in range(2)]
        sgb = [A.f32(f"sgb{k}", [BW]) for k in range(2)]
        usb = [A.f32(f"usb{k}", [BW]) for k in range(2)]
        GORD = (0, 2, 1, 3)
        nq = 0
        cnt_it = [0, 0, 0]
        for e in range(NE):
            wg = {}
            for g in GORD:
                r = ring[nq % NRING]; nq += 1
                P.dma("pool", r, dram(wgu_d[layer, e, :, g * 512:(g + 1) * 512].rearrange("(kc p) n -> p kc n", p=128), "wgud"))
                wg[g] = r
            wd = {}
            for g in range(2):
                r = ring[nq % NRING]; nq += 1
                P.dma("pool", r, dram(wdn_d[layer, e, :, g * 512:(g + 1) * 512].rearrange("(kc p) n -> p kc n", p=128), "wdnd"))
                wd[g] = r
            row = (layer * NE + e) * 2

            def regionT(q):
                P.begin_region(flags_i[0:1, q, e:e + 1], engines=("pe", "act", "sp"))
                xe = XeT[q % 2]
                for t in range(BW // 128):
                    stl = q * (BW // 128) + t
                    xr = xrow[cnt_it[0] % 2]
                    pk = PS[6 + cnt_it[0] % 2]
                    cnt_it[0] += 1
                    P.dma("sp", xr, TV(xs_d[e * CAP + stl * 128: e * CAP + (stl + 1) * 128, :], xs_t.buf), owner=xr)
                    pkb = TV(pk.ap.bitcast(BF16), pk.buf)
                    for j in range(8):
                        P.tr(pkb[:, j * 128:(j + 1) * 128], xr[:, j * 128:(j + 1) * 128], ident)
                    P.op("act", lambda e_, pkb=pkb, t=t, xe=xe: e_.activation(
                        out=xe.ap[:, :, t * 128:(t + 1) * 128], in_=pkb.ap.rearrange("p (a b) -> p a b", a=8), func=AF.Copy),
                        reads=[pkb], writes=[xe])
                P.end_region()

            def regionG(q):
                P.begin_region(flags_i[0:1, q, e:e + 1], engines=("pe", "act", "dve"))
                xe = XeT[q % 2]
                for j in range(8):
                    it = cnt_it[1]; cnt_it[1] += 1
                    pg = PS[(2 * it) % 4]; pu = PS[(2 * it + 1) % 4]
                    cg = (j % 4) * 128
                    for k in range(8):
                        P.mm(pg[:, 0:BW], wg[j // 4][:, k, cg:cg + 128], xe[:, k, :], start=(k == 0), stop=(k == 7))
                    for k in range(8):
                        P.mm(pu[:, 0:BW], wg[2 + j // 4][:, k, cg:cg + 128], xe[:, k, :], start=(k == 0), stop=(k == 7))
                    gs = gsb[it % 2]; sg = sgb[it % 2]; us = usb[it % 2]
                    P.ts("dve", gs, pg[:, 0:BW], VT2[:, j, row:row + 1], ALU.add, 7.0, ALU.min)
                    P.act(sg, gs, AF.Sigmoid, scale=1.702)
                    P.ts("dve", us, pu[:, 0:BW], VT2[:, j, row + 1:row + 2], ALU.add, 7.0, ALU.min)
                    P.ts("dve", us, us, -7.0, ALU.max, 1.0, ALU.add)
                    P.tt("dve", gs, gs, sg, ALU.mult)
                    P.tt("dve", actT[q % 2][:, j, :], gs, us, ALU.mult)
                P.end_region()

            def regionB(q):
                P.begin_region(flags_i[0:1, q, e:e + 1], engines=("pe", "act", "sp"))
                for t in range(BW // 128):
                    stl = q * (BW // 128) + t
                    yo = yout[cnt_it[2] % 2]
                    for n2 in range(2):
                        pd = PS[4 + (2 * cnt_it[2] + n2) % 2]
                        for k in range(8):
                            P.mm(pd, actT[q % 2][:, k, t * 128:(t + 1) * 128], wd[n2][:, k, :], start=(k == 0), stop=(k == 7))
                        P.copy("act", yo[:, n2 * 512:(n2 + 1) * 512], pd)
                    cnt_it[2] += 1
                    P.dma("sp", TV(ys_d[e * CAP + stl * 128: e * CAP + (stl + 1) * 128, :], ys_t.buf), yo, owner=yo)
                P.end_region()

            regionT(0)
            for q in range(NQ):
                if q + 1 < NQ:
                    regionT(q + 1)
                regionG(q)
                if q >= 1:
                    regionB(q - 1)
            regionB(NQ - 1)
        P.barrier()
        A.reset(ma)
        yk = [A.f32(f"yk{k}", [D]) for k in range(4)]
        acc = [A.f32(f"acc{k}", [D]) for k in range(2)]
        GT = [A.f32(f"GT{k}", [128]) for k in range(2)]
        for tt in range(ntt):
            s = 0 if tt < 16 else 1
            tok = slice(tt * 128, (tt + 1) * 128)
            ac = acc[tt % 2]
            for k in range(4):
                P.op("pool", lambda e, tt=tt, k=k: e.indirect_dma_start(
                    out=yk[k].ap, out_offset=None, in_=ys_d,
                    in_offset=bass.IndirectOffsetOnAxis(ap=slot_i.ap[:, tt, k:k + 1], axis=0),
                    bounds_check=REG["ga"], oob_is_err=False),
                    reads=[ys_t, slot_i], writes=[yk[k]], dma=yk[k])
                if k == 0:
                    P.ts("dve", ac, yk[0], gates[:, tt, 0:1], ALU.mult)
                else:
                    P.stt("dve", ac, yk[k], gates[:, tt, k:k + 1], ac, ALU.mult, ALU.add)
            pgt = PS[tt % 2]
            P.mm(pgt[0:NE, 0:128], G[:, tt, :], identf)
            gt = GT[tt % 2]
            P.copy("act", gt[0:NE, :], pgt[0:NE, 0:128])
            for j in range(8):
                pj = PS[2 + j % 4]
                P.mm(pj[:, 0:128], ac[:, j * 128:(j + 1) * 128], identf, start=True, stop=False)
                P.mm(pj[:, 0:128], bdT[0:NE, j * 128:(j + 1) * 128], gt[0:NE, :], start=False, stop=True)
                P.stt("dve", xTc[j][:, tok], pj[:, 0:128], MOD[:, layer, 40 + j, s:s + 1], xTc[j][:, tok], ALU.mult, ALU.add)
        P.barrier()
        A.reset(mm0)

    P.barrier(skip_bg=False)

    moe(0, 18)
    if stage == 2:
        return dump_and_finish()

    LBLK = BLK[0:4]
    m3 = A.mark()
    cqn = A.b16("cqn", [4, L])
    ckvn = A.b16("ckvn", [2, T])
    kpe = A.b16("kpe", [T])
    COS = A.f32("COS", [L]); SINS = A.f32("SINS", [L])
    m3t = A.mark()
    ti = A.i32("ti", [L]); pi_ = A.i32("pi", [1]); pf = A.f32("pf", [1]); invf = A.f32("invf", [1]); sgn = A.f32("sgn", [1])
    ang = A.f32("ang", [L]); nn = A.i32("nn", [L]); nf = A.f32("nf", [L]); fx = A.f32("fx", [L])
    R64 = slice(0, 64)
    P.op("pool", lambda e: e.iota(ti.ap[R64], pattern=[[1, L]], base=0, channel_multiplier=0), writes=[ti])
    P.op("pool", lambda e: e.iota(pi_.ap[R64], pattern=[[0, 1]], base=0, channel_multiplier=1), writes=[pi_])
    P.op("dve", lambda e: e.tensor_single_scalar(out=ti.ap[0:32], in_=ti.ap[0:32], scalar=6, op=ALU.arith_shift_right), reads=[ti], writes=[ti])
    P.op("dve", lambda e: e.tensor_single_scalar(out=ti.ap[32:64], in_=ti.ap[32:64], scalar=63, op=ALU.bitwise_and), reads=[ti], writes=[ti])
    P.copy("dve", ang[R64], ti[R64])
    P.op("dve", lambda e: e.tensor_single_scalar(out=nn.ap[R64, 0:1], in_=pi_.ap[R64], scalar=15, op=ALU.bitwise_and), reads=[pi_], writes=[nn])
    P.copy("dve", pf[R64], nn[R64, 0:1])
    P.act(invf[R64], pf[R64], AF.Exp, scale=-float(np.log(10000.0)) / 16.0)
    P.op("dve", lambda e: e.tensor_scalar(out=nn.ap[R64, 1:2], in0=pi_.ap[R64], scalar1=4, scalar2=1, op0=ALU.arith_shift_right, op1=ALU.bitwise_and), reads=[pi_], writes=[nn])
    P.copy("dve", sgn[R64], nn[R64, 1:2])
    P.ts("dve", sgn[R64], sgn[R64], 2.0, ALU.mult, -1.0, ALU.add)
    P.ts("dve", ang[R64], ang[R64], invf[R64], ALU.mult)
    TWO_PI = float(2 * np.pi)
    for (dst, shift) in ((SINS, 0.0), (COS, float(np.pi / 2))):
        P.ts("dve", nf[R64], ang[R64], shift, ALU.add, 1.0 / TWO_PI, ALU.mult)
        P.copy("dve", nn[R64], nf[R64])
        P.copy("dve", nf[R64], nn[R64])
        P.stt("dve", fx[R64], nf[R64], -TWO_PI, ang[R64], ALU.mult, ALU.add)
        if shift != 0.0:
            P.ts("dve", fx[R64], fx[R64], shift, ALU.add)
        P.ts("dve", nf[R64], fx[R64], -float(np.pi), ALU.is_lt, TWO_PI, ALU.mult)
        P.tt("dve", fx[R64], fx[R64], nf[R64], ALU.add)
        P.ts("dve", nf[R64], fx[R64], float(np.pi), ALU.is_gt, -TWO_PI, ALU.mult)
        P.tt("dve", fx[R64], fx[R64], nf[R64], ALU.add)
        P.act(dst[R64], fx[R64], AF.Sin)
    P.ts("dve", SINS[R64], SINS[R64], sgn[R64], ALU.mult)
    P.barrier()
    A.reset(m3t)
    hT = A.b16("hT", [8, T])
    wdq = A.b16("wdq", [8, 512]); wdkv = A.b16("wdkv", [8, 384])
    P.dma("pool", wdq, dram(wdq_d.rearrange("(kc p) n -> p kc n", p=128), "wdqd"))
    P.dma("pool", wdkv, dram(wdkv_d.rearrange("(kc p) n -> p kc n", p=128), "wdkvd"))
    m3a = A.mark()
    rstd1 = A.f32("rstd1", [T])
    sq3 = [A.f32(f"sq3{k}", [512]) for k in range(3)]
    rstd_all(rstd1, BLK, xTc, D, sq3, [PS[0], PS[1]])
    u3 = [A.f32(f"u3{k}", [512]) for k in range(2)]
    it3 = 0
    for j in range(8):
        for (t0, n) in BLK:
            s = 0 if t0 < L else 1
            u = u3[it3 % 2]; it3 += 1
            P.tt("dve", u[:, 0:n], xTc[j][:, t0:t0 + n], rstd1[:, t0:t0 + n], ALU.mult)
            P.act(hT[:, j, t0:t0 + n], u[:, 0:n], AF.Identity, bias=MOD[:, 1, 0 + j, s:s + 1], scale=DER[:, 1, 0, j, s:s + 1])
    P.barrier()
    A.reset(m3a)
    sq3 = [A.f32(f"sq3{k}", [512]) for k in range(3)]
    cqf = A.f32("cqf", [4, 512]); ckf = A.f32("ckf", [2, 512]); rq = A.f32("rq", [512]); rk = A.f32("rk", [512])
    ta = A.f32("ta", [512]); tb = A.f32("tb", [512])
    for bi, (t0, n) in enumerate(BLK):
        lat = t0 < L
        if lat:
            for m in range(4):
                pk = PS[2 + m % 2]
                for k in range(8):
                    P.mm(pk[:, 0:n], wdq[:, k, m * 128:(m + 1) * 128], hT[:, k, t0:t0 + n], start=(k == 0), stop=(k == 7))
                P.copy("act", cqf[:, m, 0:n], pk[:, 0:n])
            rstd_all(rq, [(0, n)], [cqf[:, m, :] for m in range(4)], 512, sq3, [PS[0]])
            for m in range(4):
                P.tt("dve", cqf[:, m, 0:n], cqf[:, m, 0:n], rq[:, 0:n], ALU.mult)
                P.act(cqn[:, m, t0:t0 + n], cqf[:, m, 0:n], AF.Copy, scale=VT1[:, m, R_QG:R_QG + 1])
        for m in range(2):
            pk = PS[4 + m % 2]
            for k in range(8):
                P.mm(pk[:, 0:n], wdkv[:, k, m * 128:(m + 1) * 128], hT[:, k, t0:t0 + n], start=(k == 0), stop=(k == 7))
            P.copy("act", ckf[:, m, 0:n], pk[:, 0:n])
        rstd_all(rk, [(0, n)], [ckf[:, m, :] for m in range(2)], 256, sq3, [PS[1]])
        for m in range(2):
            P.tt("dve", ckf[:, m, 0:n], ckf[:, m, 0:n], rk[:, 0:n], ALU.mult)
            P.act(ckvn[:, m, t0:t0 + n], ckf[:, m, 0:n], AF.Copy, scale=VT1[:, m, R_KG:R_KG + 1])
        pa = PS[6]; pb = PS[7]
        for k in range(8):
            P.mm(pa[0:64, 0:n], wdkv[:, k, 256:320], hT[:, k, t0:t0 + n], start=(k == 0), stop=(k == 7))
        if lat:
            for k in range(8):
                P.mm(pb[0:64, 0:n], wdkv[:, k, 320:384], hT[:, k, t0:t0 + n], start=(k == 0), stop=(k == 7))
            P.tt("dve", ta[R64, 0:n], pa[0:64, 0:n], COS[R64, t0:t0 + n], ALU.mult)
            P.tt("dve", tb[R64, 0:n], pb[0:64, 0:n], SINS[R64, t0:t0 + n], ALU.mult)
            P.tt("dve", kpe[R64, t0:t0 + n], ta[R64, 0:n], tb[R64, 0:n], ALU.add)
        else:
            P.copy("dve", kpe[R64, t0:t0 + n], pa[0:64, 0:n])
    P.barrier()
    A.reset(m3t)
    wo = [A.b16(f"wo{k}", [D]) for k in range(2)]
    wq = [A.b16(f"wq{k}", [4, 128]) for k in range(2)]
    wr = [A.b16(f"wr{k}", [4, 128]) for k in range(2)]
    wk = [A.b16(f"wk{k}", [2, 128]) for k in range(2)]
    wv = [A.b16(f"wv{k}", [2, 128]) for k in range(2)]
    qn = A.b16("qn", [L]); qr = A.b16("qr", [L]); kn = A.b16("kn", [T]); vh = A.b16("vh", [18, 128])
    attT = A.b16("attT", [L])
    S_sb = [A.f32(f"S_sb{k}", [T]) for k in range(2)]
    Pb = [A.b16(f"Pb{k}", [T]) for k in range(2)]
    PT = [A.b16(f"PT{k}", [18, 128]) for k in range(2)]
    mx = [A.f32(f"mx{k}", [1]) for k in range(2)]
    rsum = [A.f32(f"rsum{k}", [1]) for k in range(2)]
    onb = [A.b16(f"onb{k}", [128]) for k in range(2)]
    qt_ = A.f32("qt_", [512]); ta = A.f32("ta", [512]); tb = A.f32("tb", [512])
    for h in range(8):
        b = h % 2
        P.dma("pool", wq[b], dram(wuqn_d[:, h * 128:(h + 1) * 128].rearrange("(kc p) n -> p kc n", p=128), "wqd"))
        P.dma("pool", wr[b], dram(wuqr_d[:, h * 128:(h + 1) * 128].rearrange("(kc p) n -> p kc n", p=128), "wrd"))
        P.dma("pool", wk[b], dram(wuk_d[:, h * 128:(h + 1) * 128].rearrange("(kc p) n -> p kc n", p=128), "wkd"))
        P.dma("pool", wv[b], dram(wuv_d[:, h * 128:(h + 1) * 128].rearrange("(kc p) n -> p kc n", p=128), "wvd"))
        P.dma("pool", wo[b], dram(wo_d[h * 128:(h + 1) * 128, :], "wod"))
        for bi, (t0, n) in enumerate(LBLK):
            pk = PS[bi % 2]
            for k in range(4):
                P.mm(pk[:, 0:n], wq[b][:, k, :], cqn[:, k, t0:t0 + n], start=(k == 0), stop=(k == 3))
            P.act(qn[:, t0:t0 + n], pk[:, 0:n], AF.Copy, scale=ATTN_SCALE)
            pa = PS[2]; pb = PS[3]
            for k in range(4):
                P.mm(pa[0:64, 0:n], wr[b][:, k, 0:64], cqn[:, k, t0:t0 + n], start=(k == 0), stop=(k == 3))
            for k in range(4):
                P.mm(pb[0:64, 0:n], wr[b][:, k, 64:128], cqn[:, k, t0:t0 + n], start=(k == 0), stop=(k == 3))
            P.tt("dve", ta[R64, 0:n], pa[0:64, 0:n], COS[R64, t0:t0 + n], ALU.mult)
            P.tt("dve", tb[R64, 0:n], pb[0:64, 0:n], SINS[R64, t0:t0 + n], ALU.mult)
            P.tt("dve", qt_[R64, 0:n], ta[R64, 0:n], tb[R64, 0:n], ALU.add)
            P.act(qr[R64, t0:t0 + n], qt_[R64, 0:n], AF.Copy, scale=ATTN_SCALE)
        for bi, (t0, n) in enumerate(BLK):
            pk = PS[4 + bi % 2]
            for k in range(2):
                P.mm(pk[:, 0:n], wk[b][:, k, :], ckvn[:, k, t0:t0 + n], start=(k == 0), stop=(k == 1))
            P.copy("act", kn[:, t0:t0 + n], pk[:, 0:n])
        for kt in range(18):
            pk = PS[6 + kt % 2]
            for k in range(2):
                P.mm(pk[:, 0:128], ckvn[:, k, kt * 128:(kt + 1) * 128], wv[b][:, k, :], start=(k == 0), stop=(k == 1))
            P.copy("dve", vh[:, kt, :], pk[:, 0:128])

        def scores(qt):
            qs = slice(qt * 128, (qt + 1) * 128)
            c = qt % 2
            for bi, (t0, n) in enumerate(BLK):
                pk = PS[bi % 4]
                P.mm(pk[:, 0:n], qn[:, qs], kn[:, t0:t0 + n], start=True, stop=False)
                P.mm(pk[:, 0:n], qr[R64, qs], kpe[R64, t0:t0 + n], start=False, stop=True)
                P.copy("act", S_sb[c][:, t0:t0 + n], pk[:, 0:n])
            P.red("dve", mx[c], S_sb[c], ALU.max)
            P.ts("dve", mx[c], mx[c], -1.0, ALU.mult)
            P.act(Pb[c], S_sb[c], AF.Exp, bias=mx[c], accum_out=rsum[c])
            P.op("dve", lambda e, r=rsum[c]: e.reciprocal(out=r.ap, in_=r.ap), reads=[rsum[c]], writes=[rsum[c]])

        def attend_tile(qt):
            qs = slice(qt * 128, (qt + 1) * 128)
            c = qt % 2
            for g0 in range(0, 18, 8):
                ng = min(8, 18 - g0)
                pk = PS[4 + (g0 // 8) % 2]
                pkb = TV(pk.ap.bitcast(BF16), pk.buf)
                for kk in range(ng):
                    kt = g0 + kk
                    P.tr(pkb[:, kk * 128:(kk + 1) * 128], Pb[c][:, kt * 128:(kt + 1) * 128], ident)
                P.op("dve", lambda e, pkb=pkb, g0=g0, ng=ng, c=c: e.tensor_copy(
                    out=PT[c].ap[:, g0:g0 + ng, :], in_=pkb.ap[:, 0:ng * 128].rearrange("p (a b) -> p a b", a=ng)),
                    reads=[pkb], writes=[PT[c]])
            po = PS[6]
            for kt in range(18):
                P.mm(po[:, 0:128], PT[c][:, kt, :], vh[:, kt, :], start=(kt == 0), stop=(kt == 17))
            P.ts("dve", onb[c], po[:, 0:128], rsum[c], ALU.mult)
            pt2 = PS[7]
            pt2b = TV(pt2.ap.bitcast(BF16), pt2.buf)
            P.tr(pt2b[:, 0:128], onb[c], ident)
            P.copy("act", attT[:, qs], pt2b[:, 0:128])

        scores(0)
        for qt in range(16):
            if qt + 1 < 16:
                scores(qt + 1)
            attend_tile(qt)
        for m in range(8):
            for bi, (t0, n) in enumerate(LBLK):
                pk = PS[(m * 4 + bi) % 4]
                P.mm(pk[:, 0:n], wo[b][:, m * 128:(m + 1) * 128], attT[:, t0:t0 + n])
                P.stt("dve", xTc[m][:, t0:t0 + n], pk[:, 0:n], MOD[:, 1, 16 + m, 0:1], xTc[m][:, t0:t0 + n], ALU.mult, ALU.add)
    P.barrier()
    A.reset(m3)
    if stage == 3:
        return dump_and_finish()

    moe(1, 16)
    if stage == 4:
        return dump_and_finish()

    fgb = A.f32("fgb", [D])
    P.dma("sp", fgb, dram(v1_d[R_FG:R_FG + 1, :].to_broadcast([128, D]), "fgd"))
    xo = [A.f32(f"xo{k}", [D]) for k in range(2)]
    sqo = A.f32("sqo", [D]); ssum = A.f32("ssum", [1])
    for tt in range(16):
        tok = slice(tt * 128, (tt + 1) * 128)
        o = xo[tt % 2]
        for half in range(2):
            pk = PS[(2 * tt + half) % 4]
            for jj in range(4):
                P.mm(pk[:, jj * 128:(jj + 1) * 128], xTc[half * 4 + jj][:, tok], identf)
            P.copy("act", o[:, half * 512:(half + 1) * 512], pk)
        P.act(sqo, o, AF.Square, accum_out=ssum)
        P.ts("dve", ssum, ssum, 1.0 / D, ALU.mult, EPS, ALU.add)
        P.act(ssum, ssum, AF.Sqrt)
        P.op("dve", lambda e: e.reciprocal(out=ssum.ap, in_=ssum.ap), reads=[ssum], writes=[ssum])
        P.stt("dve", o, o, ssum, fgb, ALU.mult, ALU.mult)
        P.dma("sp", dram(out_d[tt * 128:(tt + 1) * 128, :], "outd"), o)
    P.barrier()
    P.emit()
    return nc, st, P


def _prep_inputs(inp, b, stage):
    f = np.float32
    v1 = np.zeros((NV1, D), f)
    v1[R_N1G:R_N1G + 2] = inp["norm1_g"]; v1[R_N2G:R_N2G + 2] = inp["norm2_g"]
    v1[R_PB] = inp["pool_b"][0]; v1[R_PS] = inp["pool_scale"][0]; v1[R_FG] = inp["final_g"]
    v1[R_C] = inp["c"][b]; v1[R_CC] = inp["c_ctx"]
    v1[R_ADAB:R_ADAB + 12] = inp["ada_b"].reshape(12, D)
    v1[R_QG, :512] = inp["q_norm_g"][0]; v1[R_KG, :256] = inp["kv_norm_g"][0]
    v1[R_BD:R_BD + 64] = inp["b_down"].reshape(64, D)
    v2 = np.ascontiguousarray(inp["b_gu"].reshape(128, D))
    m = {"x": np.ascontiguousarray(inp["x"][b]), "ctx": np.ascontiguousarray(inp["ctx"][b]), "v1": v1, "v2": v2,
         "ada_w": inp["ada_w"], "pool_w": np.ascontiguousarray(inp["pool_w"][0])}
    if stage >= 3:
        wuq = inp["w_uq"][0].reshape(512, 8, 192)
        perm = np.array([a * 32 + (1 - hf) * 16 + i for a in range(2) for hf in range(2) for i in range(16)])
        rope = wuq[:, :, 128:]
        wkv = inp["w_ukv"][0].reshape(256, 8, 256)
        dkv = inp["w_dkv"][0]
        m.update({"w_dq": np.ascontiguousarray(inp["w_dq"][0]),
                  "w_uq_n": np.ascontiguousarray(wuq[:, :, :128].reshape(512, 1024)),
                  "w_uq_r": np.ascontiguousarray(np.concatenate([rope, rope[:, :, perm]], axis=2).reshape(512, 1024)),
                  "w_dkv": np.ascontiguousarray(np.concatenate([dkv, dkv[:, 256:][:, perm]], axis=1)),
                  "w_uk": np.ascontiguousarray(wkv[:, :, :128].reshape(256, 1024)),
                  "w_uv": np.ascontiguousarray(wkv[:, :, 128:].reshape(256, 1024)),
                  "w_o": np.ascontiguousarray(inp["w_o"][0])})
    if stage >= 2:
        m.update({"router_w": inp["router_w"], "router_b": inp["router_b"], "w_gu": inp["w_gu"], "w_down": inp["w_down"],
                  "b_down": inp["b_down"]})
    return m


def kernel(**inputs):
    inp = {k: np.asarray(v) for k, v in inputs.items()}
    nc, st, P = build_program(STAGE)
    with st:
        in_maps = [_prep_inputs(inp, b, STAGE) for b in range(NCORES)]
        res = run_bass_kernel_spmd(nc, in_maps, core_ids=list(range(NCORES)))
    if STAGE < 99:
        return res
    return np.stack([r["out"] for r in res.results], axis=0)
```

```python
import numpy as np
from contextlib import ExitStack
import concourse.bass as bass
import concourse.mybir as mybir
from concourse.bass_utils import run_bass_kernel_spmd

F32 = mybir.dt.float32
BF16 = mybir.dt.bfloat16
I32 = mybir.dt.int32
U32 = mybir.dt.uint32
ALU = mybir.AluOpType
AF = mybir.ActivationFunctionType
AX = mybir.AxisListType

ENGINES = ("pe", "act", "dve", "pool", "sp")
EPOCH = 30000

D = 1024
L = 2048
C = 256
T = L + C
NE = 32
CAP = 1024
NSLOT = NE * CAP
EPS = 1e-6
ATTN_SCALE = 192 ** -0.5

STAGE = 99
NCORES = 8
DEBUG_TAGS = False
NO_ACT_DMA = False
CUT = 0
ZERO_YS = False


class SemGrp:
    __slots__ = ("name", "ndma", "sem", "bg")

    def __init__(self, name):
        self.name = name
        self.ndma = 0
        self.sem = None
        self.bg = False


class Buf:
    __slots__ = ("name", "last_w", "readers", "semgrp", "dram", "psum")

    def __init__(self, name, grp=None, dram=False, psum=False):
        self.name = name
        self.dram = dram
        self.psum = psum
        self.last_w = None
        self.readers = []
        self.semgrp = grp if grp is not None else SemGrp(name)


class TV:
    __slots__ = ("ap", "buf")

    def __init__(self, ap, buf):
        self.ap = ap
        self.buf = buf

    def __getitem__(self, k):
        return TV(self.ap[k], self.buf)

    def bitcast(self, dt):
        return TV(self.ap.bitcast(dt), self.buf)

    def rearrange(self, s, **kw):
        return TV(self.ap.rearrange(s, **kw), self.buf)

    def bc(self, shape):
        return TV(self.ap.to_broadcast(list(shape)), self.buf)

    def sub(self, name):
        return TV(self.ap, Buf(name))


class Op:
    __slots__ = ("eng", "fn", "deps", "is_dma", "grp", "signal", "epoch", "sigval", "tag", "cond", "dval")


def _ap(x):
    return x.ap if isinstance(x, TV) else x


class Prog:
    def __init__(self, nc, stack):
        self.nc = nc
        self.stack = stack
        self.ops = {e: [] for e in ENGINES}
        self.grps = {}
        self.n = 0
        self.cur_cond = None
        self.creg = {}
        self.nregion = 0

    def op(self, eng, fn, reads=(), writes=(), dma=None):
        o = Op()
        o.eng = eng
        o.fn = fn
        o.tag = None
        if DEBUG_TAGS:
            import sys as _s
            f = _s._getframe(1)
            while f.f_code.co_name in ("mm", "tr", "act", "tt", "ts", "stt", "copy", "memset", "red", "dma", "op"):
                f = f.f_back
            o.tag = f.f_lineno
        o.is_dma = dma is not None
        o.signal = False
        o.grp = None
        o.cond = self.cur_cond
        o.dval = 0
        deps = []
        seen = set()

        def add(d):
            if d is None or id(d) in seen:
                return
            seen.add(id(d))
            if d.is_dma:
                deps.append(("d", d.grp, 16 * d.grp.ndma))
            else:
                if d.eng == eng and eng == "pe" and not o.is_dma:
                    return
                deps.append(("c", d))

        rb = [r.buf if isinstance(r, TV) else r for r in reads if r is not None and not isinstance(r, (int, float))]
        wb = [w.buf if isinstance(w, TV) else w for w in writes]
        def latest(readers):
            last = {}
            for rd in readers:
                last[("d", id(rd.grp)) if rd.is_dma else ("c", rd.eng)] = rd
            return last.values()

        for r in rb:
            add(r.last_w)
            if r.psum:
                for rd in latest(r.readers):
                    if rd.eng != eng:
                        add(rd)
        for w in wb:
            add(w.last_w)
            for rd in latest(w.readers):
                add(rd)
        o.deps = deps
        if o.is_dma:
            g = (dma.buf if isinstance(dma, TV) else dma).semgrp
            o.dval = 16 * g.ndma
            g.ndma += 1
            o.grp = g
            self.grps[id(g)] = g
        for r in rb:
            r.readers.append(o)
        for w in wb:
            w.last_w = o
            w.readers = []
        self.ops[eng].append(o)
        self.n += 1
        return o

    def barrier(self, skip_bg=True):
        lasts = []
        for e in ENGINES:
            for o in reversed(self.ops[e]):
                if not o.is_dma and o.fn is not None:
                    lasts.append(o)
                    break
        dm = [("d", g, 16 * g.ndma) for g in self.grps.values() if g.ndma > 0 and not (skip_bg and g.bg)]
        for e in ENGINES:
            o = Op()
            o.eng = e
            o.fn = None
            o.is_dma = False
            o.signal = False
            o.grp = None
            o.cond = None
            o.dval = 0
            o.deps = [("c", x) for x in lasts if not (x.eng == e)] + list(dm)
            self.ops[e].append(o)

    def begin_region(self, flag, engines=("pe", "act", "dve", "sp")):
        assert self.cur_cond is None
        for e in engines:
            def ld(eng, e=e):
                if e not in self.creg:
                    self.creg[e] = eng.alloc_register(f"cflag_{e}")
                return eng.reg_load(self.creg[e], flag.ap)
            self.op(e, ld, reads=[flag])
        self.nregion += 1
        self.cur_cond = self.nregion

    def end_region(self):
        self.cur_cond = None

    def mm(self, out, lhsT, rhs, start=True, stop=True):
        return self.op("pe", lambda e: e.matmul(out.ap, lhsT=lhsT.ap, rhs=rhs.ap, start=start, stop=stop),
                       reads=[lhsT, rhs], writes=[out])

    def tr(self, out, in_, ident):
        return self.op("pe", lambda e: e.transpose(out=out.ap, in_=in_.ap, identity=ident.ap),
                       reads=[in_, ident], writes=[out])

    def act(self, out, in_, func, bias=0.0, scale=1.0, accum_out=None, eng="act"):
        kw = {}
        if accum_out is not None:
            kw["accum_out"] = accum_out.ap
        return self.op(eng, lambda e: e.activation(out=out.ap, in_=in_.ap, func=func, bias=_ap(bias), scale=_ap(scale), **kw),
                       reads=[in_, bias, scale], writes=[out] + ([accum_out] if accum_out is not None else []))

    def tt(self, eng, out, in0, in1, op):
        return self.op(eng, lambda e: e.tensor_tensor(out=out.ap, in0=in0.ap, in1=in1.ap, op=op),
                       reads=[in0, in1], writes=[out])

    def ts(self, eng, out, in0, s1, op0, s2=None, op1=None, accum_out=None):
        kw = {}
        if op1 is not None:
            kw["op1"] = op1
        if accum_out is not None:
            kw["accum_out"] = accum_out.ap
        return self.op(eng, lambda e: e.tensor_scalar(out=out.ap, in0=in0.ap, scalar1=_ap(s1), scalar2=_ap(s2), op0=op0, **kw),
                       reads=[in0, s1, s2], writes=[out] + ([accum_out] if accum_out is not None else []))

    def stt(self, eng, out, in0, scalar, in1, op0, op1):
        return self.op(eng, lambda e: e.scalar_tensor_tensor(out=out.ap, in0=in0.ap, scalar=_ap(scalar), in1=in1.ap, op0=op0, op1=op1),
                       reads=[in0, scalar, in1], writes=[out])

    def copy(self, eng, out, in_):
        if eng == "act":
            return self.op(eng, lambda e: e.activation(out=out.ap, in_=in_.ap, func=AF.Copy), reads=[in_], writes=[out])
        return self.op(eng, lambda e: e.tensor_copy(out=out.ap, in_=in_.ap), reads=[in_], writes=[out])

    def memset(self, eng, out, val):
        return self.op(eng, lambda e: e.memset(out.ap, val), writes=[out])

    def red(self, eng, out, in_, op, axis=AX.X):
        return self.op(eng, lambda e: e.tensor_reduce(out=out.ap, in_=in_.ap, axis=axis, op=op), reads=[in_], writes=[out])

    def dma(self, q, out, in_, owner=None, **kw):
        if q == "act" and NO_ACT_DMA:
            q = "sp"
        if owner is None:
            owner = in_ if out.buf.dram else out
        return self.op(q, lambda e: e.dma_start(out=out.ap, in_=in_.ap, **kw), reads=[in_], writes=[out], dma=owner)

    def emit(self):
        nc = self.nc
        for e in ENGINES:
            for o in self.ops[e]:
                for d in o.deps:
                    if d[0] == "c":
                        d[1].signal = True
        self.esems = {}
        for e in ENGINES:
            cnt = 0
            for o in self.ops[e]:
                if not o.is_dma and o.signal:
                    o.epoch = cnt // EPOCH
                    o.sigval = cnt % EPOCH + 1
                    cnt += 1
            nep = max(1, (cnt + EPOCH - 1) // EPOCH)
            self.esems[e] = [self.stack.enter_context(nc.semaphore(f"s_{e}{k}")) for k in range(nep)]
        for g in self.grps.values():
            g.sem = self.stack.enter_context(nc.semaphore(f"d_{g.name}"))
        self.nsem = sum(len(v) for v in self.esems.values()) + len(self.grps)
        block = self.stack.enter_context(nc.Block())
        reg = {"pe": block.tensor, "act": block.scalar, "dve": block.vector, "pool": block.gpsimd, "sp": block.sync}
        nwaits = {e: 0 for e in ENGINES}

        def make(e):
            def body(eng):
                def do_waits(o, waited):
                    for d in o.deps:
                        if d[0] == "d":
                            sem, val, key = d[1].sem, d[2], ("d", id(d[1]))
                        else:
                            x = d[1]
                            sem, val, key = self.esems[x.eng][x.epoch], x.sigval, ("c", x.eng, x.epoch)
                        if waited.get(key, 0) >= val:
                            continue
                        waited[key] = val
                        eng.wait_ge(sem, val)
                        nwaits[e] += 1

                def real(o, waited):
                    do_waits(o, waited)
                    if o.fn is None:
                        assert not o.signal
                        return
                    ins = o.fn(eng)
                    if o.is_dma:
                        ins.then_inc(o.grp.sem, 16)
                    elif o.signal:
                        ins.then_inc(self.esems[e][o.epoch], 1)

                pend = {}

                def flush():
                    for sem, n in pend.values():
                        eng.sem_inc(sem, n)
                    pend.clear()

                def ghost(o, waited, first):
                    if o.is_dma:
                        if o.dval > 0:
                            flush()
                            eng.wait_ge(o.grp.sem, o.dval)
                        k_ = id(o.grp.sem)
                        pend[k_] = (o.grp.sem, pend.get(k_, (None, 0))[1] + 16)
                        flush()
                    elif o.signal:
                        sm = self.esems[e][o.epoch]
                        k_ = id(sm)
                        pend[k_] = (sm, pend.get(k_, (None, 0))[1] + 1)

                waited = {}
                ops = self.ops[e]
                i = 0
                while i < len(ops):
                    o = ops[i]
                    if o.cond is None:
                        real(o, waited)
                        i += 1
                        continue
                    j = i
                    while j < len(ops) and ops[j].cond == o.cond:
                        j += 1
                    w1 = dict(waited)
                    with eng.If_ne(self.creg[e], 0):
                        for k in range(i, j):
                            real(ops[k], w1)
                    w2 = dict(waited)
                    with eng.Else():
                        eng.drain()
                        for k in range(i, j):
                            ghost(ops[k], w2, k == i)
                        flush()
                    i = j
            return body

        for e in ENGINES:
            if self.ops[e]:
                reg[e](make(e))
        self.nwaits = nwaits


def _shape(ap, shape):
    if len(shape) <= 1:
        return ap
    names = "abcdefg"[:len(shape)]
    kw = {names[i]: int(shape[i]) for i in range(len(shape) - 1)}
    return ap.rearrange(f"p ({' '.join(names)}) -> p {' '.join(names)}", **kw)


class Arena:
    def __init__(self, P, cols):
        self.P = P
        self.t = P.stack.enter_context(P.nc.sbuf_tensor("arena", [128, cols], F32))
        self.cols = cols
        self.top = 0
        self.n = 0

    def mark(self):
        return self.top

    def reset(self, m):
        self.top = m

    def f32(self, name, shape):
        n = int(np.prod(shape))
        off = self.top
        self.top += n
        assert self.top <= self.cols, f"arena overflow {name}: {self.top} > {self.cols}"
        ap = _shape(self.t[:, off:off + n], shape)
        self.n += 1
        return TV(ap, Buf(f"{name}{self.n}"))

    def b16(self, name, shape, dt=BF16):
        n = int(np.prod(shape))
        nf = (n + 1) // 2
        off = self.top
        self.top += nf
        assert self.top <= self.cols, f"arena overflow {name}: {self.top} > {self.cols}"
        ap = _shape(self.t[:, off:off + nf].bitcast(dt)[:, 0:n], shape)
        self.n += 1
        return TV(ap, Buf(f"{name}{self.n}"))

    def i32(self, name, shape):
        tv = self.f32(name, shape)
        return TV(tv.ap.bitcast(I32), tv.buf)


R_N1G, R_N2G, R_PB, R_PS, R_FG, R_C, R_CC, R_ADAB, R_QG, R_KG, R_BD = 0, 2, 4, 5, 6, 7, 8, 9, 21, 22, 32
NV1 = 96


def build_program(stage=99):
    nc = bass.Bass("TRN2", target_bir_lowering=False)
    dr = {}

    def din(name, shape, dt=F32):
        dr[name] = nc.dram_tensor(name, list(shape), dt, kind="ExternalInput").ap()
        return dr[name]

    x_d = din("x", [L, D]); ctx_d = din("ctx", [C, D])
    v1_d = din("v1", [NV1, D]); v2_d = din("v2", [128, D])
    adaw_d = din("ada_w", [2, D, 6 * D])
    poolw_d = din("pool_w", [4, 256, 256])
    if stage >= 2:
        rw_d = din("router_w", [2, D, NE]); rb_d = din("router_b", [2, NE])
        wgu_d = din("w_gu", [2, NE, D, 2 * D]); wdn_d = din("w_down", [2, NE, D, D]); bdn_d = din("b_down", [2, NE, D])
    if stage >= 3:
        wdq_d = din("w_dq", [D, 512]); wuqn_d = din("w_uq_n", [512, 1024]); wuqr_d = din("w_uq_r", [512, 1024])
        wdkv_d = din("w_dkv", [D, 384]); wuk_d = din("w_uk", [256, 1024]); wuv_d = din("w_uv", [256, 1024])
        wo_d = din("w_o", [D, D])
    out_d = nc.dram_tensor("out", [L, D], F32, kind="ExternalOutput").ap()
    dbg_d = nc.dram_tensor("dbg", [128, 8, T], F32, kind="ExternalOutput").ap() if stage < 99 else None
    if stage >= 2:
        xs_d = nc.dram_tensor("xs_scr", [NSLOT + 1, D], BF16, kind="Internal").ap()
        ys_d = nc.dram_tensor("ys_scr", [NSLOT + 1, D], F32, kind="Internal").ap()

    st = ExitStack()
    P = Prog(nc, st)
    A = Arena(P, 52736)
    dram = lambda ap, name: TV(ap, Buf(name, dram=True))

    PS = []
    for k in range(8):
        t = st.enter_context(nc.psum_tensor(f"ps{k}", [128, 512], F32))
        PS.append(TV(t[:, :], Buf(f"ps{k}", psum=True)))

    def ps_b16(k):
        return TV(PS[k].ap.bitcast(BF16), PS[k].buf)

    xT = A.f32("xT", [8, T])
    xTc = [TV(xT.ap[:, j, :], Buf(f"xT{j}")) for j in range(8)]
    VT1 = A.f32("VT1", [8, NV1])
    VT2 = A.f32("VT2", [8, 128])
    MOD = A.f32("MOD", [2, 48, 2])
    DER = A.f32("DER", [2, 6, 8, 2])
    identf = A.f32("identf", [128])
    onesf = A.f32("onesf", [128])
    ident = A.b16("ident", [128])
    onesb = A.b16("onesb", [128])
    tri = A.b16("tri", [128])
    zero1 = A.f32("zero1", [1])
    pmark = A.mark()

    P.memset("pool", identf, 0.0)
    P.op("pool", lambda e: e.affine_select(out=identf.ap, in_=identf.ap, pattern=[[-1, 128]], compare_op=ALU.not_equal,
                                           fill=1.0, base=0, channel_multiplier=1), reads=[identf], writes=[identf])
    P.copy("dve", ident, identf)
    P.memset("pool", onesf, 1.0)
    P.memset("pool", onesb, 1.0)
    P.memset("pool", zero1, 0.0)
    P.memset("pool", tri, 1.0)
    P.op("pool", lambda e: e.affine_select(out=tri.ap, in_=tri.ap, pattern=[[1, 128]], compare_op=ALU.is_gt,
                                           fill=0.0, base=0, channel_multiplier=-1), reads=[tri], writes=[tri])

    m0 = A.mark()
    v1s = A.f32("v1s", [D]); v2s = A.f32("v2s", [D])
    P.dma("sp", v1s[0:NV1], dram(v1_d, "v1d"))
    P.dma("sp", v2s, dram(v2_d, "v2d"))
    for (src, dst, nr) in ((v1s, VT1, NV1), (v2s, VT2, 128)):
        for j in range(8):
            pk = PS[j % 2]
            P.mm(pk[:, 0:nr], src[0:nr, j * 128:(j + 1) * 128], identf[0:nr, 0:nr])
            P.copy("dve", dst[:, j, :], pk[:, 0:nr])
    def cut():
        P.dma("sp", dram(dbg_d[:, 0, :], "dbgd"), xTc[0])
        P.barrier()
        P.emit()
        return nc, st, P
    if CUT == 1:
        return cut()
    scv = A.f32("scv", [8, 2])
    P.act(scv[:, :, 0], VT1[:, :, R_C], AF.Silu)
    P.act(scv[:, :, 1], VT1[:, :, R_CC], AF.Silu)
    aw = [A.f32(f"aw{k}", [8, 768]) for k in range(2)]
    q = 0
    for i in range(2):
        for cc in range(8):
            w = aw[q % 2]
            P.dma("sp" if q % 2 == 0 else "act", w, dram(adaw_d[i, :, cc * 768:(cc + 1) * 768].rearrange("(kc p) n -> p kc n", p=128), "adawd"))
            for mloc in range(6):
                mch = cc * 6 + mloc
                pk = PS[2 + (mch % 2)]
                for k in range(8):
                    P.mm(pk[:, 0:2], w[:, k, mloc * 128:(mloc + 1) * 128], scv[:, k, :], start=(k == 0), stop=(k == 7))
                P.ts("dve", MOD[:, i, mch, :], pk[:, 0:2], VT1[:, mch % 8, R_ADAB + i * 6 + mch // 8: R_ADAB + i * 6 + mch // 8 + 1], ALU.add)
            q += 1
    if CUT == 2:
        return cut()
    for i in range(2):
        for s in range(2):
            P.stt("dve", DER[:, i, 0, :, s], MOD[:, i, 8:16, s], 1.0, VT1[:, :, R_N1G + i], ALU.add, ALU.mult)
            P.stt("dve", DER[:, i, 1, :, s], MOD[:, i, 32:40, s], 1.0, VT1[:, :, R_N2G + i], ALU.add, ALU.mult)
        if i == 0:
            for s in range(2):
                P.tt("dve", DER[:, 0, 2, :, s], MOD[:, 0, 16:24, s], VT1[:, :, R_PS], ALU.mult)
                P.tt("dve", DER[:, 0, 3, :, s], DER[:, 0, 2, :, s], VT1[:, :, R_PB], ALU.mult)

    if CUT == 3:
        return cut()
    xin = [A.f32(f"xin{k}", [D]) for k in range(3)]
    for tt in range(18):
        xi = xin[tt % 3]
        src = x_d[tt * 128:(tt + 1) * 128, :] if tt < 16 else ctx_d[(tt - 16) * 128:(tt - 15) * 128, :]
        P.dma("sp" if tt % 2 == 0 else "act", xi, dram(src, "xd"))
        for half in range(2):
            pk = PS[4 + (2 * tt + half) % 4]
            for jj in range(4):
                j = half * 4 + jj
                P.mm(pk[:, jj * 128:(jj + 1) * 128], xi[:, j * 128:(j + 1) * 128], identf)
            dst = TV(xT.ap[:, half * 4:(half + 1) * 4, tt * 128:(tt + 1) * 128], None)
            srcv = pk.rearrange("p (a b) -> p a b", a=4)
            if half == 0:
                P.op("dve", lambda e, d=dst, s_=srcv: e.tensor_copy(out=d.ap, in_=s_.ap), reads=[pk], writes=xTc[0:4])
            else:
                P.op("act", lambda e, d=dst, s_=srcv: e.activation(out=d.ap, in_=s_.ap, func=AF.Copy), reads=[pk], writes=xTc[4:8])
    if stage >= 2:
        ZOFF = 51600
        zt = TV(A.t[:, ZOFF:ZOFF + 1024].bitcast(BF16), Buf("zt"))
        zt.buf.semgrp.bg = True
        xs_t = TV(xs_d, Buf("xs_scr", dram=True))
        ys_t = TV(ys_d, Buf("ys_scr", dram=True))
        P.memset("pool", zt, 0.0)
        for r in range(0, NSLOT, 256):
            P.dma("sp", TV(xs_d[r:r + 256, :].rearrange("(p a) n -> p (a n)", p=128), xs_t.buf), zt, owner=zt)
        ztf = TV(zt.ap.bitcast(F32), zt.buf)
        if ZERO_YS:
            for r in range(0, NSLOT, 128):
                P.dma("sp", TV(ys_d[r:r + 128, :], ys_t.buf), ztf, owner=zt)
        P.dma("sp", TV(xs_d[NSLOT:NSLOT + 1, :], xs_t.buf), zt[0:1, 0:D], owner=zt)
        P.dma("sp", TV(ys_d[NSLOT:NSLOT + 1, :], ys_t.buf), ztf[0:1, 0:D], owner=zt)

    P.barrier()
    A.reset(m0)

    def dump_and_finish():
        for j in range(8):
            P.dma("sp", dram(dbg_d[:, j, :], "dbgd"), xTc[j])
        P.barrier()
        P.emit()
        return nc, st, P

    if stage == 0:
        return dump_and_finish()

    BLK = [(0, 512), (512, 512), (1024, 512), (1536, 512), (2048, 256)]

    def rstd_all(rstd, ntok_blocks, src_chunks, nfeat, sq_pool, pss):
        nch = len(src_chunks)
        for bi, (t0, n) in enumerate(ntok_blocks):
            pk = pss[bi % len(pss)]
            for j in range(nch):
                sq = sq_pool[(bi * nch + j) % len(sq_pool)]
                P.act(sq[:, 0:n], src_chunks[j][:, t0:t0 + n], AF.Square)
                P.mm(pk[:, 0:n], onesf, sq[:, 0:n], start=(j == 0), stop=(j == nch - 1))
            P.ts("dve", rstd[:, t0:t0 + n], pk[:, 0:n], 1.0 / nfeat, ALU.mult, EPS, ALU.add)
            P.act(rstd[:, t0:t0 + n], rstd[:, t0:t0 + n], AF.Sqrt)
            P.op("dve", lambda e, o=rstd[:, t0:t0 + n]: e.reciprocal(out=o.ap, in_=o.ap), reads=[rstd], writes=[rstd])

    m1 = A.mark()
    rstd = A.f32("rstd", [T])
    sqp = [A.f32(f"sq{k}", [512]) for k in range(3)]
    rstd_all(rstd, BLK, xTc, D, sqp, [PS[0], PS[1]])
    WIN = (2, 4, 8, 16)
    PADW = 16
    segs = ((0, L, 0), (L, C, 1))
    hp = [A.f32(f"hp{s}", [ln + 2 * PADW]) for (_, ln, s) in segs]
    sa = [A.f32(f"sa{s}", [ln + 2 * PADW]) for (_, ln, s) in segs]
    sb = [A.f32(f"sb{s}", [ln + 2 * PADW]) for (_, ln, s) in segs]
    icnt = [[A.f32(f"ic{s}_{g}", [ln]) for g in range(4)] for (_, ln, s) in segs]
    dT = A.b16("dT", [2, T])
    uu = A.f32("uu", [T])
    pw = A.b16("pw", [4, 2, 256])
    P.dma("pool", pw, dram(poolw_d.rearrange("g (kc p) n -> p g kc n", p=128), "pwd"))

    def window_sum(si, w, src, dst_final):
        ln = segs[si][1]
        W = ln + 2 * PADW
        cur = src
        bufs = [sa[si], sb[si]]
        step = 1
        k = 0
        ww = 2
        while ww <= w:
            dst = bufs[k % 2]
            if ww == 2:
                P.tt("dve", dst[:, 1:W], cur[:, 0:W - 1], cur[:, 1:W], ALU.add)
            else:
                sh = ww // 4
                P.tt("dve", dst[:, sh:W - sh], cur[:, 0:W - 2 * sh], cur[:, 2 * sh:W], ALU.add)
            cur = dst
            k += 1
            ww *= 2
        return cur

    for si, (t0, ln, s) in enumerate(segs):
        for bufz in (hp[si], sa[si], sb[si]):
            P.memset("pool", bufz, 0.0)
    for si, (t0, ln, s) in enumerate(segs):
        P.memset("pool", hp[si][:, PADW:PADW + ln], 1.0)
        for g in range(4):
            cur = window_sum(si, WIN[g], hp[si], None)
            P.op("dve", lambda e, o=icnt[si][g], c=cur, ln=ln: e.reciprocal(out=o.ap, in_=c.ap[:, PADW:PADW + ln]),
                 reads=[cur], writes=[icnt[si][g]])
    for g in range(4):
        for kc in range(2):
            j = 2 * g + kc
            P.tt("dve", uu, xTc[j], rstd, ALU.mult)
            for si, (t0, ln, s) in enumerate(segs):
                P.act(hp[si][:, PADW:PADW + ln], uu[:, t0:t0 + ln], AF.Identity,
                      bias=MOD[:, 0, 0 + j, s:s + 1], scale=DER[:, 0, 0, j, s:s + 1])
                cur = window_sum(si, WIN[g], hp[si], None)
                P.tt("pool", cur[:, PADW:PADW + ln], cur[:, PADW:PADW + ln], icnt[si][g], ALU.mult)
                P.tt("pool", dT[:, kc, t0:t0 + ln], cur[:, PADW:PADW + ln], hp[si][:, PADW:PADW + ln], ALU.subtract)
        for mc in range(2):
            j = 2 * g + mc
            for bi, (t0, n) in enumerate(BLK):
                s = 0 if t0 < L else 1
                pk = PS[2 + (bi % 4)]
                for kc in range(2):
                    P.mm(pk[:, 0:n], pw[:, g, kc, mc * 128:(mc + 1) * 128], dT[:, kc, t0:t0 + n], start=(kc == 0), stop=(kc == 1))
                P.stt("dve", xTc[j][:, t0:t0 + n], pk[:, 0:n], DER[:, 0, 2, j, s:s + 1], xTc[j][:, t0:t0 + n], ALU.mult, ALU.add)
                P.ts("dve", xTc[j][:, t0:t0 + n], xTc[j][:, t0:t0 + n], DER[:, 0, 3, j, s:s + 1], ALU.add)
    P.barrier()
    A.reset(m1)

    if stage == 1:
        return dump_and_finish()

    REG = {}

    def mkregs(e):
        for nm, val in (("sc", NSLOT - 1), ("ga", NSLOT)):
            r = e.alloc_register(f"bc_{nm}")
            e.reg_mov(r, val)
            REG[nm] = r
        return e.nop()
    P.op("pool", mkregs)
    def moe(layer, ntt):
        ntok = ntt * 128
        blks = [b for b in BLK if b[0] < ntok]
        mm0 = A.mark()
        slot_i = A.i32("slot_i", [18, 4])
        gates = A.f32("gates", [18, 4])
        G = A.f32("G", [18, NE])
        masks = A.b16("masks", [18, NE])
        ebase = A.f32("ebase", [NE])
        ebi = A.i32("ebi", [NE])
        rw = A.f32("rw", [8, NE])
        rb = A.f32("rb", [NE])
        bdT = A.f32("bdT", [D])
        flags_f = A.f32("flags_f", [4, NE]); flags_i = A.i32("flags_i", [4, NE])
        P.op("pool", lambda e: e.iota(ebi.ap, pattern=[[CAP, NE]], base=0, channel_multiplier=0), writes=[ebi])
        P.copy("dve", ebase, ebi)
        P.dma("sp", rw, dram(rw_d[layer].rearrange("(kc p) n -> p kc n", p=128), "rwd"))
        P.dma("sp", rb[0:1, :], dram(rb_d[layer:layer + 1, :], "rbd"))
        P.dma("sp", bdT[0:NE, :], dram(bdn_d[layer], "bdd"))
        ma = A.mark()
        rstd2 = A.f32("rstd2", [T])
        sq2 = [A.f32(f"sq2{k}", [512]) for k in range(3)]
        rstd_all(rstd2, blks, xTc, D, sq2, [PS[0], PS[1]])
        u2 = [A.f32(f"u2{k}", [128]) for k in range(2)]
        h2f = [A.f32(f"h2f{k}", [8, 128]) for k in range(2)]
        h2b = [A.b16(f"h2b{k}", [8, 128]) for k in range(2)]
        h2tok = [A.b16(f"h2tok{k}", [D]) for k in range(2)]
        lg = A.f32("lg", [NE]); top8 = A.f32("top8", [8]); maskf = A.f32("maskf", [NE])
        pos = A.f32("pos", [NE]); t1 = A.f32("t1", [NE]); oh = A.f32("oh", [NE]); sel = A.f32("sel", [NE])
        slotsf = A.f32("slotsf", [4]); negm = A.f32("negm", [1]); ex = A.f32("ex", [4]); sumex = A.f32("sumex", [1])
        for tt in range(ntt):
            s = 0 if tt < 16 else 1
            tok = slice(tt * 128, (tt + 1) * 128)
            hf = h2f[tt % 2]; hb = h2b[tt % 2]
            for j in range(8):
                u = u2[j % 2]
                P.tt("dve", u, xTc[j][:, tok], rstd2[:, tok], ALU.mult)
                P.act(hf[:, j, :], u, AF.Identity, bias=MOD[:, layer, 24 + j, s:s + 1], scale=DER[:, layer, 1, j, s:s + 1])
                P.act(hb[:, j, :], u, AF.Identity, bias=MOD[:, layer, 24 + j, s:s + 1], scale=DER[:, layer, 1, j, s:s + 1])
            pl = PS[2 + tt % 2]
            for j in range(8):
                P.mm(pl[:, 0:NE], hf[:, j, :], rw[:, j, :], start=(j == 0), stop=False)
            P.mm(pl[:, 0:NE], onesf[0:1, :], rb[0:1, :], start=False, stop=True)
            P.copy("dve", lg, pl[:, 0:NE])
            P.op("dve", lambda e: e.max(out=top8.ap, in_=lg.ap), reads=[lg], writes=[top8])
            P.ts("dve", maskf, lg, top8[:, 3:4], ALU.is_ge)
            P.copy("dve", masks[:, tt, :], maskf)
            pp = PS[4 + tt % 2]
            P.mm(pp[:, 0:NE], tri, masks[:, tt, :], start=True, stop=(tt == 0))
            for i in range(tt):
                P.mm(pp[:, 0:NE], onesb, masks[:, i, :], start=False, stop=(i == tt - 1))
            P.copy("dve", pos, pp[:, 0:NE])
            P.ts("dve", t1, pos, float(CAP), ALU.is_ge, 1.0e6, ALU.mult)
            P.tt("dve", pos, pos, ebase, ALU.add)
            P.tt("dve", pos, pos, t1, ALU.add)
            P.ts("dve", pos, pos, float(NSLOT), ALU.min)
            P.ts("dve", negm, top8[:, 0:1], -1.0, ALU.mult)
            P.act(ex, top8[:, 0:4], AF.Exp, bias=negm, accum_out=sumex)
            P.op("dve", lambda e: e.reciprocal(out=sumex.ap, in_=sumex.ap), reads=[sumex], writes=[sumex])
            P.ts("dve", gates[:, tt, :], ex, sumex, ALU.mult)
            for k in range(4):
                P.ts("dve", oh, lg, top8[:, k:k + 1], ALU.is_equal)
                P.tt("dve", sel, oh, pos, ALU.mult)
                P.red("dve", slotsf[:, k:k + 1], sel, ALU.add)
                if k == 0:
                    P.ts("dve", G[:, tt, :], oh, gates[:, tt, 0:1], ALU.mult)
                else:
                    P.stt("dve", G[:, tt, :], oh, gates[:, tt, k:k + 1], G[:, tt, :], ALU.mult, ALU.add)
            P.copy("dve", slot_i[:, tt, :], slotsf)
            pt_ = PS[6 + tt % 2]
            ptb = TV(pt_.ap.bitcast(BF16), pt_.buf)
            for j in range(8):
                P.tr(ptb[:, j * 128:(j + 1) * 128], hb[:, j, :], ident)
            ht = h2tok[tt % 2]
            P.copy("act", ht, ptb)
            for k in range(4):
                P.op("pool", lambda e, ht=ht, tt=tt, k=k: e.indirect_dma_start(
                    out=xs_d, out_offset=bass.IndirectOffsetOnAxis(ap=slot_i.ap[:, tt, k:k + 1], axis=0),
                    in_=ht.ap, in_offset=None, bounds_check=REG["sc"], oob_is_err=False),
                    reads=[ht, slot_i], writes=[xs_t], dma=ht)
        pc = PS[0]
        for tt in range(ntt):
            P.mm(pc[:, 0:NE], onesb, masks[:, tt, :], start=(tt == 0), stop=(tt == ntt - 1))
        for q in range(4):
            P.ts("dve", flags_f[:, q, :], pc[:, 0:NE], float(q * 256), ALU.is_gt)
        P.copy("dve", flags_i, flags_f)
        P.barrier()
        A.reset(ma)
        BW = 256
        NQ = CAP // BW
        NRING = 9
        ring = [A.b16(f"ring{k}", [8, 512]) for k in range(NRING)]
        XeT = [A.b16(f"XeT{k}", [8, BW]) for k in range(2)]
        actT = [A.b16(f"actT{k}", [8, BW]) for k in range(2)]
        xrow = [A.b16(f"xrow{k}", [D]) for k in range(2)]
        yout = [A.f32(f"yout{k}", [D]) for k in range(2)]
        gsb = [A.f32(f"gsb{k}", [BW]) for k in range(2)]
        sgb = [A.f32(f"sgb{k}", [BW]) for k in range(2)]
        usb = [A.f32(f"usb{k}", [BW]) for k in range(2)]
        GORD = (0, 2, 1, 3)
        nq = 0
        cnt_it = [0, 0, 0]
        for e in range(NE):
            wg = {}
            for g in GORD:
                r = ring[nq % NRING]; nq += 1
                P.dma("pool", r, dram(wgu_d[layer, e, :, g * 512:(g + 1) * 512].rearrange("(kc p) n -> p kc n", p=128), "wgud"))
                wg[g] = r
            wd = {}
            for g in range(2):
                r = ring[nq % NRING]; nq += 1
                P.dma("pool", r, dram(wdn_d[layer, e, :, g * 512:(g + 1) * 512].rearrange("(kc p) n -> p kc n", p=128), "wdnd"))
                wd[g] = r
            row = (layer * NE + e) * 2

            def regionT(q, ee=e):
                P.begin_region(flags_i[0:1, q, ee:ee + 1], engines=("pe", "act", "sp"))
                xe = XeT[q % 2]
                for t in range(BW // 128):
                    stl = q * (BW // 128) + t
                    xr = xrow[cnt_it[0] % 2]
                    pk = PS[6 + cnt_it[0] % 2]
                    cnt_it[0] += 1
                    P.dma("sp", xr, TV(xs_d[ee * CAP + stl * 128: ee * CAP + (stl + 1) * 128, :], xs_t.buf), owner=xr)
                    pkb = TV(pk.ap.bitcast(BF16), pk.buf)
                    for j in range(8):
                        P.tr(pkb[:, j * 128:(j + 1) * 128], xr[:, j * 128:(j + 1) * 128], ident)
                    P.op("act", lambda e_, pkb=pkb, t=t, xe=xe: e_.activation(
                        out=xe.ap[:, :, t * 128:(t + 1) * 128], in_=pkb.ap.rearrange("p (a b) -> p a b", a=8), func=AF.Copy),
                        reads=[pkb], writes=[xe])
                P.end_region()

            def regionG(q):
                P.begin_region(flags_i[0:1, q, e:e + 1], engines=("pe", "act", "dve"))
                xe = XeT[q % 2]
                for j in range(8):
                    it = cnt_it[1]; cnt_it[1] += 1
                    pg = PS[(2 * it) % 4]; pu = PS[(2 * it + 1) % 4]
                    cg = (j % 4) * 128
                    for k in range(8):
                        P.mm(pg[:, 0:BW], wg[j // 4][:, k, cg:cg + 128], xe[:, k, :], start=(k == 0), stop=(k == 7))
                    for k in range(8):
                        P.mm(pu[:, 0:BW], wg[2 + j // 4][:, k, cg:cg + 128], xe[:, k, :], start=(k == 0), stop=(k == 7))
                    gs = gsb[it % 2]; sg = sgb[it % 2]; us = usb[it % 2]
                    P.ts("dve", gs, pg[:, 0:BW], VT2[:, j, row:row + 1], ALU.add, 7.0, ALU.min)
                    P.act(sg, gs, AF.Sigmoid, scale=1.702)
                    P.ts("dve", us, pu[:, 0:BW], VT2[:, j, row + 1:row + 2], ALU.add, 7.0, ALU.min)
                    P.ts("dve", us, us, -7.0, ALU.max, 1.0, ALU.add)
                    P.tt("dve", gs, gs, sg, ALU.mult)
                    P.tt("dve", actT[q % 2][:, j, :], gs, us, ALU.mult)
                P.end_region()

            def regionB(q):
                P.begin_region(flags_i[0:1, q, e:e + 1], engines=("pe", "act"))
                for t in range(BW // 128):
                    stl = q * (BW // 128) + t
                    yo = yout[cnt_it[2] % 2]
                    for n2 in range(2):
                        pd = PS[4 + (2 * cnt_it[2] + n2) % 2]
                        for k in range(8):
                            P.mm(pd, actT[q % 2][:, k, t * 128:(t + 1) * 128], wd[n2][:, k, :], start=(k == 0), stop=(k == 7))
                        P.copy("act", yo[:, n2 * 512:(n2 + 1) * 512], pd)
                    cnt_it[2] += 1
                    P.dma("act", TV(ys_d[e * CAP + stl * 128: e * CAP + (stl + 1) * 128, :], ys_t.buf), yo, owner=yo)
                P.end_region()

            if e == 0:
                regionT(0)
            for q in range(NQ):
                if q + 1 < NQ:
                    regionT(q + 1)
                regionG(q)
                if q >= 1:
                    regionB(q - 1)
            if e + 1 < NE:
                regionT(0, e + 1)
            regionB(NQ - 1)
        P.barrier()
        A.reset(ma)
        yk = [A.f32(f"yk{k}", [D]) for k in range(4)]
        acc = [A.f32(f"acc{k}", [D]) for k in range(2)]
        GT = [A.f32(f"GT{k}", [128]) for k in range(2)]
        for tt in range(ntt):
            s = 0 if tt < 16 else 1
            tok = slice(tt * 128, (tt + 1) * 128)
            ac = acc[tt % 2]
            for k in range(4):
                P.op("pool", lambda e, tt=tt, k=k: e.indirect_dma_start(
                    out=yk[k].ap, out_offset=None, in_=ys_d,
                    in_offset=bass.IndirectOffsetOnAxis(ap=slot_i.ap[:, tt, k:k + 1], axis=0),
                    bounds_check=REG["ga"], oob_is_err=False),
                    reads=[ys_t, slot_i], writes=[yk[k]], dma=yk[k])
                if k == 0:
                    P.ts("dve", ac, yk[0], gates[:, tt, 0:1], ALU.mult)
                else:
                    P.stt("dve", ac, yk[k], gates[:, tt, k:k + 1], ac, ALU.mult, ALU.add)
            pgt = PS[tt % 2]
            P.mm(pgt[0:NE, 0:128], G[:, tt, :], identf)
            gt = GT[tt % 2]
            P.copy("act", gt[0:NE, :], pgt[0:NE, 0:128])
            for j in range(8):
                pj = PS[2 + j % 4]
                P.mm(pj[:, 0:128], ac[:, j * 128:(j + 1) * 128], identf, start=True, stop=False)
                P.mm(pj[:, 0:128], bdT[0:NE, j * 128:(j + 1) * 128], gt[0:NE, :], start=False, stop=True)
                P.stt("dve", xTc[j][:, tok], pj[:, 0:128], MOD[:, layer, 40 + j, s:s + 1], xTc[j][:, tok], ALU.mult, ALU.add)
        P.barrier()
        A.reset(mm0)

    P.barrier(skip_bg=False)

    moe(0, 18)
    if stage == 2:
        return dump_and_finish()

    LBLK = BLK[0:4]
    m3 = A.mark()
    cqn = A.b16("cqn", [4, L])
    ckvn = A.b16("ckvn", [2, T])
    kpe = A.b16("kpe", [T])
    COS = A.f32("COS", [L]); SINS = A.f32("SINS", [L])
    m3t = A.mark()
    ti = A.i32("ti", [L]); pi_ = A.i32("pi", [1]); pf = A.f32("pf", [1]); invf = A.f32("invf", [1]); sgn = A.f32("sgn", [1])
    ang = A.f32("ang", [L]); nn = A.i32("nn", [L]); nf = A.f32("nf", [L]); fx = A.f32("fx", [L])
    R64 = slice(0, 64)
    P.op("pool", lambda e: e.iota(ti.ap[R64], pattern=[[1, L]], base=0, channel_multiplier=0), writes=[ti])
    P.op("pool", lambda e: e.iota(pi_.ap[R64], pattern=[[0, 1]], base=0, channel_multiplier=1), writes=[pi_])
    P.op("dve", lambda e: e.tensor_single_scalar(out=ti.ap[0:32], in_=ti.ap[0:32], scalar=6, op=ALU.arith_shift_right), reads=[ti], writes=[ti])
    P.op("dve", lambda e: e.tensor_single_scalar(out=ti.ap[32:64], in_=ti.ap[32:64], scalar=63, op=ALU.bitwise_and), reads=[ti], writes=[ti])
    P.copy("dve", ang[R64], ti[R64])
    P.op("dve", lambda e: e.tensor_single_scalar(out=nn.ap[R64, 0:1], in_=pi_.ap[R64], scalar=15, op=ALU.bitwise_and), reads=[pi_], writes=[nn])
    P.copy("dve", pf[R64], nn[R64, 0:1])
    P.act(invf[R64], pf[R64], AF.Exp, scale=-float(np.log(10000.0)) / 16.0)
    P.op("dve", lambda e: e.tensor_scalar(out=nn.ap[R64, 1:2], in0=pi_.ap[R64], scalar1=4, scalar2=1, op0=ALU.arith_shift_right, op1=ALU.bitwise_and), reads=[pi_], writes=[nn])
    P.copy("dve", sgn[R64], nn[R64, 1:2])
    P.ts("dve", sgn[R64], sgn[R64], 2.0, ALU.mult, -1.0, ALU.add)
    P.ts("dve", ang[R64], ang[R64], invf[R64], ALU.mult)
    TWO_PI = float(2 * np.pi)
    for (dst, shift) in ((SINS, 0.0), (COS, float(np.pi / 2))):
        P.ts("dve", nf[R64], ang[R64], shift, ALU.add, 1.0 / TWO_PI, ALU.mult)
        P.copy("dve", nn[R64], nf[R64])
        P.copy("dve", nf[R64], nn[R64])
        P.stt("dve", fx[R64], nf[R64], -TWO_PI, ang[R64], ALU.mult, ALU.add)
        if shift != 0.0:
            P.ts("dve", fx[R64], fx[R64], shift, ALU.add)
        P.ts("dve", nf[R64], fx[R64], -float(np.pi), ALU.is_lt, TWO_PI, ALU.mult)
        P.tt("dve", fx[R64], fx[R64], nf[R64], ALU.add)
        P.ts("dve", nf[R64], fx[R64], float(np.pi), ALU.is_gt, -TWO_PI, ALU.mult)
        P.tt("dve", fx[R64], fx[R64], nf[R64], ALU.add)
        P.act(dst[R64], fx[R64], AF.Sin)
    P.ts("dve", SINS[R64], SINS[R64], sgn[R64], ALU.mult)
    P.barrier()
    A.reset(m3t)
    hT = A.b16("hT", [8, T])
    wdq = A.b16("wdq", [8, 512]); wdkv = A.b16("wdkv", [8, 384])
    P.dma("pool", wdq, dram(wdq_d.rearrange("(kc p) n -> p kc n", p=128), "wdqd"))
    P.dma("pool", wdkv, dram(wdkv_d.rearrange("(kc p) n -> p kc n", p=128), "wdkvd"))
    m3a = A.mark()
    rstd1 = A.f32("rstd1", [T])
    sq3 = [A.f32(f"sq3{k}", [512]) for k in range(3)]
    rstd_all(rstd1, BLK, xTc, D, sq3, [PS[0], PS[1]])
    u3 = [A.f32(f"u3{k}", [512]) for k in range(2)]
    it3 = 0
    for j in range(8):
        for (t0, n) in BLK:
            s = 0 if t0 < L else 1
            u = u3[it3 % 2]; it3 += 1
            P.tt("dve", u[:, 0:n], xTc[j][:, t0:t0 + n], rstd1[:, t0:t0 + n], ALU.mult)
            P.act(hT[:, j, t0:t0 + n], u[:, 0:n], AF.Identity, bias=MOD[:, 1, 0 + j, s:s + 1], scale=DER[:, 1, 0, j, s:s + 1])
    P.barrier()
    A.reset(m3a)
    sq3 = [A.f32(f"sq3{k}", [512]) for k in range(3)]
    cqf = A.f32("cqf", [4, 512]); ckf = A.f32("ckf", [2, 512]); rq = A.f32("rq", [512]); rk = A.f32("rk", [512])
    ta = A.f32("ta", [512]); tb = A.f32("tb", [512])
    for bi, (t0, n) in enumerate(BLK):
        lat = t0 < L
        if lat:
            for m in range(4):
                pk = PS[2 + m % 2]
                for k in range(8):
                    P.mm(pk[:, 0:n], wdq[:, k, m * 128:(m + 1) * 128], hT[:, k, t0:t0 + n], start=(k == 0), stop=(k == 7))
                P.copy("act", cqf[:, m, 0:n], pk[:, 0:n])
            rstd_all(rq, [(0, n)], [cqf[:, m, :] for m in range(4)], 512, sq3, [PS[0]])
            for m in range(4):
                P.tt("dve", cqf[:, m, 0:n], cqf[:, m, 0:n], rq[:, 0:n], ALU.mult)
                P.act(cqn[:, m, t0:t0 + n], cqf[:, m, 0:n], AF.Copy, scale=VT1[:, m, R_QG:R_QG + 1])
        for m in range(2):
            pk = PS[4 + m % 2]
            for k in range(8):
                P.mm(pk[:, 0:n], wdkv[:, k, m * 128:(m + 1) * 128], hT[:, k, t0:t0 + n], start=(k == 0), stop=(k == 7))
            P.copy("act", ckf[:, m, 0:n], pk[:, 0:n])
        rstd_all(rk, [(0, n)], [ckf[:, m, :] for m in range(2)], 256, sq3, [PS[1]])
        for m in range(2):
            P.tt("dve", ckf[:, m, 0:n], ckf[:, m, 0:n], rk[:, 0:n], ALU.mult)
            P.act(ckvn[:, m, t0:t0 + n], ckf[:, m, 0:n], AF.Copy, scale=VT1[:, m, R_KG:R_KG + 1])
        pa = PS[6]; pb = PS[7]
        for k in range(8):
            P.mm(pa[0:64, 0:n], wdkv[:, k, 256:320], hT[:, k, t0:t0 + n], start=(k == 0), stop=(k == 7))
        if lat:
            for k in range(8):
                P.mm(pb[0:64, 0:n], wdkv[:, k, 320:384], hT[:, k, t0:t0 + n], start=(k == 0), stop=(k == 7))
            P.tt("dve", ta[R64, 0:n], pa[0:64, 0:n], COS[R64, t0:t0 + n], ALU.mult)
            P.tt("dve", tb[R64, 0:n], pb[0:64, 0:n], SINS[R64, t0:t0 + n], ALU.mult)
            P.tt("dve", kpe[R64, t0:t0 + n], ta[R64, 0:n], tb[R64, 0:n], ALU.add)
        else:
            P.copy("dve", kpe[R64, t0:t0 + n], pa[0:64, 0:n])
    P.barrier()
    A.reset(m3t)
    wo = [A.b16(f"wo{k}", [D]) for k in range(2)]
    wq = [A.b16(f"wq{k}", [4, 128]) for k in range(2)]
    wr = [A.b16(f"wr{k}", [4, 128]) for k in range(2)]
    wk = [A.b16(f"wk{k}", [2, 128]) for k in range(2)]
    wv = [A.b16(f"wv{k}", [2, 128]) for k in range(2)]
    qn = A.b16("qn", [L]); qr = A.b16("qr", [L]); kn = A.b16("kn", [T]); vh = A.b16("vh", [18, 128])
    attT = A.b16("attT", [L])
    S_sb = [A.f32(f"S_sb{k}", [T]) for k in range(2)]
    Pb = [A.b16(f"Pb{k}", [T]) for k in range(2)]
    PT = [A.b16(f"PT{k}", [18, 128]) for k in range(2)]
    mx = [A.f32(f"mx{k}", [1]) for k in range(2)]
    bmx = [A.f32(f"bmx{k}", [8]) for k in range(2)]
    rsum = [A.f32(f"rsum{k}", [1]) for k in range(2)]
    onb = [A.b16(f"onb{k}", [128]) for k in range(2)]
    qt_ = A.f32("qt_", [512]); ta = A.f32("ta", [512]); tb = A.f32("tb", [512])
    for h in range(8):
        b = h % 2
        P.dma("pool", wq[b], dram(wuqn_d[:, h * 128:(h + 1) * 128].rearrange("(kc p) n -> p kc n", p=128), "wqd"))
        P.dma("pool", wr[b], dram(wuqr_d[:, h * 128:(h + 1) * 128].rearrange("(kc p) n -> p kc n", p=128), "wrd"))
        P.dma("pool", wk[b], dram(wuk_d[:, h * 128:(h + 1) * 128].rearrange("(kc p) n -> p kc n", p=128), "wkd"))
        P.dma("pool", wv[b], dram(wuv_d[:, h * 128:(h + 1) * 128].rearrange("(kc p) n -> p kc n", p=128), "wvd"))
        P.dma("pool", wo[b], dram(wo_d[h * 128:(h + 1) * 128, :], "wod"))
        for bi, (t0, n) in enumerate(LBLK):
            pk = PS[bi % 2]
            for k in range(4):
                P.mm(pk[:, 0:n], wq[b][:, k, :], cqn[:, k, t0:t0 + n], start=(k == 0), stop=(k == 3))
            P.act(qn[:, t0:t0 + n], pk[:, 0:n], AF.Copy, scale=ATTN_SCALE)
            pa = PS[2]; pb = PS[3]
            for k in range(4):
                P.mm(pa[0:64, 0:n], wr[b][:, k, 0:64], cqn[:, k, t0:t0 + n], start=(k == 0), stop=(k == 3))
            for k in range(4):
                P.mm(pb[0:64, 0:n], wr[b][:, k, 64:128], cqn[:, k, t0:t0 + n], start=(k == 0), stop=(k == 3))
            P.tt("dve", ta[R64, 0:n], pa[0:64, 0:n], COS[R64, t0:t0 + n], ALU.mult)
            P.tt("dve", tb[R64, 0:n], pb[0:64, 0:n], SINS[R64, t0:t0 + n], ALU.mult)
            P.tt("dve", qt_[R64, 0:n], ta[R64, 0:n], tb[R64, 0:n], ALU.add)
            P.act(qr[R64, t0:t0 + n], qt_[R64, 0:n], AF.Copy, scale=ATTN_SCALE)
        for bi, (t0, n) in enumerate(BLK):
            pk = PS[4 + bi % 2]
            for k in range(2):
                P.mm(pk[:, 0:n], wk[b][:, k, :], ckvn[:, k, t0:t0 + n], start=(k == 0), stop=(k == 1))
            P.copy("act", kn[:, t0:t0 + n], pk[:, 0:n])
        for kt in range(18):
            pk = PS[6 + kt % 2]
            for k in range(2):
                P.mm(pk[:, 0:128], ckvn[:, k, kt * 128:(kt + 1) * 128], wv[b][:, k, :], start=(k == 0), stop=(k == 1))
            P.copy("dve", vh[:, kt, :], pk[:, 0:128])

        def scores(qt):
            qs = slice(qt * 128, (qt + 1) * 128)
            c = qt % 2
            for bi, (t0, n) in enumerate(BLK):
                pk = PS[bi % 4]
                P.mm(pk[:, 0:n], qn[:, qs], kn[:, t0:t0 + n], start=True, stop=False)
                P.mm(pk[:, 0:n], qr[R64, qs], kpe[R64, t0:t0 + n], start=False, stop=True)
                P.copy("act", S_sb[c][:, t0:t0 + n], pk[:, 0:n])
                P.red("dve", bmx[c][:, bi:bi + 1], S_sb[c][:, t0:t0 + n], ALU.max)

        def softmax(qt):
            c = qt % 2
            P.red("dve", mx[c], bmx[c][:, 0:5], ALU.max)
            P.ts("dve", mx[c], mx[c], -1.0, ALU.mult)
            P.act(Pb[c], S_sb[c], AF.Exp, bias=mx[c], accum_out=rsum[c])
            P.op("dve", lambda e, r=rsum[c]: e.reciprocal(out=r.ap, in_=r.ap), reads=[rsum[c]], writes=[rsum[c]])

        def attend_tile(qt):
            qs = slice(qt * 128, (qt + 1) * 128)
            c = qt % 2
            for g0 in range(0, 18, 8):
                ng = min(8, 18 - g0)
                pk = PS[4 + (g0 // 8) % 2]
                pkb = TV(pk.ap.bitcast(BF16), pk.buf)
                for kk in range(ng):
                    kt = g0 + kk
                    P.tr(pkb[:, kk * 128:(kk + 1) * 128], Pb[c][:, kt * 128:(kt + 1) * 128], ident)
                P.op("dve", lambda e, pkb=pkb, g0=g0, ng=ng, c=c: e.tensor_copy(
                    out=PT[c].ap[:, g0:g0 + ng, :], in_=pkb.ap[:, 0:ng * 128].rearrange("p (a b) -> p a b", a=ng)),
                    reads=[pkb], writes=[PT[c]])
            po = PS[6]
            for kt in range(18):
                P.mm(po[:, 0:128], PT[c][:, kt, :], vh[:, kt, :], start=(kt == 0), stop=(kt == 17))
            P.ts("dve", onb[c], po[:, 0:128], rsum[c], ALU.mult)
            pt2 = PS[7]
            pt2b = TV(pt2.ap.bitcast(BF16), pt2.buf)
            P.tr(pt2b[:, 0:128], onb[c], ident)
            P.copy("act", attT[:, qs], pt2b[:, 0:128])

        scores(0)
        for qt in range(17):
            if qt + 1 < 16:
                scores(qt + 1)
            if qt < 16:
                softmax(qt)
            if qt >= 1:
                attend_tile(qt - 1)
        for m in range(8):
            for bi, (t0, n) in enumerate(LBLK):
                pk = PS[(m * 4 + bi) % 4]
                P.mm(pk[:, 0:n], wo[b][:, m * 128:(m + 1) * 128], attT[:, t0:t0 + n])
                P.stt("dve", xTc[m][:, t0:t0 + n], pk[:, 0:n], MOD[:, 1, 16 + m, 0:1], xTc[m][:, t0:t0 + n], ALU.mult, ALU.add)
    P.barrier()
    A.reset(m3)
    if stage == 3:
        return dump_and_finish()

    moe(1, 16)
    if stage == 4:
        return dump_and_finish()

    fgb = A.f32("fgb", [D])
    P.dma("sp", fgb, dram(v1_d[R_FG:R_FG + 1, :].to_broadcast([128, D]), "fgd"))
    xo = [A.f32(f"xo{k}", [D]) for k in range(2)]
    sqo = A.f32("sqo", [D]); ssum = A.f32("ssum", [1])
    for tt in range(16):
        tok = slice(tt * 128, (tt + 1) * 128)
        o = xo[tt % 2]
        for half in range(2):
            pk = PS[(2 * tt + half) % 4]
            for jj in range(4):
                P.mm(pk[:, jj * 128:(jj + 1) * 128], xTc[half * 4 + jj][:, tok], identf)
            P.copy("act", o[:, half * 512:(half + 1) * 512], pk)
        P.act(sqo, o, AF.Square, accum_out=ssum)
        P.ts("dve", ssum, ssum, 1.0 / D, ALU.mult, EPS, ALU.add)
        P.act(ssum, ssum, AF.Sqrt)
        P.op("dve", lambda e: e.reciprocal(out=ssum.ap, in_=ssum.ap), reads=[ssum], writes=[ssum])
        P.stt("dve", o, o, ssum, fgb, ALU.mult, ALU.mult)
        P.dma("sp", dram(out_d[tt * 128:(tt + 1) * 128, :], "outd"), o)
    P.barrier()
    P.emit()
    return nc, st, P


def _prep_inputs(inp, b, stage):
    f = np.float32
    v1 = np.zeros((NV1, D), f)
    v1[R_N1G:R_N1G + 2] = inp["norm1_g"]; v1[R_N2G:R_N2G + 2] = inp["norm2_g"]
    v1[R_PB] = inp["pool_b"][0]; v1[R_PS] = inp["pool_scale"][0]; v1[R_FG] = inp["final_g"]
    v1[R_C] = inp["c"][b]; v1[R_CC] = inp["c_ctx"]
    v1[R_ADAB:R_ADAB + 12] = inp["ada_b"].reshape(12, D)
    v1[R_QG, :512] = inp["q_norm_g"][0]; v1[R_KG, :256] = inp["kv_norm_g"][0]
    v1[R_BD:R_BD + 64] = inp["b_down"].reshape(64, D)
    v2 = np.ascontiguousarray(inp["b_gu"].reshape(128, D))
    m = {"x": np.ascontiguousarray(inp["x"][b]), "ctx": np.ascontiguousarray(inp["ctx"][b]), "v1": v1, "v2": v2,
         "ada_w": inp["ada_w"], "pool_w": np.ascontiguousarray(inp["pool_w"][0])}
    if stage >= 3:
        wuq = inp["w_uq"][0].reshape(512, 8, 192)
        perm = np.array([a * 32 + (1 - hf) * 16 + i for a in range(2) for hf in range(2) for i in range(16)])
        rope = wuq[:, :, 128:]
        wkv = inp["w_ukv"][0].reshape(256, 8, 256)
        dkv = inp["w_dkv"][0]
        m.update({"w_dq": np.ascontiguousarray(inp["w_dq"][0]),
                  "w_uq_n": np.ascontiguousarray(wuq[:, :, :128].reshape(512, 1024)),
                  "w_uq_r": np.ascontiguousarray(np.concatenate([rope, rope[:, :, perm]], axis=2).reshape(512, 1024)),
                  "w_dkv": np.ascontiguousarray(np.concatenate([dkv, dkv[:, 256:][:, perm]], axis=1)),
                  "w_uk": np.ascontiguousarray(wkv[:, :, :128].reshape(256, 1024)),
                  "w_uv": np.ascontiguousarray(wkv[:, :, 128:].reshape(256, 1024)),
                  "w_o": np.ascontiguousarray(inp["w_o"][0])})
    if stage >= 2:
        m.update({"router_w": inp["router_w"], "router_b": inp["router_b"], "w_gu": inp["w_gu"], "w_down": inp["w_down"],
                  "b_down": inp["b_down"]})
    return m


def kernel(**inputs):
    inp = {k: np.asarray(v) for k, v in inputs.items()}
    nc, st, P = build_program(STAGE)
    with st:
        in_maps = [_prep_inputs(inp, b, STAGE) for b in range(NCORES)]
        res = run_bass_kernel_spmd(nc, in_maps, core_ids=list(range(NCORES)))
    if STAGE < 99:
        return res
    return np.stack([r["out"] for r in res.results], axis=0)
```

```python
import numpy as np
from contextlib import ExitStack
import concourse.bass as bass
import concourse.mybir as mybir
from concourse.bass_utils import run_bass_kernel_spmd

F32 = mybir.dt.float32
BF16 = mybir.dt.bfloat16
I32 = mybir.dt.int32
U32 = mybir.dt.uint32
ALU = mybir.AluOpType
AF = mybir.ActivationFunctionType
AX = mybir.AxisListType

ENGINES = ("pe", "act", "dve", "pool", "sp")
EPOCH = 30000

D = 1024
L = 2048
C = 256
T = L + C
NE = 32
CAP = 1024
NSLOT = NE * CAP
EPS = 1e-6
ATTN_SCALE = 192 ** -0.5

STAGE = 99
NCORES = 8
DEBUG_TAGS = False
NO_ACT_DMA = False
CUT = 0
ZERO_YS = False


class SemGrp:
    __slots__ = ("name", "ndma", "sem", "bg")

    def __init__(self, name):
        self.name = name
        self.ndma = 0
        self.sem = None
        self.bg = False


class Buf:
    __slots__ = ("name", "last_w", "readers", "semgrp", "dram", "psum")

    def __init__(self, name, grp=None, dram=False, psum=False):
        self.name = name
        self.dram = dram
        self.psum = psum
        self.last_w = None
        self.readers = []
        self.semgrp = grp if grp is not None else SemGrp(name)


class TV:
    __slots__ = ("ap", "buf")

    def __init__(self, ap, buf):
        self.ap = ap
        self.buf = buf

    def __getitem__(self, k):
        return TV(self.ap[k], self.buf)

    def bitcast(self, dt):
        return TV(self.ap.bitcast(dt), self.buf)

    def rearrange(self, s, **kw):
        return TV(self.ap.rearrange(s, **kw), self.buf)

    def bc(self, shape):
        return TV(self.ap.to_broadcast(list(shape)), self.buf)

    def sub(self, name):
        return TV(self.ap, Buf(name))


class Op:
    __slots__ = ("eng", "fn", "deps", "is_dma", "grp", "signal", "epoch", "sigval", "tag", "cond", "dval")


def _ap(x):
    return x.ap if isinstance(x, TV) else x


class Prog:
    def __init__(self, nc, stack):
        self.nc = nc
        self.stack = stack
        self.ops = {e: [] for e in ENGINES}
        self.grps = {}
        self.n = 0
        self.cur_cond = None
        self.creg = {}
        self.nregion = 0

    def op(self, eng, fn, reads=(), writes=(), dma=None):
        o = Op()
        o.eng = eng
        o.fn = fn
        o.tag = None
        if DEBUG_TAGS:
            import sys as _s
            f = _s._getframe(1)
            while f.f_code.co_name in ("mm", "tr", "act", "tt", "ts", "stt", "copy", "memset", "red", "dma", "op"):
                f = f.f_back
            o.tag = f.f_lineno
        o.is_dma = dma is not None
        o.signal = False
        o.grp = None
        o.cond = self.cur_cond
        o.dval = 0
        deps = []
        seen = set()

        def add(d):
            if d is None or id(d) in seen:
                return
            seen.add(id(d))
            if d.is_dma:
                deps.append(("d", d.grp, 16 * d.grp.ndma))
            else:
                if d.eng == eng and eng == "pe" and not o.is_dma:
                    return
                deps.append(("c", d))

        rb = [r.buf if isinstance(r, TV) else r for r in reads if r is not None and not isinstance(r, (int, float))]
        wb = [w.buf if isinstance(w, TV) else w for w in writes]
        def latest(readers):
            last = {}
            for rd in readers:
                last[("d", id(rd.grp)) if rd.is_dma else ("c", rd.eng)] = rd
            return last.values()

        for r in rb:
            add(r.last_w)
            if r.psum:
                for rd in latest(r.readers):
                    if rd.eng != eng:
                        add(rd)
        for w in wb:
            add(w.last_w)
            for rd in latest(w.readers):
                add(rd)
        o.deps = deps
        if o.is_dma:
            g = (dma.buf if isinstance(dma, TV) else dma).semgrp
            o.dval = 16 * g.ndma
            g.ndma += 1
            o.grp = g
            self.grps[id(g)] = g
        for r in rb:
            r.readers.append(o)
        for w in wb:
            w.last_w = o
            w.readers = []
        self.ops[eng].append(o)
        self.n += 1
        return o

    def barrier(self, skip_bg=True):
        lasts = []
        for e in ENGINES:
            for o in reversed(self.ops[e]):
                if not o.is_dma and o.fn is not None:
                    lasts.append(o)
                    break
        dm = [("d", g, 16 * g.ndma) for g in self.grps.values() if g.ndma > 0 and not (skip_bg and g.bg)]
        for e in ENGINES:
            o = Op()
            o.eng = e
            o.fn = None
            o.is_dma = False
            o.signal = False
            o.grp = None
            o.cond = None
            o.dval = 0
            o.deps = [("c", x) for x in lasts if not (x.eng == e)] + list(dm)
            self.ops[e].append(o)

    def begin_region(self, flag, engines=("pe", "act", "dve", "sp")):
        assert self.cur_cond is None
        for e in engines:
            def ld(eng, e=e):
                if e not in self.creg:
                    self.creg[e] = eng.alloc_register(f"cflag_{e}")
                return eng.reg_load(self.creg[e], flag.ap)
            self.op(e, ld, reads=[flag])
        self.nregion += 1
        self.cur_cond = self.nregion

    def end_region(self):
        self.cur_cond = None

    def mm(self, out, lhsT, rhs, start=True, stop=True):
        return self.op("pe", lambda e: e.matmul(out.ap, lhsT=lhsT.ap, rhs=rhs.ap, start=start, stop=stop),
                       reads=[lhsT, rhs], writes=[out])

    def tr(self, out, in_, ident):
        return self.op("pe", lambda e: e.transpose(out=out.ap, in_=in_.ap, identity=ident.ap),
                       reads=[in_, ident], writes=[out])

    def act(self, out, in_, func, bias=0.0, scale=1.0, accum_out=None, eng="act"):
        kw = {}
        if accum_out is not None:
            kw["accum_out"] = accum_out.ap
        return self.op(eng, lambda e: e.activation(out=out.ap, in_=in_.ap, func=func, bias=_ap(bias), scale=_ap(scale), **kw),
                       reads=[in_, bias, scale], writes=[out] + ([accum_out] if accum_out is not None else []))

    def tt(self, eng, out, in0, in1, op):
        return self.op(eng, lambda e: e.tensor_tensor(out=out.ap, in0=in0.ap, in1=in1.ap, op=op),
                       reads=[in0, in1], writes=[out])

    def ts(self, eng, out, in0, s1, op0, s2=None, op1=None, accum_out=None):
        kw = {}
        if op1 is not None:
            kw["op1"] = op1
        if accum_out is not None:
            kw["accum_out"] = accum_out.ap
        return self.op(eng, lambda e: e.tensor_scalar(out=out.ap, in0=in0.ap, scalar1=_ap(s1), scalar2=_ap(s2), op0=op0, **kw),
                       reads=[in0, s1, s2], writes=[out] + ([accum_out] if accum_out is not None else []))

    def stt(self, eng, out, in0, scalar, in1, op0, op1):
        return self.op(eng, lambda e: e.scalar_tensor_tensor(out=out.ap, in0=in0.ap, scalar=_ap(scalar), in1=in1.ap, op0=op0, op1=op1),
                       reads=[in0, scalar, in1], writes=[out])

    def copy(self, eng, out, in_):
        if eng == "act":
            return self.op(eng, lambda e: e.activation(out=out.ap, in_=in_.ap, func=AF.Copy), reads=[in_], writes=[out])
        return self.op(eng, lambda e: e.tensor_copy(out=out.ap, in_=in_.ap), reads=[in_], writes=[out])

    def memset(self, eng, out, val):
        return self.op(eng, lambda e: e.memset(out.ap, val), writes=[out])

    def red(self, eng, out, in_, op, axis=AX.X):
        return self.op(eng, lambda e: e.tensor_reduce(out=out.ap, in_=in_.ap, axis=axis, op=op), reads=[in_], writes=[out])

    def dma(self, q, out, in_, owner=None, **kw):
        if q == "act" and NO_ACT_DMA:
            q = "sp"
        if owner is None:
            owner = in_ if out.buf.dram else out
        return self.op(q, lambda e: e.dma_start(out=out.ap, in_=in_.ap, **kw), reads=[in_], writes=[out], dma=owner)

    def emit(self):
        nc = self.nc
        for e in ENGINES:
            for o in self.ops[e]:
                for d in o.deps:
                    if d[0] == "c":
                        d[1].signal = True
        self.esems = {}
        for e in ENGINES:
            cnt = 0
            for o in self.ops[e]:
                if not o.is_dma and o.signal:
                    o.epoch = cnt // EPOCH
                    o.sigval = cnt % EPOCH + 1
                    cnt += 1
            nep = max(1, (cnt + EPOCH - 1) // EPOCH)
            self.esems[e] = [self.stack.enter_context(nc.semaphore(f"s_{e}{k}")) for k in range(nep)]
        for g in self.grps.values():
            g.sem = self.stack.enter_context(nc.semaphore(f"d_{g.name}"))
        self.nsem = sum(len(v) for v in self.esems.values()) + len(self.grps)
        block = self.stack.enter_context(nc.Block())
        reg = {"pe": block.tensor, "act": block.scalar, "dve": block.vector, "pool": block.gpsimd, "sp": block.sync}
        nwaits = {e: 0 for e in ENGINES}

        def make(e):
            def body(eng):
                def do_waits(o, waited):
                    for d in o.deps:
                        if d[0] == "d":
                            sem, val, key = d[1].sem, d[2], ("d", id(d[1]))
                        else:
                            x = d[1]
                            sem, val, key = self.esems[x.eng][x.epoch], x.sigval, ("c", x.eng, x.epoch)
                        if waited.get(key, 0) >= val:
                            continue
                        waited[key] = val
                        eng.wait_ge(sem, val)
                        nwaits[e] += 1

                def real(o, waited):
                    do_waits(o, waited)
                    if o.fn is None:
                        assert not o.signal
                        return
                    ins = o.fn(eng)
                    if o.is_dma:
                        ins.then_inc(o.grp.sem, 16)
                    elif o.signal:
                        ins.then_inc(self.esems[e][o.epoch], 1)

                pend = {}

                def flush():
                    for sem, n in pend.values():
                        eng.sem_inc(sem, n)
                    pend.clear()

                def ghost(o, waited, first):
                    if o.is_dma:
                        if o.dval > 0:
                            flush()
                            eng.wait_ge(o.grp.sem, o.dval)
                        k_ = id(o.grp.sem)
                        pend[k_] = (o.grp.sem, pend.get(k_, (None, 0))[1] + 16)
                        flush()
                    elif o.signal:
                        sm = self.esems[e][o.epoch]
                        k_ = id(sm)
                        pend[k_] = (sm, pend.get(k_, (None, 0))[1] + 1)

                waited = {}
                ops = self.ops[e]
                i = 0
                while i < len(ops):
                    o = ops[i]
                    if o.cond is None:
                        real(o, waited)
                        i += 1
                        continue
                    j = i
                    while j < len(ops) and ops[j].cond == o.cond:
                        j += 1
                    w1 = dict(waited)
                    with eng.If_ne(self.creg[e], 0):
                        for k in range(i, j):
                            real(ops[k], w1)
                    w2 = dict(waited)
                    with eng.Else():
                        eng.drain()
                        for k in range(i, j):
                            ghost(ops[k], w2, k == i)
                        flush()
                    i = j
            return body

        for e in ENGINES:
            if self.ops[e]:
                reg[e](make(e))
        self.nwaits = nwaits


def _shape(ap, shape):
    if len(shape) <= 1:
        return ap
    names = "abcdefg"[:len(shape)]
    kw = {names[i]: int(shape[i]) for i in range(len(shape) - 1)}
    return ap.rearrange(f"p ({' '.join(names)}) -> p {' '.join(names)}", **kw)


class Arena:
    def __init__(self, P, cols):
        self.P = P
        self.t = P.stack.enter_context(P.nc.sbuf_tensor("arena", [128, cols], F32))
        self.cols = cols
        self.top = 0
        self.n = 0

    def mark(self):
        return self.top

    def reset(self, m):
        self.top = m

    def f32(self, name, shape):
        n = int(np.prod(shape))
        off = self.top
        self.top += n
        assert self.top <= self.cols, f"arena overflow {name}: {self.top} > {self.cols}"
        ap = _shape(self.t[:, off:off + n], shape)
        self.n += 1
        return TV(ap, Buf(f"{name}{self.n}"))

    def b16(self, name, shape, dt=BF16):
        n = int(np.prod(shape))
        nf = (n + 1) // 2
        off = self.top
        self.top += nf
        assert self.top <= self.cols, f"arena overflow {name}: {self.top} > {self.cols}"
        ap = _shape(self.t[:, off:off + nf].bitcast(dt)[:, 0:n], shape)
        self.n += 1
        return TV(ap, Buf(f"{name}{self.n}"))

    def i32(self, name, shape):
        tv = self.f32(name, shape)
        return TV(tv.ap.bitcast(I32), tv.buf)


R_N1G, R_N2G, R_PB, R_PS, R_FG, R_C, R_CC, R_ADAB, R_QG, R_KG, R_BD = 0, 2, 4, 5, 6, 7, 8, 9, 21, 22, 32
NV1 = 96


def build_program(stage=99):
    nc = bass.Bass("TRN2", target_bir_lowering=False)
    dr = {}

    def din(name, shape, dt=F32):
        dr[name] = nc.dram_tensor(name, list(shape), dt, kind="ExternalInput").ap()
        return dr[name]

    x_d = din("x", [L, D]); ctx_d = din("ctx", [C, D])
    v1_d = din("v1", [NV1, D]); v2_d = din("v2", [128, D])
    adaw_d = din("ada_w", [2, D, 6 * D])
    poolw_d = din("pool_w", [4, 256, 256])
    if stage >= 2:
        rw_d = din("router_w", [2, D, NE]); rb_d = din("router_b", [2, NE])
        wgu_d = din("w_gu", [2, NE, D, 2 * D]); wdn_d = din("w_down", [2, NE, D, D]); bdn_d = din("b_down", [2, NE, D])
    if stage >= 3:
        wdq_d = din("w_dq", [D, 512]); wuqn_d = din("w_uq_n", [512, 1024]); wuqr_d = din("w_uq_r", [512, 1024])
        wdkv_d = din("w_dkv", [D, 384]); wuk_d = din("w_uk", [256, 1024]); wuv_d = din("w_uv", [256, 1024])
        wo_d = din("w_o", [D, D])
    out_d = nc.dram_tensor("out", [L, D], F32, kind="ExternalOutput").ap()
    dbg_d = nc.dram_tensor("dbg", [128, 8, T], F32, kind="ExternalOutput").ap() if stage < 99 else None
    if stage >= 2:
        xs_d = nc.dram_tensor("xs_scr", [NSLOT + 1, D], BF16, kind="Internal").ap()
        ys_d = nc.dram_tensor("ys_scr", [NSLOT + 1, D], F32, kind="Internal").ap()

    st = ExitStack()
    P = Prog(nc, st)
    A = Arena(P, 52736)
    dram = lambda ap, name: TV(ap, Buf(name, dram=True))

    PS = []
    for k in range(8):
        t = st.enter_context(nc.psum_tensor(f"ps{k}", [128, 512], F32))
        PS.append(TV(t[:, :], Buf(f"ps{k}", psum=True)))

    def ps_b16(k):
        return TV(PS[k].ap.bitcast(BF16), PS[k].buf)

    xT = A.f32("xT", [8, T])
    xTc = [TV(xT.ap[:, j, :], Buf(f"xT{j}")) for j in range(8)]
    VT1 = A.f32("VT1", [8, NV1])
    VT2 = A.f32("VT2", [8, 128])
    MOD = A.f32("MOD", [2, 48, 2])
    DER = A.f32("DER", [2, 6, 8, 2])
    identf = A.f32("identf", [128])
    onesf = A.f32("onesf", [128])
    ident = A.b16("ident", [128])
    onesb = A.b16("onesb", [128])
    tri = A.b16("tri", [128])
    zero1 = A.f32("zero1", [1])
    pmark = A.mark()

    P.memset("pool", identf, 0.0)
    P.op("pool", lambda e: e.affine_select(out=identf.ap, in_=identf.ap, pattern=[[-1, 128]], compare_op=ALU.not_equal,
                                           fill=1.0, base=0, channel_multiplier=1), reads=[identf], writes=[identf])
    P.copy("dve", ident, identf)
    P.memset("pool", onesf, 1.0)
    P.memset("pool", onesb, 1.0)
    P.memset("pool", zero1, 0.0)
    P.memset("pool", tri, 1.0)
    P.op("pool", lambda e: e.affine_select(out=tri.ap, in_=tri.ap, pattern=[[1, 128]], compare_op=ALU.is_gt,
                                           fill=0.0, base=0, channel_multiplier=-1), reads=[tri], writes=[tri])

    m0 = A.mark()
    v1s = A.f32("v1s", [D]); v2s = A.f32("v2s", [D])
    P.dma("sp", v1s[0:NV1], dram(v1_d, "v1d"))
    P.dma("sp", v2s, dram(v2_d, "v2d"))
    for (src, dst, nr) in ((v1s, VT1, NV1), (v2s, VT2, 128)):
        for j in range(8):
            pk = PS[j % 2]
            P.mm(pk[:, 0:nr], src[0:nr, j * 128:(j + 1) * 128], identf[0:nr, 0:nr])
            P.copy("dve", dst[:, j, :], pk[:, 0:nr])
    def cut():
        P.dma("sp", dram(dbg_d[:, 0, :], "dbgd"), xTc[0])
        P.barrier()
        P.emit()
        return nc, st, P
    if CUT == 1:
        return cut()
    scv = A.f32("scv", [8, 2])
    P.act(scv[:, :, 0], VT1[:, :, R_C], AF.Silu)
    P.act(scv[:, :, 1], VT1[:, :, R_CC], AF.Silu)
    aw = [A.f32(f"aw{k}", [8, 768]) for k in range(2)]
    q = 0
    for i in range(2):
        for cc in range(8):
            w = aw[q % 2]
            P.dma("sp" if q % 2 == 0 else "act", w, dram(adaw_d[i, :, cc * 768:(cc + 1) * 768].rearrange("(kc p) n -> p kc n", p=128), "adawd"))
            for mloc in range(6):
                mch = cc * 6 + mloc
                pk = PS[2 + (mch % 2)]
                for k in range(8):
                    P.mm(pk[:, 0:2], w[:, k, mloc * 128:(mloc + 1) * 128], scv[:, k, :], start=(k == 0), stop=(k == 7))
                P.ts("dve", MOD[:, i, mch, :], pk[:, 0:2], VT1[:, mch % 8, R_ADAB + i * 6 + mch // 8: R_ADAB + i * 6 + mch // 8 + 1], ALU.add)
            q += 1
    if CUT == 2:
        return cut()
    for i in range(2):
        for s in range(2):
            P.stt("dve", DER[:, i, 0, :, s], MOD[:, i, 8:16, s], 1.0, VT1[:, :, R_N1G + i], ALU.add, ALU.mult)
            P.stt("dve", DER[:, i, 1, :, s], MOD[:, i, 32:40, s], 1.0, VT1[:, :, R_N2G + i], ALU.add, ALU.mult)
        if i == 0:
            for s in range(2):
                P.tt("dve", DER[:, 0, 2, :, s], MOD[:, 0, 16:24, s], VT1[:, :, R_PS], ALU.mult)
                P.tt("dve", DER[:, 0, 3, :, s], DER[:, 0, 2, :, s], VT1[:, :, R_PB], ALU.mult)

    if CUT == 3:
        return cut()
    xin = [A.f32(f"xin{k}", [D]) for k in range(3)]
    for tt in range(18):
        xi = xin[tt % 3]
        src = x_d[tt * 128:(tt + 1) * 128, :] if tt < 16 else ctx_d[(tt - 16) * 128:(tt - 15) * 128, :]
        P.dma("sp" if tt % 2 == 0 else "act", xi, dram(src, "xd"))
        for half in range(2):
            pk = PS[4 + (2 * tt + half) % 4]
            for jj in range(4):
                j = half * 4 + jj
                P.mm(pk[:, jj * 128:(jj + 1) * 128], xi[:, j * 128:(j + 1) * 128], identf)
            dst = TV(xT.ap[:, half * 4:(half + 1) * 4, tt * 128:(tt + 1) * 128], None)
            srcv = pk.rearrange("p (a b) -> p a b", a=4)
            if half == 0:
                P.op("dve", lambda e, d=dst, s_=srcv: e.tensor_copy(out=d.ap, in_=s_.ap), reads=[pk], writes=xTc[0:4])
            else:
                P.op("act", lambda e, d=dst, s_=srcv: e.activation(out=d.ap, in_=s_.ap, func=AF.Copy), reads=[pk], writes=xTc[4:8])
    if stage >= 2:
        ZOFF = 51600
        zt = TV(A.t[:, ZOFF:ZOFF + 1024].bitcast(BF16), Buf("zt"))
        zt.buf.semgrp.bg = True
        xs_t = TV(xs_d, Buf("xs_scr", dram=True))
        ys_t = TV(ys_d, Buf("ys_scr", dram=True))
        P.memset("pool", zt, 0.0)
        for r in range(0, NSLOT, 256):
            P.dma("sp", TV(xs_d[r:r + 256, :].rearrange("(p a) n -> p (a n)", p=128), xs_t.buf), zt, owner=zt)
        ztf = TV(zt.ap.bitcast(F32), zt.buf)
        if ZERO_YS:
            for r in range(0, NSLOT, 128):
                P.dma("sp", TV(ys_d[r:r + 128, :], ys_t.buf), ztf, owner=zt)
        P.dma("sp", TV(xs_d[NSLOT:NSLOT + 1, :], xs_t.buf), zt[0:1, 0:D], owner=zt)
        P.dma("sp", TV(ys_d[NSLOT:NSLOT + 1, :], ys_t.buf), ztf[0:1, 0:D], owner=zt)

    P.barrier()
    A.reset(m0)

    def dump_and_finish():
        for j in range(8):
            P.dma("sp", dram(dbg_d[:, j, :], "dbgd"), xTc[j])
        P.barrier()
        P.emit()
        return nc, st, P

    if stage == 0:
        return dump_and_finish()

    BLK = [(0, 512), (512, 512), (1024, 512), (1536, 512), (2048, 256)]

    def rstd_all(rstd, ntok_blocks, src_chunks, nfeat, sq_pool, pss):
        nch = len(src_chunks)
        for bi, (t0, n) in enumerate(ntok_blocks):
            pk = pss[bi % len(pss)]
            for j in range(nch):
                sq = sq_pool[(bi * nch + j) % len(sq_pool)]
                P.act(sq[:, 0:n], src_chunks[j][:, t0:t0 + n], AF.Square)
                P.mm(pk[:, 0:n], onesf, sq[:, 0:n], start=(j == 0), stop=(j == nch - 1))
            P.ts("dve", rstd[:, t0:t0 + n], pk[:, 0:n], 1.0 / nfeat, ALU.mult, EPS, ALU.add)
            P.act(rstd[:, t0:t0 + n], rstd[:, t0:t0 + n], AF.Sqrt)
            P.op("dve", lambda e, o=rstd[:, t0:t0 + n]: e.reciprocal(out=o.ap, in_=o.ap), reads=[rstd], writes=[rstd])

    m1 = A.mark()
    rstd = A.f32("rstd", [T])
    sqp = [A.f32(f"sq{k}", [512]) for k in range(3)]
    rstd_all(rstd, BLK, xTc, D, sqp, [PS[0], PS[1]])
    WIN = (2, 4, 8, 16)
    PADW = 16
    segs = ((0, L, 0), (L, C, 1))
    hp = [A.f32(f"hp{s}", [ln + 2 * PADW]) for (_, ln, s) in segs]
    sa = [A.f32(f"sa{s}", [ln + 2 * PADW]) for (_, ln, s) in segs]
    sb = [A.f32(f"sb{s}", [ln + 2 * PADW]) for (_, ln, s) in segs]
    icnt = [[A.f32(f"ic{s}_{g}", [ln]) for g in range(4)] for (_, ln, s) in segs]
    dT = A.b16("dT", [2, T])
    uu = A.f32("uu", [T])
    pw = A.b16("pw", [4, 2, 256])
    P.dma("pool", pw, dram(poolw_d.rearrange("g (kc p) n -> p g kc n", p=128), "pwd"))

    def window_sum(si, w, src, dst_final):
        ln = segs[si][1]
        W = ln + 2 * PADW
        cur = src
        bufs = [sa[si], sb[si]]
        step = 1
        k = 0
        ww = 2
        while ww <= w:
            dst = bufs[k % 2]
            if ww == 2:
                P.tt("dve", dst[:, 1:W], cur[:, 0:W - 1], cur[:, 1:W], ALU.add)
            else:
                sh = ww // 4
                P.tt("dve", dst[:, sh:W - sh], cur[:, 0:W - 2 * sh], cur[:, 2 * sh:W], ALU.add)
            cur = dst
            k += 1
            ww *= 2
        return cur

    for si, (t0, ln, s) in enumerate(segs):
        for bufz in (hp[si], sa[si], sb[si]):
            P.memset("pool", bufz, 0.0)
    for si, (t0, ln, s) in enumerate(segs):
        P.memset("pool", hp[si][:, PADW:PADW + ln], 1.0)
        for g in range(4):
            cur = window_sum(si, WIN[g], hp[si], None)
            P.op("dve", lambda e, o=icnt[si][g], c=cur, ln=ln: e.reciprocal(out=o.ap, in_=c.ap[:, PADW:PADW + ln]),
                 reads=[cur], writes=[icnt[si][g]])
    for g in range(4):
        for kc in range(2):
            j = 2 * g + kc
            P.tt("dve", uu, xTc[j], rstd, ALU.mult)
            for si, (t0, ln, s) in enumerate(segs):
                P.act(hp[si][:, PADW:PADW + ln], uu[:, t0:t0 + ln], AF.Identity,
                      bias=MOD[:, 0, 0 + j, s:s + 1], scale=DER[:, 0, 0, j, s:s + 1])
                cur = window_sum(si, WIN[g], hp[si], None)
                P.tt("pool", cur[:, PADW:PADW + ln], cur[:, PADW:PADW + ln], icnt[si][g], ALU.mult)
                P.tt("pool", dT[:, kc, t0:t0 + ln], cur[:, PADW:PADW + ln], hp[si][:, PADW:PADW + ln], ALU.subtract)
        for mc in range(2):
            j = 2 * g + mc
            for bi, (t0, n) in enumerate(BLK):
                s = 0 if t0 < L else 1
                pk = PS[2 + (bi % 4)]
                for kc in range(2):
                    P.mm(pk[:, 0:n], pw[:, g, kc, mc * 128:(mc + 1) * 128], dT[:, kc, t0:t0 + n], start=(kc == 0), stop=(kc == 1))
                P.stt("dve", xTc[j][:, t0:t0 + n], pk[:, 0:n], DER[:, 0, 2, j, s:s + 1], xTc[j][:, t0:t0 + n], ALU.mult, ALU.add)
                P.ts("dve", xTc[j][:, t0:t0 + n], xTc[j][:, t0:t0 + n], DER[:, 0, 3, j, s:s + 1], ALU.add)
    P.barrier()
    A.reset(m1)

    if stage == 1:
        return dump_and_finish()

    REG = {}

    def mkregs(e):
        for nm, val in (("sc", NSLOT - 1), ("ga", NSLOT)):
            r = e.alloc_register(f"bc_{nm}")
            e.reg_mov(r, val)
            REG[nm] = r
        return e.nop()
    P.op("pool", mkregs)
    def moe(layer, ntt):
        ntok = ntt * 128
        blks = [b for b in BLK if b[0] < ntok]
        mm0 = A.mark()
        slot_i = A.i32("slot_i", [18, 4])
        gates = A.f32("gates", [18, 4])
        G = A.f32("G", [18, NE])
        masks = A.b16("masks", [18, NE])
        ebase = A.f32("ebase", [NE])
        ebi = A.i32("ebi", [NE])
        rw = A.f32("rw", [8, NE])
        rb = A.f32("rb", [NE])
        bdT = A.f32("bdT", [D])
        flags_f = A.f32("flags_f", [4, NE]); flags_i = A.i32("flags_i", [4, NE])
        P.op("pool", lambda e: e.iota(ebi.ap, pattern=[[CAP, NE]], base=0, channel_multiplier=0), writes=[ebi])
        P.copy("dve", ebase, ebi)
        P.dma("sp", rw, dram(rw_d[layer].rearrange("(kc p) n -> p kc n", p=128), "rwd"))
        P.dma("sp", rb[0:1, :], dram(rb_d[layer:layer + 1, :], "rbd"))
        P.dma("sp", bdT[0:NE, :], dram(bdn_d[layer], "bdd"))
        ma = A.mark()
        rstd2 = A.f32("rstd2", [T])
        sq2 = [A.f32(f"sq2{k}", [512]) for k in range(3)]
        rstd_all(rstd2, blks, xTc, D, sq2, [PS[0], PS[1]])
        u2 = [A.f32(f"u2{k}", [128]) for k in range(2)]
        h2f = [A.f32(f"h2f{k}", [8, 128]) for k in range(2)]
        h2b = [A.b16(f"h2b{k}", [8, 128]) for k in range(2)]
        h2tok = [A.b16(f"h2tok{k}", [D]) for k in range(2)]
        lg = A.f32("lg", [NE]); top8 = A.f32("top8", [8]); maskf = A.f32("maskf", [NE])
        pos = A.f32("pos", [NE]); t1 = A.f32("t1", [NE]); oh = A.f32("oh", [NE]); sel = A.f32("sel", [NE])
        slotsf = A.f32("slotsf", [4]); negm = A.f32("negm", [1]); ex = A.f32("ex", [4]); sumex = A.f32("sumex", [1])
        for tt in range(ntt):
            s = 0 if tt < 16 else 1
            tok = slice(tt * 128, (tt + 1) * 128)
            hf = h2f[tt % 2]; hb = h2b[tt % 2]
            for j in range(8):
                u = u2[j % 2]
                P.tt("dve", u, xTc[j][:, tok], rstd2[:, tok], ALU.mult)
                P.act(hf[:, j, :], u, AF.Identity, bias=MOD[:, layer, 24 + j, s:s + 1], scale=DER[:, layer, 1, j, s:s + 1])
                P.act(hb[:, j, :], u, AF.Identity, bias=MOD[:, layer, 24 + j, s:s + 1], scale=DER[:, layer, 1, j, s:s + 1])
            pl = PS[2 + tt % 2]
            for j in range(8):
                P.mm(pl[:, 0:NE], hf[:, j, :], rw[:, j, :], start=(j == 0), stop=False)
            P.mm(pl[:, 0:NE], onesf[0:1, :], rb[0:1, :], start=False, stop=True)
            P.copy("dve", lg, pl[:, 0:NE])
            P.op("dve", lambda e: e.max(out=top8.ap, in_=lg.ap), reads=[lg], writes=[top8])
            P.ts("dve", maskf, lg, top8[:, 3:4], ALU.is_ge)
            P.copy("dve", masks[:, tt, :], maskf)
            pp = PS[4 + tt % 2]
            P.mm(pp[:, 0:NE], tri, masks[:, tt, :], start=True, stop=(tt == 0))
            for i in range(tt):
                P.mm(pp[:, 0:NE], onesb, masks[:, i, :], start=False, stop=(i == tt - 1))
            P.copy("dve", pos, pp[:, 0:NE])
            P.ts("dve", t1, pos, float(CAP), ALU.is_ge, 1.0e6, ALU.mult)
            P.tt("dve", pos, pos, ebase, ALU.add)
            P.tt("dve", pos, pos, t1, ALU.add)
            P.ts("dve", pos, pos, float(NSLOT), ALU.min)
            P.ts("dve", negm, top8[:, 0:1], -1.0, ALU.mult)
            P.act(ex, top8[:, 0:4], AF.Exp, bias=negm, accum_out=sumex)
            P.op("dve", lambda e: e.reciprocal(out=sumex.ap, in_=sumex.ap), reads=[sumex], writes=[sumex])
            P.ts("dve", gates[:, tt, :], ex, sumex, ALU.mult)
            for k in range(4):
                P.ts("dve", oh, lg, top8[:, k:k + 1], ALU.is_equal)
                P.tt("dve", sel, oh, pos, ALU.mult)
                P.red("dve", slotsf[:, k:k + 1], sel, ALU.add)
                if k == 0:
                    P.ts("dve", G[:, tt, :], oh, gates[:, tt, 0:1], ALU.mult)
                else:
                    P.stt("dve", G[:, tt, :], oh, gates[:, tt, k:k + 1], G[:, tt, :], ALU.mult, ALU.add)
            P.copy("dve", slot_i[:, tt, :], slotsf)
            pt_ = PS[6 + tt % 2]
            ptb = TV(pt_.ap.bitcast(BF16), pt_.buf)
            for j in range(8):
                P.tr(ptb[:, j * 128:(j + 1) * 128], hb[:, j, :], ident)
            ht = h2tok[tt % 2]
            P.copy("act", ht, ptb)
            for k in range(4):
                P.op("pool", lambda e, ht=ht, tt=tt, k=k: e.indirect_dma_start(
                    out=xs_d, out_offset=bass.IndirectOffsetOnAxis(ap=slot_i.ap[:, tt, k:k + 1], axis=0),
                    in_=ht.ap, in_offset=None, bounds_check=REG["sc"], oob_is_err=False),
                    reads=[ht, slot_i], writes=[xs_t], dma=ht)
        pc = PS[0]
        for tt in range(ntt):
            P.mm(pc[:, 0:NE], onesb, masks[:, tt, :], start=(tt == 0), stop=(tt == ntt - 1))
        for q in range(4):
            P.ts("dve", flags_f[:, q, :], pc[:, 0:NE], float(q * 256), ALU.is_gt)
        P.copy("dve", flags_i, flags_f)
        P.barrier()
        A.reset(ma)
        BW = 256
        NQ = CAP // BW
        NRING = 9
        ring = [A.b16(f"ring{k}", [8, 512]) for k in range(NRING)]
        XeT = [A.b16(f"XeT{k}", [8, BW]) for k in range(2)]
        actT = [A.b16(f"actT{k}", [8, BW]) for k in range(2)]
        xrow = [A.b16(f"xrow{k}", [D]) for k in range(2)]
        yout = [A.f32(f"yout{k}", [D]) for k in range(2)]
        gsb = [A.f32(f"gsb{k}", [BW]) for k in range(2)]
        sgb = [A.f32(f"sgb{k}", [BW]) for k in range(2)]
        usb = [A.f32(f"usb{k}", [BW]) for k in range(2)]
        GORD = (0, 2, 1, 3)
        nq = 0
        cnt_it = [0, 0, 0]
        for e in range(NE):
            wg = {}
            for g in GORD:
                r = ring[nq % NRING]; nq += 1
                P.dma("pool", r, dram(wgu_d[layer, e, :, g * 512:(g + 1) * 512].rearrange("(kc p) n -> p kc n", p=128), "wgud"))
                wg[g] = r
            wd = {}
            for g in range(2):
                r = ring[nq % NRING]; nq += 1
                P.dma("pool", r, dram(wdn_d[layer, e, :, g * 512:(g + 1) * 512].rearrange("(kc p) n -> p kc n", p=128), "wdnd"))
                wd[g] = r
            row = (layer * NE + e) * 2

            def regionT(q, ee=e):
                P.begin_region(flags_i[0:1, q, ee:ee + 1], engines=("pe", "act", "sp"))
                xe = XeT[q % 2]
                for t in range(BW // 128):
                    stl = q * (BW // 128) + t
                    xr = xrow[cnt_it[0] % 2]
                    pk = PS[6 + cnt_it[0] % 2]
                    cnt_it[0] += 1
                    P.dma("sp", xr, TV(xs_d[ee * CAP + stl * 128: ee * CAP + (stl + 1) * 128, :], xs_t.buf), owner=xr)
                    pkb = TV(pk.ap.bitcast(BF16), pk.buf)
                    for j in range(8):
                        P.tr(pkb[:, j * 128:(j + 1) * 128], xr[:, j * 128:(j + 1) * 128], ident)
                    P.op("act", lambda e_, pkb=pkb, t=t, xe=xe: e_.activation(
                        out=xe.ap[:, :, t * 128:(t + 1) * 128], in_=pkb.ap.rearrange("p (a b) -> p a b", a=8), func=AF.Copy),
                        reads=[pkb], writes=[xe])
                P.end_region()

            def regionG(q):
                P.begin_region(flags_i[0:1, q, e:e + 1], engines=("pe", "act", "dve"))
                xe = XeT[q % 2]
                for j in range(8):
                    it = cnt_it[1]; cnt_it[1] += 1
                    pg = PS[(2 * it) % 4]; pu = PS[(2 * it + 1) % 4]
                    cg = (j % 4) * 128
                    for k in range(8):
                        P.mm(pg[:, 0:BW], wg[j // 4][:, k, cg:cg + 128], xe[:, k, :], start=(k == 0), stop=(k == 7))
                    for k in range(8):
                        P.mm(pu[:, 0:BW], wg[2 + j // 4][:, k, cg:cg + 128], xe[:, k, :], start=(k == 0), stop=(k == 7))
                    gs = gsb[it % 2]; sg = sgb[it % 2]; us = usb[it % 2]
                    P.ts("dve", gs, pg[:, 0:BW], VT2[:, j, row:row + 1], ALU.add, 7.0, ALU.min)
                    P.act(sg, gs, AF.Sigmoid, scale=1.702)
                    P.ts("dve", us, pu[:, 0:BW], VT2[:, j, row + 1:row + 2], ALU.add, 7.0, ALU.min)
                    P.ts("dve", us, us, -7.0, ALU.max, 1.0, ALU.add)
                    P.tt("dve", gs, gs, sg, ALU.mult)
                    P.tt("dve", actT[q % 2][:, j, :], gs, us, ALU.mult)
                P.end_region()

            def regionB(q):
                P.begin_region(flags_i[0:1, q, e:e + 1], engines=("pe", "act"))
                for t in range(BW // 128):
                    stl = q * (BW // 128) + t
                    yo = yout[cnt_it[2] % 2]
                    for n2 in range(2):
                        pd = PS[4 + (2 * cnt_it[2] + n2) % 2]
                        for k in range(8):
                            P.mm(pd, actT[q % 2][:, k, t * 128:(t + 1) * 128], wd[n2][:, k, :], start=(k == 0), stop=(k == 7))
                        P.copy("act", yo[:, n2 * 512:(n2 + 1) * 512], pd)
                    cnt_it[2] += 1
                    P.dma("act", TV(ys_d[e * CAP + stl * 128: e * CAP + (stl + 1) * 128, :], ys_t.buf), yo, owner=yo)
                P.end_region()

            if e == 0:
                regionT(0)
            for q in range(NQ):
                if q + 1 < NQ:
                    regionT(q + 1)
                regionG(q)
                if q >= 1:
                    regionB(q - 1)
            if e + 1 < NE:
                regionT(0, e + 1)
            regionB(NQ - 1)
        P.barrier()
        A.reset(ma)
        yk = [A.f32(f"yk{k}", [D]) for k in range(4)]
        acc = [A.f32(f"acc{k}", [D]) for k in range(2)]
        GT = [A.f32(f"GT{k}", [128]) for k in range(2)]
        for tt in range(ntt):
            s = 0 if tt < 16 else 1
            tok = slice(tt * 128, (tt + 1) * 128)
            ac = acc[tt % 2]
            for k in range(4):
                P.op("pool", lambda e, tt=tt, k=k: e.indirect_dma_start(
                    out=yk[k].ap, out_offset=None, in_=ys_d,
                    in_offset=bass.IndirectOffsetOnAxis(ap=slot_i.ap[:, tt, k:k + 1], axis=0),
                    bounds_check=REG["ga"], oob_is_err=False),
                    reads=[ys_t, slot_i], writes=[yk[k]], dma=yk[k])
                if k == 0:
                    P.ts("dve", ac, yk[0], gates[:, tt, 0:1], ALU.mult)
                else:
                    P.stt("dve", ac, yk[k], gates[:, tt, k:k + 1], ac, ALU.mult, ALU.add)
            pgt = PS[tt % 2]
            P.mm(pgt[0:NE, 0:128], G[:, tt, :], identf)
            gt = GT[tt % 2]
            P.copy("act", gt[0:NE, :], pgt[0:NE, 0:128])
            for j in range(8):
                pj = PS[2 + j % 4]
                P.mm(pj[:, 0:128], ac[:, j * 128:(j + 1) * 128], identf, start=True, stop=False)
                P.mm(pj[:, 0:128], bdT[0:NE, j * 128:(j + 1) * 128], gt[0:NE, :], start=False, stop=True)
                P.stt("dve", xTc[j][:, tok], pj[:, 0:128], MOD[:, layer, 40 + j, s:s + 1], xTc[j][:, tok], ALU.mult, ALU.add)
        P.barrier()
        A.reset(mm0)

    P.barrier(skip_bg=False)

    moe(0, 18)
    if stage == 2:
        return dump_and_finish()

    LBLK = BLK[0:4]
    m3 = A.mark()
    cqn = A.b16("cqn", [4, L])
    ckvn = A.b16("ckvn", [2, T])
    kpe = A.b16("kpe", [T])
    COS = A.f32("COS", [L]); SINS = A.f32("SINS", [L])
    m3t = A.mark()
    ti = A.i32("ti", [L]); pi_ = A.i32("pi", [1]); pf = A.f32("pf", [1]); invf = A.f32("invf", [1]); sgn = A.f32("sgn", [1])
    ang = A.f32("ang", [L]); nn = A.i32("nn", [L]); nf = A.f32("nf", [L]); fx = A.f32("fx", [L])
    R64 = slice(0, 64)
    P.op("pool", lambda e: e.iota(ti.ap[R64], pattern=[[1, L]], base=0, channel_multiplier=0), writes=[ti])
    P.op("pool", lambda e: e.iota(pi_.ap[R64], pattern=[[0, 1]], base=0, channel_multiplier=1), writes=[pi_])
    P.op("dve", lambda e: e.tensor_single_scalar(out=ti.ap[0:32], in_=ti.ap[0:32], scalar=6, op=ALU.arith_shift_right), reads=[ti], writes=[ti])
    P.op("dve", lambda e: e.tensor_single_scalar(out=ti.ap[32:64], in_=ti.ap[32:64], scalar=63, op=ALU.bitwise_and), reads=[ti], writes=[ti])
    P.copy("dve", ang[R64], ti[R64])
    P.op("dve", lambda e: e.tensor_single_scalar(out=nn.ap[R64, 0:1], in_=pi_.ap[R64], scalar=15, op=ALU.bitwise_and), reads=[pi_], writes=[nn])
    P.copy("dve", pf[R64], nn[R64, 0:1])
    P.act(invf[R64], pf[R64], AF.Exp, scale=-float(np.log(10000.0)) / 16.0)
    P.op("dve", lambda e: e.tensor_scalar(out=nn.ap[R64, 1:2], in0=pi_.ap[R64], scalar1=4, scalar2=1, op0=ALU.arith_shift_right, op1=ALU.bitwise_and), reads=[pi_], writes=[nn])
    P.copy("dve", sgn[R64], nn[R64, 1:2])
    P.ts("dve", sgn[R64], sgn[R64], 2.0, ALU.mult, -1.0, ALU.add)
    P.ts("dve", ang[R64], ang[R64], invf[R64], ALU.mult)
    TWO_PI = float(2 * np.pi)
    for (dst, shift) in ((SINS, 0.0), (COS, float(np.pi / 2))):
        P.ts("dve", nf[R64], ang[R64], shift, ALU.add, 1.0 / TWO_PI, ALU.mult)
        P.copy("dve", nn[R64], nf[R64])
        P.copy("dve", nf[R64], nn[R64])
        P.stt("dve", fx[R64], nf[R64], -TWO_PI, ang[R64], ALU.mult, ALU.add)
        if shift != 0.0:
            P.ts("dve", fx[R64], fx[R64], shift, ALU.add)
        P.ts("dve", nf[R64], fx[R64], -float(np.pi), ALU.is_lt, TWO_PI, ALU.mult)
        P.tt("dve", fx[R64], fx[R64], nf[R64], ALU.add)
        P.ts("dve", nf[R64], fx[R64], float(np.pi), ALU.is_gt, -TWO_PI, ALU.mult)
        P.tt("dve", fx[R64], fx[R64], nf[R64], ALU.add)
        P.act(dst[R64], fx[R64], AF.Sin)
    P.ts("dve", SINS[R64], SINS[R64], sgn[R64], ALU.mult)
    P.barrier()
    A.reset(m3t)
    hT = A.b16("hT", [8, T])
    wdq = A.b16("wdq", [8, 512]); wdkv = A.b16("wdkv", [8, 384])
    P.dma("pool", wdq, dram(wdq_d.rearrange("(kc p) n -> p kc n", p=128), "wdqd"))
    P.dma("pool", wdkv, dram(wdkv_d.rearrange("(kc p) n -> p kc n", p=128), "wdkvd"))
    m3a = A.mark()
    rstd1 = A.f32("rstd1", [T])
    sq3 = [A.f32(f"sq3{k}", [512]) for k in range(3)]
    rstd_all(rstd1, BLK, xTc, D, sq3, [PS[0], PS[1]])
    u3 = [A.f32(f"u3{k}", [512]) for k in range(2)]
    it3 = 0
    for j in range(8):
        for (t0, n) in BLK:
            s = 0 if t0 < L else 1
            u = u3[it3 % 2]; it3 += 1
            P.tt("dve", u[:, 0:n], xTc[j][:, t0:t0 + n], rstd1[:, t0:t0 + n], ALU.mult)
            P.act(hT[:, j, t0:t0 + n], u[:, 0:n], AF.Identity, bias=MOD[:, 1, 0 + j, s:s + 1], scale=DER[:, 1, 0, j, s:s + 1])
    P.barrier()
    A.reset(m3a)
    sq3 = [A.f32(f"sq3{k}", [512]) for k in range(3)]
    cqf = A.f32("cqf", [4, 512]); ckf = A.f32("ckf", [2, 512]); rq = A.f32("rq", [512]); rk = A.f32("rk", [512])
    ta = A.f32("ta", [512]); tb = A.f32("tb", [512])
    for bi, (t0, n) in enumerate(BLK):
        lat = t0 < L
        if lat:
            for m in range(4):
                pk = PS[2 + m % 2]
                for k in range(8):
                    P.mm(pk[:, 0:n], wdq[:, k, m * 128:(m + 1) * 128], hT[:, k, t0:t0 + n], start=(k == 0), stop=(k == 7))
                P.copy("act", cqf[:, m, 0:n], pk[:, 0:n])
            rstd_all(rq, [(0, n)], [cqf[:, m, :] for m in range(4)], 512, sq3, [PS[0]])
            for m in range(4):
                P.tt("dve", cqf[:, m, 0:n], cqf[:, m, 0:n], rq[:, 0:n], ALU.mult)
                P.act(cqn[:, m, t0:t0 + n], cqf[:, m, 0:n], AF.Copy, scale=VT1[:, m, R_QG:R_QG + 1])
        for m in range(2):
            pk = PS[4 + m % 2]
            for k in range(8):
                P.mm(pk[:, 0:n], wdkv[:, k, m * 128:(m + 1) * 128], hT[:, k, t0:t0 + n], start=(k == 0), stop=(k == 7))
            P.copy("act", ckf[:, m, 0:n], pk[:, 0:n])
        rstd_all(rk, [(0, n)], [ckf[:, m, :] for m in range(2)], 256, sq3, [PS[1]])
        for m in range(2):
            P.tt("dve", ckf[:, m, 0:n], ckf[:, m, 0:n], rk[:, 0:n], ALU.mult)
            P.act(ckvn[:, m, t0:t0 + n], ckf[:, m, 0:n], AF.Copy, scale=VT1[:, m, R_KG:R_KG + 1])
        pa = PS[6]; pb = PS[7]
        for k in range(8):
            P.mm(pa[0:64, 0:n], wdkv[:, k, 256:320], hT[:, k, t0:t0 + n], start=(k == 0), stop=(k == 7))
        if lat:
            for k in range(8):
                P.mm(pb[0:64, 0:n], wdkv[:, k, 320:384], hT[:, k, t0:t0 + n], start=(k == 0), stop=(k == 7))
            P.tt("dve", ta[R64, 0:n], pa[0:64, 0:n], COS[R64, t0:t0 + n], ALU.mult)
            P.tt("dve", tb[R64, 0:n], pb[0:64, 0:n], SINS[R64, t0:t0 + n], ALU.mult)
            P.tt("dve", kpe[R64, t0:t0 + n], ta[R64, 0:n], tb[R64, 0:n], ALU.add)
        else:
            P.copy("dve", kpe[R64, t0:t0 + n], pa[0:64, 0:n])
    P.barrier()
    A.reset(m3t)
    wo = [A.b16(f"wo{k}", [D]) for k in range(2)]
    wq = [A.b16(f"wq{k}", [4, 128]) for k in range(2)]
    wr = [A.b16(f"wr{k}", [4, 128]) for k in range(2)]
    wk = [A.b16(f"wk{k}", [2, 128]) for k in range(2)]
    wv = [A.b16(f"wv{k}", [2, 128]) for k in range(2)]
    qn = A.b16("qn", [L]); qr = A.b16("qr", [L]); kn = A.b16("kn", [T]); vh = A.b16("vh", [18, 128])
    attT = A.b16("attT", [L])
    S_sb = [A.f32(f"S_sb{k}", [T]) for k in range(2)]
    Pb = [A.b16(f"Pb{k}", [T]) for k in range(2)]
    PT = [A.b16(f"PT{k}", [18, 128]) for k in range(2)]
    mx = [A.f32(f"mx{k}", [1]) for k in range(2)]
    bmx = [A.f32(f"bmx{k}", [8]) for k in range(2)]
    rsum = [A.f32(f"rsum{k}", [1]) for k in range(2)]
    onb = [A.b16(f"onb{k}", [128]) for k in range(2)]
    qt_ = A.f32("qt_", [512]); ta = A.f32("ta", [512]); tb = A.f32("tb", [512])
    for h in range(8):
        b = h % 2
        P.dma("pool", wq[b], dram(wuqn_d[:, h * 128:(h + 1) * 128].rearrange("(kc p) n -> p kc n", p=128), "wqd"))
        P.dma("pool", wr[b], dram(wuqr_d[:, h * 128:(h + 1) * 128].rearrange("(kc p) n -> p kc n", p=128), "wrd"))
        P.dma("pool", wk[b], dram(wuk_d[:, h * 128:(h + 1) * 128].rearrange("(kc p) n -> p kc n", p=128), "wkd"))
        P.dma("pool", wv[b], dram(wuv_d[:, h * 128:(h + 1) * 128].rearrange("(kc p) n -> p kc n", p=128), "wvd"))
        P.dma("pool", wo[b], dram(wo_d[h * 128:(h + 1) * 128, :], "wod"))
        for bi, (t0, n) in enumerate(LBLK):
            pk = PS[bi % 2]
            for k in range(4):
                P.mm(pk[:, 0:n], wq[b][:, k, :], cqn[:, k, t0:t0 + n], start=(k == 0), stop=(k == 3))
            P.act(qn[:, t0:t0 + n], pk[:, 0:n], AF.Copy, scale=ATTN_SCALE)
            pa = PS[2]; pb = PS[3]
            for k in range(4):
                P.mm(pa[0:64, 0:n], wr[b][:, k, 0:64], cqn[:, k, t0:t0 + n], start=(k == 0), stop=(k == 3))
            for k in range(4):
                P.mm(pb[0:64, 0:n], wr[b][:, k, 64:128], cqn[:, k, t0:t0 + n], start=(k == 0), stop=(k == 3))
            P.tt("dve", ta[R64, 0:n], pa[0:64, 0:n], COS[R64, t0:t0 + n], ALU.mult)
            P.tt("dve", tb[R64, 0:n], pb[0:64, 0:n], SINS[R64, t0:t0 + n], ALU.mult)
            P.tt("dve", qt_[R64, 0:n], ta[R64, 0:n], tb[R64, 0:n], ALU.add)
            P.act(qr[R64, t0:t0 + n], qt_[R64, 0:n], AF.Copy, scale=ATTN_SCALE)
        for bi, (t0, n) in enumerate(BLK):
            pk = PS[4 + bi % 2]
            for k in range(2):
                P.mm(pk[:, 0:n], wk[b][:, k, :], ckvn[:, k, t0:t0 + n], start=(k == 0), stop=(k == 1))
            P.copy("act", kn[:, t0:t0 + n], pk[:, 0:n])
        for kt in range(18):
            pk = PS[6 + kt % 2]
            for k in range(2):
                P.mm(pk[:, 0:128], ckvn[:, k, kt * 128:(kt + 1) * 128], wv[b][:, k, :], start=(k == 0), stop=(k == 1))
            P.copy("dve", vh[:, kt, :], pk[:, 0:128])

        def scores(qt):
            qs = slice(qt * 128, (qt + 1) * 128)
            c = qt % 2
            for bi, (t0, n) in enumerate(BLK):
                pk = PS[bi % 4]
                P.mm(pk[:, 0:n], qn[:, qs], kn[:, t0:t0 + n], start=True, stop=False)
                P.mm(pk[:, 0:n], qr[R64, qs], kpe[R64, t0:t0 + n], start=False, stop=True)
                P.copy("act" if bi % 2 == 0 else "dve", S_sb[c][:, t0:t0 + n], pk[:, 0:n])
                P.red("dve", bmx[c][:, bi:bi + 1], S_sb[c][:, t0:t0 + n], ALU.max)

        def softmax(qt):
            c = qt % 2
            P.red("dve", mx[c], bmx[c][:, 0:5], ALU.max)
            P.ts("dve", mx[c], mx[c], -1.0, ALU.mult)
            P.act(Pb[c], S_sb[c], AF.Exp, bias=mx[c], accum_out=rsum[c])

        def attend_tile(qt, part):
            qs = slice(qt * 128, (qt + 1) * 128)
            c = qt % 2
            for g0 in range(0, 18, 8):
                if (part == 0) != (g0 < 16):
                    continue
                ng = min(8, 18 - g0)
                pk = PS[4 + (g0 // 8) % 2]
                pkb = TV(pk.ap.bitcast(BF16), pk.buf)
                for kk in range(ng):
                    kt = g0 + kk
                    P.tr(pkb[:, kk * 128:(kk + 1) * 128], Pb[c][:, kt * 128:(kt + 1) * 128], ident)
                P.op("dve", lambda e, pkb=pkb, g0=g0, ng=ng, c=c: e.tensor_copy(
                    out=PT[c].ap[:, g0:g0 + ng, :], in_=pkb.ap[:, 0:ng * 128].rearrange("p (a b) -> p a b", a=ng)),
                    reads=[pkb], writes=[PT[c]])
            if part == 0:
                return
            po = PS[6]
            for kt in range(18):
                P.mm(po[:, 0:128], PT[c][:, kt, :], vh[:, kt, :], start=(kt == 0), stop=(kt == 17))
            P.op("dve", lambda e, r=rsum[c]: e.reciprocal(out=r.ap, in_=r.ap), reads=[rsum[c]], writes=[rsum[c]])
            P.ts("dve", onb[c], po[:, 0:128], rsum[c], ALU.mult)
            pt2 = PS[7]
            pt2b = TV(pt2.ap.bitcast(BF16), pt2.buf)
            P.tr(pt2b[:, 0:128], onb[c], ident)
            P.copy("act", attT[:, qs], pt2b[:, 0:128])

        scores(0)
        for qt in range(17):
            if qt >= 1:
                attend_tile(qt - 1, 0)
            if qt + 1 < 16:
                scores(qt + 1)
            if qt < 16:
                softmax(qt)
            if qt >= 1:
                attend_tile(qt - 1, 1)
        for m in range(8):
            for bi, (t0, n) in enumerate(LBLK):
                pk = PS[(m * 4 + bi) % 4]
                P.mm(pk[:, 0:n], wo[b][:, m * 128:(m + 1) * 128], attT[:, t0:t0 + n])
                P.stt("dve", xTc[m][:, t0:t0 + n], pk[:, 0:n], MOD[:, 1, 16 + m, 0:1], xTc[m][:, t0:t0 + n], ALU.mult, ALU.add)
    P.barrier()
    A.reset(m3)
    if stage == 3:
        return dump_and_finish()

    moe(1, 16)
    if stage == 4:
        return dump_and_finish()

    fgb = A.f32("fgb", [D])
    P.dma("sp", fgb, dram(v1_d[R_FG:R_FG + 1, :].to_broadcast([128, D]), "fgd"))
    xo = [A.f32(f"xo{k}", [D]) for k in range(2)]
    sqo = A.f32("sqo", [D]); ssum = A.f32("ssum", [1])
    for tt in range(16):
        tok = slice(tt * 128, (tt + 1) * 128)
        o = xo[tt % 2]
        for half in range(2):
            pk = PS[(2 * tt + half) % 4]
            for jj in range(4):
                P.mm(pk[:, jj * 128:(jj + 1) * 128], xTc[half * 4 + jj][:, tok], identf)
            P.copy("act", o[:, half * 512:(half + 1) * 512], pk)
        P.act(sqo, o, AF.Square, accum_out=ssum)
        P.ts("dve", ssum, ssum, 1.0 / D, ALU.mult, EPS, ALU.add)
        P.act(ssum, ssum, AF.Sqrt)
        P.op("dve", lambda e: e.reciprocal(out=ssum.ap, in_=ssum.ap), reads=[ssum], writes=[ssum])
        P.stt("dve", o, o, ssum, fgb, ALU.mult, ALU.mult)
        P.dma("sp", dram(out_d[tt * 128:(tt + 1) * 128, :], "outd"), o)
    P.barrier()
    P.emit()
    return nc, st, P


def _prep_inputs(inp, b, stage):
    f = np.float32
    v1 = np.zeros((NV1, D), f)
    v1[R_N1G:R_N1G + 2] = inp["norm1_g"]; v1[R_N2G:R_N2G + 2] = inp["norm2_g"]
    v1[R_PB] = inp["pool_b"][0]; v1[R_PS] = inp["pool_scale"][0]; v1[R_FG] = inp["final_g"]
    v1[R_C] = inp["c"][b]; v1[R_CC] = inp["c_ctx"]
    v1[R_ADAB:R_ADAB + 12] = inp["ada_b"].reshape(12, D)
    v1[R_QG, :512] = inp["q_norm_g"][0]; v1[R_KG, :256] = inp["kv_norm_g"][0]
    v1[R_BD:R_BD + 64] = inp["b_down"].reshape(64, D)
    v2 = np.ascontiguousarray(inp["b_gu"].reshape(128, D))
    m = {"x": np.ascontiguousarray(inp["x"][b]), "ctx": np.ascontiguousarray(inp["ctx"][b]), "v1": v1, "v2": v2,
         "ada_w": inp["ada_w"], "pool_w": np.ascontiguousarray(inp["pool_w"][0])}
    if stage >= 3:
        wuq = inp["w_uq"][0].reshape(512, 8, 192)
        perm = np.array([a * 32 + (1 - hf) * 16 + i for a in range(2) for hf in range(2) for i in range(16)])
        rope = wuq[:, :, 128:]
        wkv = inp["w_ukv"][0].reshape(256, 8, 256)
        dkv = inp["w_dkv"][0]
        m.update({"w_dq": np.ascontiguousarray(inp["w_dq"][0]),
                  "w_uq_n": np.ascontiguousarray(wuq[:, :, :128].reshape(512, 1024)),
                  "w_uq_r": np.ascontiguousarray(np.concatenate([rope, rope[:, :, perm]], axis=2).reshape(512, 1024)),
                  "w_dkv": np.ascontiguousarray(np.concatenate([dkv, dkv[:, 256:][:, perm]], axis=1)),
                  "w_uk": np.ascontiguousarray(wkv[:, :, :128].reshape(256, 1024)),
                  "w_uv": np.ascontiguousarray(wkv[:, :, 128:].reshape(256, 1024)),
                  "w_o": np.ascontiguousarray(inp["w_o"][0])})
    if stage >= 2:
        m.update({"router_w": inp["router_w"], "router_b": inp["router_b"], "w_gu": inp["w_gu"], "w_down": inp["w_down"],
                  "b_down": inp["b_down"]})
    return m


def kernel(**inputs):
    inp = {k: np.asarray(v) for k, v in inputs.items()}
    nc, st, P = build_program(STAGE)
    with st:
        in_maps = [_prep_inputs(inp, b, STAGE) for b in range(NCORES)]
        res = run_bass_kernel_spmd(nc, in_maps, core_ids=list(range(NCORES)))
    if STAGE < 99:
        return res
    return np.stack([r["out"] for r in res.results], axis=0)
```

```python
import numpy as np
from contextlib import ExitStack
import concourse.bass as bass
import concourse.mybir as mybir
from concourse.bass_utils import run_bass_kernel_spmd

F32 = mybir.dt.float32
BF16 = mybir.dt.bfloat16
I32 = mybir.dt.int32
U32 = mybir.dt.uint32
ALU = mybir.AluOpType
AF = mybir.ActivationFunctionType
AX = mybir.AxisListType

ENGINES = ("pe", "act", "dve", "pool", "sp")
EPOCH = 30000

D = 1024
L = 2048
C = 256
T = L + C
NE = 32
CAP = 1024
NSLOT = NE * CAP
EPS = 1e-6
ATTN_SCALE = 192 ** -0.5

STAGE = 99
NCORES = 8
DEBUG_TAGS = False
NO_ACT_DMA = False
CUT = 0
ZERO_YS = False


class SemGrp:
    __slots__ = ("name", "ndma", "sem", "bg")

    def __init__(self, name):
        self.name = name
        self.ndma = 0
        self.sem = None
        self.bg = False


class Buf:
    __slots__ = ("name", "last_w", "readers", "semgrp", "dram", "psum")

    def __init__(self, name, grp=None, dram=False, psum=False):
        self.name = name
        self.dram = dram
        self.psum = psum
        self.last_w = None
        self.readers = []
        self.semgrp = grp if grp is not None else SemGrp(name)


class TV:
    __slots__ = ("ap", "buf")

    def __init__(self, ap, buf):
        self.ap = ap
        self.buf = buf

    def __getitem__(self, k):
        return TV(self.ap[k], self.buf)

    def bitcast(self, dt):
        return TV(self.ap.bitcast(dt), self.buf)

    def rearrange(self, s, **kw):
        return TV(self.ap.rearrange(s, **kw), self.buf)

    def bc(self, shape):
        return TV(self.ap.to_broadcast(list(shape)), self.buf)

    def sub(self, name):
        return TV(self.ap, Buf(name))


class Op:
    __slots__ = ("eng", "fn", "deps", "is_dma", "grp", "signal", "epoch", "sigval", "tag", "cond", "dval")


def _ap(x):
    return x.ap if isinstance(x, TV) else x


class Prog:
    def __init__(self, nc, stack):
        self.nc = nc
        self.stack = stack
        self.ops = {e: [] for e in ENGINES}
        self.grps = {}
        self.n = 0
        self.cur_cond = None
        self.creg = {}
        self.nregion = 0

    def op(self, eng, fn, reads=(), writes=(), dma=None):
        o = Op()
        o.eng = eng
        o.fn = fn
        o.tag = None
        if DEBUG_TAGS:
            import sys as _s
            f = _s._getframe(1)
            while f.f_code.co_name in ("mm", "tr", "act", "tt", "ts", "stt", "copy", "memset", "red", "dma", "op"):
                f = f.f_back
            o.tag = f.f_lineno
        o.is_dma = dma is not None
        o.signal = False
        o.grp = None
        o.cond = self.cur_cond
        o.dval = 0
        deps = []
        seen = set()

        def add(d):
            if d is None or id(d) in seen:
                return
            seen.add(id(d))
            if d.is_dma:
                deps.append(("d", d.grp, 16 * d.grp.ndma))
            else:
                if d.eng == eng and eng == "pe" and not o.is_dma:
                    return
                deps.append(("c", d))

        rb = [r.buf if isinstance(r, TV) else r for r in reads if r is not None and not isinstance(r, (int, float))]
        wb = [w.buf if isinstance(w, TV) else w for w in writes]
        def latest(readers):
            last = {}
            for rd in readers:
                last[("d", id(rd.grp)) if rd.is_dma else ("c", rd.eng)] = rd
            return last.values()

        for r in rb:
            add(r.last_w)
            if r.psum:
                for rd in latest(r.readers):
                    if rd.eng != eng:
                        add(rd)
        for w in wb:
            add(w.last_w)
            for rd in latest(w.readers):
                add(rd)
        o.deps = deps
        if o.is_dma:
            g = (dma.buf if isinstance(dma, TV) else dma).semgrp
            o.dval = 16 * g.ndma
            g.ndma += 1
            o.grp = g
            self.grps[id(g)] = g
        for r in rb:
            r.readers.append(o)
        for w in wb:
            w.last_w = o
            w.readers = []
        self.ops[eng].append(o)
        self.n += 1
        return o

    def barrier(self, skip_bg=True):
        lasts = []
        for e in ENGINES:
            for o in reversed(self.ops[e]):
                if not o.is_dma and o.fn is not None:
                    lasts.append(o)
                    break
        dm = [("d", g, 16 * g.ndma) for g in self.grps.values() if g.ndma > 0 and not (skip_bg and g.bg)]
        for e in ENGINES:
            o = Op()
            o.eng = e
            o.fn = None
            o.is_dma = False
            o.signal = False
            o.grp = None
            o.cond = None
            o.dval = 0
            o.deps = [("c", x) for x in lasts if not (x.eng == e)] + list(dm)
            self.ops[e].append(o)

    def begin_region(self, flag, engines=("pe", "act", "dve", "sp")):
        assert self.cur_cond is None
        for e in engines:
            def ld(eng, e=e):
                if e not in self.creg:
                    self.creg[e] = eng.alloc_register(f"cflag_{e}")
                return eng.reg_load(self.creg[e], flag.ap)
            self.op(e, ld, reads=[flag])
        self.nregion += 1
        self.cur_cond = self.nregion

    def end_region(self):
        self.cur_cond = None

    def mm(self, out, lhsT, rhs, start=True, stop=True):
        return self.op("pe", lambda e: e.matmul(out.ap, lhsT=lhsT.ap, rhs=rhs.ap, start=start, stop=stop),
                       reads=[lhsT, rhs], writes=[out])

    def tr(self, out, in_, ident):
        return self.op("pe", lambda e: e.transpose(out=out.ap, in_=in_.ap, identity=ident.ap),
                       reads=[in_, ident], writes=[out])

    def act(self, out, in_, func, bias=0.0, scale=1.0, accum_out=None, eng="act"):
        kw = {}
        if accum_out is not None:
            kw["accum_out"] = accum_out.ap
        return self.op(eng, lambda e: e.activation(out=out.ap, in_=in_.ap, func=func, bias=_ap(bias), scale=_ap(scale), **kw),
                       reads=[in_, bias, scale], writes=[out] + ([accum_out] if accum_out is not None else []))

    def tt(self, eng, out, in0, in1, op):
        return self.op(eng, lambda e: e.tensor_tensor(out=out.ap, in0=in0.ap, in1=in1.ap, op=op),
                       reads=[in0, in1], writes=[out])

    def ts(self, eng, out, in0, s1, op0, s2=None, op1=None, accum_out=None):
        kw = {}
        if op1 is not None:
            kw["op1"] = op1
        if accum_out is not None:
            kw["accum_out"] = accum_out.ap
        return self.op(eng, lambda e: e.tensor_scalar(out=out.ap, in0=in0.ap, scalar1=_ap(s1), scalar2=_ap(s2), op0=op0, **kw),
                       reads=[in0, s1, s2], writes=[out] + ([accum_out] if accum_out is not None else []))

    def stt(self, eng, out, in0, scalar, in1, op0, op1):
        return self.op(eng, lambda e: e.scalar_tensor_tensor(out=out.ap, in0=in0.ap, scalar=_ap(scalar), in1=in1.ap, op0=op0, op1=op1),
                       reads=[in0, scalar, in1], writes=[out])

    def copy(self, eng, out, in_):
        if eng == "act":
            return self.op(eng, lambda e: e.activation(out=out.ap, in_=in_.ap, func=AF.Copy), reads=[in_], writes=[out])
        return self.op(eng, lambda e: e.tensor_copy(out=out.ap, in_=in_.ap), reads=[in_], writes=[out])

    def memset(self, eng, out, val):
        return self.op(eng, lambda e: e.memset(out.ap, val), writes=[out])

    def red(self, eng, out, in_, op, axis=AX.X):
        return self.op(eng, lambda e: e.tensor_reduce(out=out.ap, in_=in_.ap, axis=axis, op=op), reads=[in_], writes=[out])

    def dma(self, q, out, in_, owner=None, **kw):
        if q == "act" and NO_ACT_DMA:
            q = "sp"
        if owner is None:
            owner = in_ if out.buf.dram else out
        return self.op(q, lambda e: e.dma_start(out=out.ap, in_=in_.ap, **kw), reads=[in_], writes=[out], dma=owner)

    def emit(self):
        nc = self.nc
        for e in ENGINES:
            for o in self.ops[e]:
                for d in o.deps:
                    if d[0] == "c":
                        d[1].signal = True
        self.esems = {}
        for e in ENGINES:
            cnt = 0
            for o in self.ops[e]:
                if not o.is_dma and o.signal:
                    o.epoch = cnt // EPOCH
                    o.sigval = cnt % EPOCH + 1
                    cnt += 1
            nep = max(1, (cnt + EPOCH - 1) // EPOCH)
            self.esems[e] = [self.stack.enter_context(nc.semaphore(f"s_{e}{k}")) for k in range(nep)]
        for g in self.grps.values():
            g.sem = self.stack.enter_context(nc.semaphore(f"d_{g.name}"))
        self.nsem = sum(len(v) for v in self.esems.values()) + len(self.grps)
        block = self.stack.enter_context(nc.Block())
        reg = {"pe": block.tensor, "act": block.scalar, "dve": block.vector, "pool": block.gpsimd, "sp": block.sync}
        nwaits = {e: 0 for e in ENGINES}

        def make(e):
            def body(eng):
                def do_waits(o, waited):
                    for d in o.deps:
                        if d[0] == "d":
                            sem, val, key = d[1].sem, d[2], ("d", id(d[1]))
                        else:
                            x = d[1]
                            sem, val, key = self.esems[x.eng][x.epoch], x.sigval, ("c", x.eng, x.epoch)
                        if waited.get(key, 0) >= val:
                            continue
                        waited[key] = val
                        eng.wait_ge(sem, val)
                        nwaits[e] += 1

                def real(o, waited):
                    do_waits(o, waited)
                    if o.fn is None:
                        assert not o.signal
                        return
                    ins = o.fn(eng)
                    if o.is_dma:
                        ins.then_inc(o.grp.sem, 16)
                    elif o.signal:
                        ins.then_inc(self.esems[e][o.epoch], 1)

                pend = {}

                def flush():
                    for sem, n in pend.values():
                        eng.sem_inc(sem, n)
                    pend.clear()

                def ghost(o, waited, first):
                    if o.is_dma:
                        if o.dval > 0:
                            flush()
                            eng.wait_ge(o.grp.sem, o.dval)
                        k_ = id(o.grp.sem)
                        pend[k_] = (o.grp.sem, pend.get(k_, (None, 0))[1] + 16)
                        flush()
                    elif o.signal:
                        sm = self.esems[e][o.epoch]
                        k_ = id(sm)
                        pend[k_] = (sm, pend.get(k_, (None, 0))[1] + 1)

                waited = {}
                ops = self.ops[e]
                i = 0
                while i < len(ops):
                    o = ops[i]
                    if o.cond is None:
                        real(o, waited)
                        i += 1
                        continue
                    j = i
                    while j < len(ops) and ops[j].cond == o.cond:
                        j += 1
                    w1 = dict(waited)
                    with eng.If_ne(self.creg[e], 0):
                        for k in range(i, j):
                            real(ops[k], w1)
                    w2 = dict(waited)
                    with eng.Else():
                        eng.drain()
                        for k in range(i, j):
                            ghost(ops[k], w2, k == i)
                        flush()
                    i = j
            return body

        for e in ENGINES:
            if self.ops[e]:
                reg[e](make(e))
        self.nwaits = nwaits


def _shape(ap, shape):
    if len(shape) <= 1:
        return ap
    names = "abcdefg"[:len(shape)]
    kw = {names[i]: int(shape[i]) for i in range(len(shape) - 1)}
    return ap.rearrange(f"p ({' '.join(names)}) -> p {' '.join(names)}", **kw)


class Arena:
    def __init__(self, P, cols):
        self.P = P
        self.t = P.stack.enter_context(P.nc.sbuf_tensor("arena", [128, cols], F32))
        self.cols = cols
        self.top = 0
        self.n = 0

    def mark(self):
        return self.top

    def reset(self, m):
        self.top = m

    def f32(self, name, shape):
        n = int(np.prod(shape))
        off = self.top
        self.top += n
        assert self.top <= self.cols, f"arena overflow {name}: {self.top} > {self.cols}"
        ap = _shape(self.t[:, off:off + n], shape)
        self.n += 1
        return TV(ap, Buf(f"{name}{self.n}"))

    def b16(self, name, shape, dt=BF16):
        n = int(np.prod(shape))
        nf = (n + 1) // 2
        off = self.top
        self.top += nf
        assert self.top <= self.cols, f"arena overflow {name}: {self.top} > {self.cols}"
        ap = _shape(self.t[:, off:off + nf].bitcast(dt)[:, 0:n], shape)
        self.n += 1
        return TV(ap, Buf(f"{name}{self.n}"))

    def i32(self, name, shape):
        tv = self.f32(name, shape)
        return TV(tv.ap.bitcast(I32), tv.buf)


R_N1G, R_N2G, R_PB, R_PS, R_FG, R_C, R_CC, R_ADAB, R_QG, R_KG, R_BD = 0, 2, 4, 5, 6, 7, 8, 9, 21, 22, 32
NV1 = 96


def build_program(stage=99):
    nc = bass.Bass("TRN2", target_bir_lowering=False)
    dr = {}

    def din(name, shape, dt=F32):
        dr[name] = nc.dram_tensor(name, list(shape), dt, kind="ExternalInput").ap()
        return dr[name]

    x_d = din("x", [L, D]); ctx_d = din("ctx", [C, D])
    v1_d = din("v1", [NV1, D]); v2_d = din("v2", [128, D])
    adaw_d = din("ada_w", [2, D, 6 * D])
    poolw_d = din("pool_w", [4, 256, 256])
    if stage >= 2:
        rw_d = din("router_w", [2, D, NE]); rb_d = din("router_b", [2, NE])
        wgu_d = din("w_gu", [2, NE, D, 2 * D]); wdn_d = din("w_down", [2, NE, D, D]); bdn_d = din("b_down", [2, NE, D])
    if stage >= 3:
        wdq_d = din("w_dq", [D, 512]); wuqn_d = din("w_uq_n", [512, 1024]); wuqr_d = din("w_uq_r", [512, 1024])
        wdkv_d = din("w_dkv", [D, 384]); wuk_d = din("w_uk", [256, 1024]); wuv_d = din("w_uv", [256, 1024])
        wo_d = din("w_o", [D, D])
    out_d = nc.dram_tensor("out", [L, D], F32, kind="ExternalOutput").ap()
    dbg_d = nc.dram_tensor("dbg", [128, 8, T], F32, kind="ExternalOutput").ap() if stage < 99 else None
    if stage >= 2:
        xs_d = nc.dram_tensor("xs_scr", [NSLOT + 1, D], BF16, kind="Internal").ap()
        ys_d = nc.dram_tensor("ys_scr", [NSLOT + 1, D], F32, kind="Internal").ap()

    st = ExitStack()
    P = Prog(nc, st)
    A = Arena(P, 52736)
    dram = lambda ap, name: TV(ap, Buf(name, dram=True))

    PS = []
    for k in range(8):
        t = st.enter_context(nc.psum_tensor(f"ps{k}", [128, 512], F32))
        PS.append(TV(t[:, :], Buf(f"ps{k}", psum=True)))

    def ps_b16(k):
        return TV(PS[k].ap.bitcast(BF16), PS[k].buf)

    xT = A.f32("xT", [8, T])
    xTc = [TV(xT.ap[:, j, :], Buf(f"xT{j}")) for j in range(8)]
    VT1 = A.f32("VT1", [8, NV1])
    VT2 = A.f32("VT2", [8, 128])
    MOD = A.f32("MOD", [2, 48, 2])
    DER = A.f32("DER", [2, 6, 8, 2])
    identf = A.f32("identf", [128])
    onesf = A.f32("onesf", [128])
    ident = A.b16("ident", [128])
    onesb = A.b16("onesb", [128])
    tri = A.b16("tri", [128])
    zero1 = A.f32("zero1", [1])
    pmark = A.mark()

    P.memset("pool", identf, 0.0)
    P.op("pool", lambda e: e.affine_select(out=identf.ap, in_=identf.ap, pattern=[[-1, 128]], compare_op=ALU.not_equal,
                                           fill=1.0, base=0, channel_multiplier=1), reads=[identf], writes=[identf])
    P.copy("dve", ident, identf)
    P.memset("pool", onesf, 1.0)
    P.memset("pool", onesb, 1.0)
    P.memset("pool", zero1, 0.0)
    P.memset("pool", tri, 1.0)
    P.op("pool", lambda e: e.affine_select(out=tri.ap, in_=tri.ap, pattern=[[1, 128]], compare_op=ALU.is_gt,
                                           fill=0.0, base=0, channel_multiplier=-1), reads=[tri], writes=[tri])

    m0 = A.mark()
    v1s = A.f32("v1s", [D]); v2s = A.f32("v2s", [D])
    P.dma("sp", v1s[0:NV1], dram(v1_d, "v1d"))
    P.dma("sp", v2s, dram(v2_d, "v2d"))
    for (src, dst, nr) in ((v1s, VT1, NV1), (v2s, VT2, 128)):
        for j in range(8):
            pk = PS[j % 2]
            P.mm(pk[:, 0:nr], src[0:nr, j * 128:(j + 1) * 128], identf[0:nr, 0:nr])
            P.copy("dve", dst[:, j, :], pk[:, 0:nr])
    def cut():
        P.dma("sp", dram(dbg_d[:, 0, :], "dbgd"), xTc[0])
        P.barrier()
        P.emit()
        return nc, st, P
    if CUT == 1:
        return cut()
    scv = A.f32("scv", [8, 2])
    P.act(scv[:, :, 0], VT1[:, :, R_C], AF.Silu)
    P.act(scv[:, :, 1], VT1[:, :, R_CC], AF.Silu)
    aw = [A.f32(f"aw{k}", [8, 768]) for k in range(2)]
    q = 0
    for i in range(2):
        for cc in range(8):
            w = aw[q % 2]
            P.dma("sp" if q % 2 == 0 else "act", w, dram(adaw_d[i, :, cc * 768:(cc + 1) * 768].rearrange("(kc p) n -> p kc n", p=128), "adawd"))
            for mloc in range(6):
                mch = cc * 6 + mloc
                pk = PS[2 + (mch % 2)]
                for k in range(8):
                    P.mm(pk[:, 0:2], w[:, k, mloc * 128:(mloc + 1) * 128], scv[:, k, :], start=(k == 0), stop=(k == 7))
                P.ts("dve", MOD[:, i, mch, :], pk[:, 0:2], VT1[:, mch % 8, R_ADAB + i * 6 + mch // 8: R_ADAB + i * 6 + mch // 8 + 1], ALU.add)
            q += 1
    if CUT == 2:
        return cut()
    for i in range(2):
        for s in range(2):
            P.stt("dve", DER[:, i, 0, :, s], MOD[:, i, 8:16, s], 1.0, VT1[:, :, R_N1G + i], ALU.add, ALU.mult)
            P.stt("dve", DER[:, i, 1, :, s], MOD[:, i, 32:40, s], 1.0, VT1[:, :, R_N2G + i], ALU.add, ALU.mult)
        if i == 0:
            for s in range(2):
                P.tt("dve", DER[:, 0, 2, :, s], MOD[:, 0, 16:24, s], VT1[:, :, R_PS], ALU.mult)
                P.tt("dve", DER[:, 0, 3, :, s], DER[:, 0, 2, :, s], VT1[:, :, R_PB], ALU.mult)

    if CUT == 3:
        return cut()
    xin = [A.f32(f"xin{k}", [D]) for k in range(3)]
    for tt in range(18):
        xi = xin[tt % 3]
        src = x_d[tt * 128:(tt + 1) * 128, :] if tt < 16 else ctx_d[(tt - 16) * 128:(tt - 15) * 128, :]
        P.dma("sp" if tt % 2 == 0 else "act", xi, dram(src, "xd"))
        for half in range(2):
            pk = PS[4 + (2 * tt + half) % 4]
            for jj in range(4):
                j = half * 4 + jj
                P.mm(pk[:, jj * 128:(jj + 1) * 128], xi[:, j * 128:(j + 1) * 128], identf)
            dst = TV(xT.ap[:, half * 4:(half + 1) * 4, tt * 128:(tt + 1) * 128], None)
            srcv = pk.rearrange("p (a b) -> p a b", a=4)
            if half == 0:
                P.op("dve", lambda e, d=dst, s_=srcv: e.tensor_copy(out=d.ap, in_=s_.ap), reads=[pk], writes=xTc[0:4])
            else:
                P.op("act", lambda e, d=dst, s_=srcv: e.activation(out=d.ap, in_=s_.ap, func=AF.Copy), reads=[pk], writes=xTc[4:8])
    if stage >= 2:
        ZOFF = 51600
        zt = TV(A.t[:, ZOFF:ZOFF + 1024].bitcast(BF16), Buf("zt"))
        zt.buf.semgrp.bg = True
        xs_t = TV(xs_d, Buf("xs_scr", dram=True))
        ys_t = TV(ys_d, Buf("ys_scr", dram=True))
        P.memset("pool", zt, 0.0)
        for r in range(0, NSLOT, 256):
            P.dma("sp", TV(xs_d[r:r + 256, :].rearrange("(p a) n -> p (a n)", p=128), xs_t.buf), zt, owner=zt)
        ztf = TV(zt.ap.bitcast(F32), zt.buf)
        if ZERO_YS:
            for r in range(0, NSLOT, 128):
                P.dma("sp", TV(ys_d[r:r + 128, :], ys_t.buf), ztf, owner=zt)
        P.dma("sp", TV(xs_d[NSLOT:NSLOT + 1, :], xs_t.buf), zt[0:1, 0:D], owner=zt)
        P.dma("sp", TV(ys_d[NSLOT:NSLOT + 1, :], ys_t.buf), ztf[0:1, 0:D], owner=zt)

    P.barrier()
    A.reset(m0)

    def dump_and_finish():
        for j in range(8):
            P.dma("sp", dram(dbg_d[:, j, :], "dbgd"), xTc[j])
        P.barrier()
        P.emit()
        return nc, st, P

    if stage == 0:
        return dump_and_finish()

    BLK = [(0, 512), (512, 512), (1024, 512), (1536, 512), (2048, 256)]

    def rstd_all(rstd, ntok_blocks, src_chunks, nfeat, sq_pool, pss):
        nch = len(src_chunks)
        for bi, (t0, n) in enumerate(ntok_blocks):
            pk = pss[bi % len(pss)]
            for j in range(nch):
                sq = sq_pool[(bi * nch + j) % len(sq_pool)]
                P.act(sq[:, 0:n], src_chunks[j][:, t0:t0 + n], AF.Square)
                P.mm(pk[:, 0:n], onesf, sq[:, 0:n], start=(j == 0), stop=(j == nch - 1))
            P.ts("dve", rstd[:, t0:t0 + n], pk[:, 0:n], 1.0 / nfeat, ALU.mult, EPS, ALU.add)
            P.act(rstd[:, t0:t0 + n], rstd[:, t0:t0 + n], AF.Sqrt)
            P.op("dve", lambda e, o=rstd[:, t0:t0 + n]: e.reciprocal(out=o.ap, in_=o.ap), reads=[rstd], writes=[rstd])

    m1 = A.mark()
    rstd = A.f32("rstd", [T])
    sqp = [A.f32(f"sq{k}", [512]) for k in range(3)]
    rstd_all(rstd, BLK, xTc, D, sqp, [PS[0], PS[1]])
    WIN = (2, 4, 8, 16)
    PADW = 16
    segs = ((0, L, 0), (L, C, 1))
    hp = [A.f32(f"hp{s}", [ln + 2 * PADW]) for (_, ln, s) in segs]
    sa = [A.f32(f"sa{s}", [ln + 2 * PADW]) for (_, ln, s) in segs]
    sb = [A.f32(f"sb{s}", [ln + 2 * PADW]) for (_, ln, s) in segs]
    icnt = [[A.f32(f"ic{s}_{g}", [ln]) for g in range(4)] for (_, ln, s) in segs]
    dT = A.b16("dT", [2, T])
    uu = A.f32("uu", [T])
    pw = A.b16("pw", [4, 2, 256])
    P.dma("pool", pw, dram(poolw_d.rearrange("g (kc p) n -> p g kc n", p=128), "pwd"))

    def window_sum(si, w, src, dst_final):
        ln = segs[si][1]
        W = ln + 2 * PADW
        cur = src
        bufs = [sa[si], sb[si]]
        step = 1
        k = 0
        ww = 2
        while ww <= w:
            dst = bufs[k % 2]
            if ww == 2:
                P.tt("dve", dst[:, 1:W], cur[:, 0:W - 1], cur[:, 1:W], ALU.add)
            else:
                sh = ww // 4
                P.tt("dve", dst[:, sh:W - sh], cur[:, 0:W - 2 * sh], cur[:, 2 * sh:W], ALU.add)
            cur = dst
            k += 1
            ww *= 2
        return cur

    for si, (t0, ln, s) in enumerate(segs):
        for bufz in (hp[si], sa[si], sb[si]):
            P.memset("pool", bufz, 0.0)
    for si, (t0, ln, s) in enumerate(segs):
        P.memset("pool", hp[si][:, PADW:PADW + ln], 1.0)
        for g in range(4):
            cur = window_sum(si, WIN[g], hp[si], None)
            P.op("dve", lambda e, o=icnt[si][g], c=cur, ln=ln: e.reciprocal(out=o.ap, in_=c.ap[:, PADW:PADW + ln]),
                 reads=[cur], writes=[icnt[si][g]])
    for g in range(4):
        for kc in range(2):
            j = 2 * g + kc
            P.tt("dve", uu, xTc[j], rstd, ALU.mult)
            for si, (t0, ln, s) in enumerate(segs):
                P.act(hp[si][:, PADW:PADW + ln], uu[:, t0:t0 + ln], AF.Identity,
                      bias=MOD[:, 0, 0 + j, s:s + 1], scale=DER[:, 0, 0, j, s:s + 1])
                cur = window_sum(si, WIN[g], hp[si], None)
                P.tt("pool", cur[:, PADW:PADW + ln], cur[:, PADW:PADW + ln], icnt[si][g], ALU.mult)
                P.tt("pool", dT[:, kc, t0:t0 + ln], cur[:, PADW:PADW + ln], hp[si][:, PADW:PADW + ln], ALU.subtract)
        for mc in range(2):
            j = 2 * g + mc
            for bi, (t0, n) in enumerate(BLK):
                s = 0 if t0 < L else 1
                pk = PS[2 + (bi % 4)]
                for kc in range(2):
                    P.mm(pk[:, 0:n], pw[:, g, kc, mc * 128:(mc + 1) * 128], dT[:, kc, t0:t0 + n], start=(kc == 0), stop=(kc == 1))
                P.stt("dve", xTc[j][:, t0:t0 + n], pk[:, 0:n], DER[:, 0, 2, j, s:s + 1], xTc[j][:, t0:t0 + n], ALU.mult, ALU.add)
                P.ts("dve", xTc[j][:, t0:t0 + n], xTc[j][:, t0:t0 + n], DER[:, 0, 3, j, s:s + 1], ALU.add)
    P.barrier()
    A.reset(m1)

    if stage == 1:
        return dump_and_finish()

    REG = {}

    def mkregs(e):
        for nm, val in (("sc", NSLOT - 1), ("ga", NSLOT)):
            r = e.alloc_register(f"bc_{nm}")
            e.reg_mov(r, val)
            REG[nm] = r
        return e.nop()
    P.op("pool", mkregs)
    def moe(layer, ntt):
        ntok = ntt * 128
        blks = [b for b in BLK if b[0] < ntok]
        mm0 = A.mark()
        slot_i = A.i32("slot_i", [18, 4])
        gates = A.f32("gates", [18, 4])
        G = A.f32("G", [18, NE])
        masks = A.b16("masks", [18, NE])
        ebase = A.f32("ebase", [NE])
        ebi = A.i32("ebi", [NE])
        rw = A.f32("rw", [8, NE])
        rb = A.f32("rb", [NE])
        bdT = A.f32("bdT", [D])
        flags_f = A.f32("flags_f", [4, NE]); flags_i = A.i32("flags_i", [4, NE])
        P.op("pool", lambda e: e.iota(ebi.ap, pattern=[[CAP, NE]], base=0, channel_multiplier=0), writes=[ebi])
        P.copy("dve", ebase, ebi)
        P.dma("sp", rw, dram(rw_d[layer].rearrange("(kc p) n -> p kc n", p=128), "rwd"))
        P.dma("sp", rb[0:1, :], dram(rb_d[layer:layer + 1, :], "rbd"))
        P.dma("sp", bdT[0:NE, :], dram(bdn_d[layer], "bdd"))
        ma = A.mark()
        rstd2 = A.f32("rstd2", [T])
        sq2 = [A.f32(f"sq2{k}", [512]) for k in range(3)]
        rstd_all(rstd2, blks, xTc, D, sq2, [PS[0], PS[1]])
        u2 = [A.f32(f"u2{k}", [128]) for k in range(2)]
        h2f = [A.f32(f"h2f{k}", [8, 128]) for k in range(2)]
        h2b = [A.b16(f"h2b{k}", [8, 128]) for k in range(2)]
        h2tok = [A.b16(f"h2tok{k}", [D]) for k in range(2)]
        lg = A.f32("lg", [NE]); top8 = A.f32("top8", [8]); maskf = A.f32("maskf", [NE])
        pos = A.f32("pos", [NE]); t1 = A.f32("t1", [NE]); oh = A.f32("oh", [NE]); sel = A.f32("sel", [NE])
        slotsf = A.f32("slotsf", [4]); negm = A.f32("negm", [1]); ex = A.f32("ex", [4]); sumex = A.f32("sumex", [1])
        for tt in range(ntt):
            s = 0 if tt < 16 else 1
            tok = slice(tt * 128, (tt + 1) * 128)
            hf = h2f[tt % 2]; hb = h2b[tt % 2]
            for j in range(8):
                u = u2[j % 2]
                P.tt("dve", u, xTc[j][:, tok], rstd2[:, tok], ALU.mult)
                P.act(hf[:, j, :], u, AF.Identity, bias=MOD[:, layer, 24 + j, s:s + 1], scale=DER[:, layer, 1, j, s:s + 1])
                P.act(hb[:, j, :], u, AF.Identity, bias=MOD[:, layer, 24 + j, s:s + 1], scale=DER[:, layer, 1, j, s:s + 1])
            pl = PS[2 + tt % 2]
            for j in range(8):
                P.mm(pl[:, 0:NE], hf[:, j, :], rw[:, j, :], start=(j == 0), stop=False)
            P.mm(pl[:, 0:NE], onesf[0:1, :], rb[0:1, :], start=False, stop=True)
            P.copy("dve", lg, pl[:, 0:NE])
            P.op("dve", lambda e: e.max(out=top8.ap, in_=lg.ap), reads=[lg], writes=[top8])
            P.ts("dve", maskf, lg, top8[:, 3:4], ALU.is_ge)
            P.copy("dve", masks[:, tt, :], maskf)
            pp = PS[4 + tt % 2]
            P.mm(pp[:, 0:NE], tri, masks[:, tt, :], start=True, stop=(tt == 0))
            for i in range(tt):
                P.mm(pp[:, 0:NE], onesb, masks[:, i, :], start=False, stop=(i == tt - 1))
            P.copy("dve", pos, pp[:, 0:NE])
            P.ts("dve", t1, pos, float(CAP), ALU.is_ge, 1.0e6, ALU.mult)
            P.tt("dve", pos, pos, ebase, ALU.add)
            P.tt("dve", pos, pos, t1, ALU.add)
            P.ts("dve", pos, pos, float(NSLOT), ALU.min)
            P.ts("dve", negm, top8[:, 0:1], -1.0, ALU.mult)
            P.act(ex, top8[:, 0:4], AF.Exp, bias=negm, accum_out=sumex)
            P.op("dve", lambda e: e.reciprocal(out=sumex.ap, in_=sumex.ap), reads=[sumex], writes=[sumex])
            P.ts("dve", gates[:, tt, :], ex, sumex, ALU.mult)
            for k in range(4):
                P.ts("dve", oh, lg, top8[:, k:k + 1], ALU.is_equal)
                P.tt("dve", sel, oh, pos, ALU.mult)
                P.red("dve", slotsf[:, k:k + 1], sel, ALU.add)
                if k == 0:
                    P.ts("dve", G[:, tt, :], oh, gates[:, tt, 0:1], ALU.mult)
                else:
                    P.stt("dve", G[:, tt, :], oh, gates[:, tt, k:k + 1], G[:, tt, :], ALU.mult, ALU.add)
            P.copy("dve", slot_i[:, tt, :], slotsf)
            pt_ = PS[6 + tt % 2]
            ptb = TV(pt_.ap.bitcast(BF16), pt_.buf)
            for j in range(8):
                P.tr(ptb[:, j * 128:(j + 1) * 128], hb[:, j, :], ident)
            ht = h2tok[tt % 2]
            P.copy("act", ht, ptb)
            for k in range(4):
                P.op("pool", lambda e, ht=ht, tt=tt, k=k: e.indirect_dma_start(
                    out=xs_d, out_offset=bass.IndirectOffsetOnAxis(ap=slot_i.ap[:, tt, k:k + 1], axis=0),
                    in_=ht.ap, in_offset=None, bounds_check=REG["sc"], oob_is_err=False),
                    reads=[ht, slot_i], writes=[xs_t], dma=ht)
        pc = PS[0]
        for tt in range(ntt):
            P.mm(pc[:, 0:NE], onesb, masks[:, tt, :], start=(tt == 0), stop=(tt == ntt - 1))
        for q in range(4):
            P.ts("dve", flags_f[:, q, :], pc[:, 0:NE], float(q * 256), ALU.is_gt)
        P.copy("dve", flags_i, flags_f)
        P.barrier()
        A.reset(ma)
        BW = 256
        NQ = CAP // BW
        NRING = 9
        ring = [A.b16(f"ring{k}", [8, 512]) for k in range(NRING)]
        XeT = [A.b16(f"XeT{k}", [8, BW]) for k in range(2)]
        actT = [A.b16(f"actT{k}", [8, BW]) for k in range(2)]
        xrow = [A.b16(f"xrow{k}", [D]) for k in range(2)]
        yout = [A.f32(f"yout{k}", [D]) for k in range(2)]
        gsb = [A.f32(f"gsb{k}", [BW]) for k in range(2)]
        sgb = [A.f32(f"sgb{k}", [BW]) for k in range(2)]
        usb = [A.f32(f"usb{k}", [BW]) for k in range(2)]
        GORD = (0, 2, 1, 3)
        nq = 0
        cnt_it = [0, 0, 0]
        for e in range(NE):
            wg = {}
            for g in GORD:
                r = ring[nq % NRING]; nq += 1
                P.dma("pool", r, dram(wgu_d[layer, e, :, g * 512:(g + 1) * 512].rearrange("(kc p) n -> p kc n", p=128), "wgud"))
                wg[g] = r
            wd = {}
            for g in range(2):
                r = ring[nq % NRING]; nq += 1
                P.dma("pool", r, dram(wdn_d[layer, e, :, g * 512:(g + 1) * 512].rearrange("(kc p) n -> p kc n", p=128), "wdnd"))
                wd[g] = r
            row = (layer * NE + e) * 2

            def regionT(q, ee=e):
                P.begin_region(flags_i[0:1, q, ee:ee + 1], engines=("pe", "act", "sp"))
                xe = XeT[q % 2]
                for t in range(BW // 128):
                    stl = q * (BW // 128) + t
                    xr = xrow[cnt_it[0] % 2]
                    pk = PS[6 + cnt_it[0] % 2]
                    cnt_it[0] += 1
                    P.dma("sp", xr, TV(xs_d[ee * CAP + stl * 128: ee * CAP + (stl + 1) * 128, :], xs_t.buf), owner=xr)
                    pkb = TV(pk.ap.bitcast(BF16), pk.buf)
                    for j in range(8):
                        P.tr(pkb[:, j * 128:(j + 1) * 128], xr[:, j * 128:(j + 1) * 128], ident)
                    P.op("act", lambda e_, pkb=pkb, t=t, xe=xe: e_.activation(
                        out=xe.ap[:, :, t * 128:(t + 1) * 128], in_=pkb.ap.rearrange("p (a b) -> p a b", a=8), func=AF.Copy),
                        reads=[pkb], writes=[xe])
                P.end_region()

            def regionG(q):
                P.begin_region(flags_i[0:1, q, e:e + 1], engines=("pe", "act", "dve"))
                xe = XeT[q % 2]
                for j in range(8):
                    it = cnt_it[1]; cnt_it[1] += 1
                    pg = PS[(2 * it) % 4]; pu = PS[(2 * it + 1) % 4]
                    cg = (j % 4) * 128
                    for k in range(8):
                        P.mm(pg[:, 0:BW], wg[j // 4][:, k, cg:cg + 128], xe[:, k, :], start=(k == 0), stop=(k == 7))
                    for k in range(8):
                        P.mm(pu[:, 0:BW], wg[2 + j // 4][:, k, cg:cg + 128], xe[:, k, :], start=(k == 0), stop=(k == 7))
                    gs = gsb[it % 2]; sg = sgb[it % 2]; us = usb[it % 2]
                    P.ts("dve", gs, pg[:, 0:BW], VT2[:, j, row:row + 1], ALU.add, 7.0, ALU.min)
                    P.act(sg, gs, AF.Sigmoid, scale=1.702)
                    P.ts("dve", us, pu[:, 0:BW], VT2[:, j, row + 1:row + 2], ALU.add, 7.0, ALU.min)
                    P.ts("dve", us, us, -7.0, ALU.max, 1.0, ALU.add)
                    P.tt("dve", gs, gs, sg, ALU.mult)
                    P.tt("dve", actT[q % 2][:, j, :], gs, us, ALU.mult)
                P.end_region()

            def regionB(q):
                P.begin_region(flags_i[0:1, q, e:e + 1], engines=("pe", "act"))
                for t in range(BW // 128):
                    stl = q * (BW // 128) + t
                    yo = yout[cnt_it[2] % 2]
                    for n2 in range(2):
                        pd = PS[4 + (2 * cnt_it[2] + n2) % 2]
                        for k in range(8):
                            P.mm(pd, actT[q % 2][:, k, t * 128:(t + 1) * 128], wd[n2][:, k, :], start=(k == 0), stop=(k == 7))
                        P.copy("act", yo[:, n2 * 512:(n2 + 1) * 512], pd)
                    cnt_it[2] += 1
                    P.dma("act", TV(ys_d[e * CAP + stl * 128: e * CAP + (stl + 1) * 128, :], ys_t.buf), yo, owner=yo)
                P.end_region()

            if e == 0:
                regionT(0)
            for q in range(NQ):
                if q + 1 < NQ:
                    regionT(q + 1)
                regionG(q)
                if q >= 1:
                    regionB(q - 1)
            if e + 1 < NE:
                regionT(0, e + 1)
            regionB(NQ - 1)
        P.barrier()
        A.reset(ma)
        yk = [A.f32(f"yk{k}", [D]) for k in range(4)]
        acc = [A.f32(f"acc{k}", [D]) for k in range(2)]
        GT = [A.f32(f"GT{k}", [128]) for k in range(2)]
        for tt in range(ntt):
            s = 0 if tt < 16 else 1
            tok = slice(tt * 128, (tt + 1) * 128)
            ac = acc[tt % 2]
            for k in range(4):
                P.op("pool", lambda e, tt=tt, k=k: e.indirect_dma_start(
                    out=yk[k].ap, out_offset=None, in_=ys_d,
                    in_offset=bass.IndirectOffsetOnAxis(ap=slot_i.ap[:, tt, k:k + 1], axis=0),
                    bounds_check=REG["ga"], oob_is_err=False),
                    reads=[ys_t, slot_i], writes=[yk[k]], dma=yk[k])
                if k == 0:
                    P.ts("dve", ac, yk[0], gates[:, tt, 0:1], ALU.mult)
                else:
                    P.stt("dve", ac, yk[k], gates[:, tt, k:k + 1], ac, ALU.mult, ALU.add)
            pgt = PS[tt % 2]
            P.mm(pgt[0:NE, 0:128], G[:, tt, :], identf)
            gt = GT[tt % 2]
            P.copy("act", gt[0:NE, :], pgt[0:NE, 0:128])
            for j in range(8):
                pj = PS[2 + j % 4]
                P.mm(pj[:, 0:128], ac[:, j * 128:(j + 1) * 128], identf, start=True, stop=False)
                P.mm(pj[:, 0:128], bdT[0:NE, j * 128:(j + 1) * 128], gt[0:NE, :], start=False, stop=True)
                P.stt("dve", xTc[j][:, tok], pj[:, 0:128], MOD[:, layer, 40 + j, s:s + 1], xTc[j][:, tok], ALU.mult, ALU.add)
        P.barrier()
        A.reset(mm0)

    P.barrier(skip_bg=False)

    moe(0, 18)
    if stage == 2:
        return dump_and_finish()

    LBLK = BLK[0:4]
    m3 = A.mark()
    cqn = A.b16("cqn", [4, L])
    ckvn = A.b16("ckvn", [2, T])
    kpe = A.b16("kpe", [T])
    COS = A.f32("COS", [L]); SINS = A.f32("SINS", [L])
    m3t = A.mark()
    ti = A.i32("ti", [L]); pi_ = A.i32("pi", [1]); pf = A.f32("pf", [1]); invf = A.f32("invf", [1]); sgn = A.f32("sgn", [1])
    ang = A.f32("ang", [L]); nn = A.i32("nn", [L]); nf = A.f32("nf", [L]); fx = A.f32("fx", [L])
    R64 = slice(0, 64)
    P.op("pool", lambda e: e.iota(ti.ap[R64], pattern=[[1, L]], base=0, channel_multiplier=0), writes=[ti])
    P.op("pool", lambda e: e.iota(pi_.ap[R64], pattern=[[0, 1]], base=0, channel_multiplier=1), writes=[pi_])
    P.op("dve", lambda e: e.tensor_single_scalar(out=ti.ap[0:32], in_=ti.ap[0:32], scalar=6, op=ALU.arith_shift_right), reads=[ti], writes=[ti])
    P.op("dve", lambda e: e.tensor_single_scalar(out=ti.ap[32:64], in_=ti.ap[32:64], scalar=63, op=ALU.bitwise_and), reads=[ti], writes=[ti])
    P.copy("dve", ang[R64], ti[R64])
    P.op("dve", lambda e: e.tensor_single_scalar(out=nn.ap[R64, 0:1], in_=pi_.ap[R64], scalar=15, op=ALU.bitwise_and), reads=[pi_], writes=[nn])
    P.copy("dve", pf[R64], nn[R64, 0:1])
    P.act(invf[R64], pf[R64], AF.Exp, scale=-float(np.log(10000.0)) / 16.0)
    P.op("dve", lambda e: e.tensor_scalar(out=nn.ap[R64, 1:2], in0=pi_.ap[R64], scalar1=4, scalar2=1, op0=ALU.arith_shift_right, op1=ALU.bitwise_and), reads=[pi_], writes=[nn])
    P.copy("dve", sgn[R64], nn[R64, 1:2])
    P.ts("dve", sgn[R64], sgn[R64], 2.0, ALU.mult, -1.0, ALU.add)
    P.ts("dve", ang[R64], ang[R64], invf[R64], ALU.mult)
    TWO_PI = float(2 * np.pi)
    for (dst, shift) in ((SINS, 0.0), (COS, float(np.pi / 2))):
        P.ts("dve", nf[R64], ang[R64], shift, ALU.add, 1.0 / TWO_PI, ALU.mult)
        P.copy("dve", nn[R64], nf[R64])
        P.copy("dve", nf[R64], nn[R64])
        P.stt("dve", fx[R64], nf[R64], -TWO_PI, ang[R64], ALU.mult, ALU.add)
        if shift != 0.0:
            P.ts("dve", fx[R64], fx[R64], shift, ALU.add)
        P.ts("dve", nf[R64], fx[R64], -float(np.pi), ALU.is_lt, TWO_PI, ALU.mult)
        P.tt("dve", fx[R64], fx[R64], nf[R64], ALU.add)
        P.ts("dve", nf[R64], fx[R64], float(np.pi), ALU.is_gt, -TWO_PI, ALU.mult)
        P.tt("dve", fx[R64], fx[R64], nf[R64], ALU.add)
        P.act(dst[R64], fx[R64], AF.Sin)
    P.ts("dve", SINS[R64], SINS[R64], sgn[R64], ALU.mult)
    P.barrier()
    A.reset(m3t)
    hT = A.b16("hT", [8, T])
    wdq = A.b16("wdq", [8, 512]); wdkv = A.b16("wdkv", [8, 384])
    P.dma("pool", wdq, dram(wdq_d.rearrange("(kc p) n -> p kc n", p=128), "wdqd"))
    P.dma("pool", wdkv, dram(wdkv_d.rearrange("(kc p) n -> p kc n", p=128), "wdkvd"))
    m3a = A.mark()
    rstd1 = A.f32("rstd1", [T])
    sq3 = [A.f32(f"sq3{k}", [512]) for k in range(3)]
    rstd_all(rstd1, BLK, xTc, D, sq3, [PS[0], PS[1]])
    u3 = [A.f32(f"u3{k}", [512]) for k in range(2)]
    it3 = 0
    for j in range(8):
        for (t0, n) in BLK:
            s = 0 if t0 < L else 1
            u = u3[it3 % 2]; it3 += 1
            P.tt("dve", u[:, 0:n], xTc[j][:, t0:t0 + n], rstd1[:, t0:t0 + n], ALU.mult)
            P.act(hT[:, j, t0:t0 + n], u[:, 0:n], AF.Identity, bias=MOD[:, 1, 0 + j, s:s + 1], scale=DER[:, 1, 0, j, s:s + 1])
    P.barrier()
    A.reset(m3a)
    sq3 = [A.f32(f"sq3{k}", [512]) for k in range(3)]
    cqf = A.f32("cqf", [4, 512]); ckf = A.f32("ckf", [2, 512]); rq = A.f32("rq", [512]); rk = A.f32("rk", [512])
    ta = A.f32("ta", [512]); tb = A.f32("tb", [512])
    for bi, (t0, n) in enumerate(BLK):
        lat = t0 < L
        if lat:
            for m in range(4):
                pk = PS[2 + m % 2]
                for k in range(8):
                    P.mm(pk[:, 0:n], wdq[:, k, m * 128:(m + 1) * 128], hT[:, k, t0:t0 + n], start=(k == 0), stop=(k == 7))
                P.copy("act", cqf[:, m, 0:n], pk[:, 0:n])
            rstd_all(rq, [(0, n)], [cqf[:, m, :] for m in range(4)], 512, sq3, [PS[0]])
            for m in range(4):
                P.tt("dve", cqf[:, m, 0:n], cqf[:, m, 0:n], rq[:, 0:n], ALU.mult)
                P.act(cqn[:, m, t0:t0 + n], cqf[:, m, 0:n], AF.Copy, scale=VT1[:, m, R_QG:R_QG + 1])
        for m in range(2):
            pk = PS[4 + m % 2]
            for k in range(8):
                P.mm(pk[:, 0:n], wdkv[:, k, m * 128:(m + 1) * 128], hT[:, k, t0:t0 + n], start=(k == 0), stop=(k == 7))
            P.copy("act", ckf[:, m, 0:n], pk[:, 0:n])
        rstd_all(rk, [(0, n)], [ckf[:, m, :] for m in range(2)], 256, sq3, [PS[1]])
        for m in range(2):
            P.tt("dve", ckf[:, m, 0:n], ckf[:, m, 0:n], rk[:, 0:n], ALU.mult)
            P.act(ckvn[:, m, t0:t0 + n], ckf[:, m, 0:n], AF.Copy, scale=VT1[:, m, R_KG:R_KG + 1])
        pa = PS[6]; pb = PS[7]
        for k in range(8):
            P.mm(pa[0:64, 0:n], wdkv[:, k, 256:320], hT[:, k, t0:t0 + n], start=(k == 0), stop=(k == 7))
        if lat:
            for k in range(8):
                P.mm(pb[0:64, 0:n], wdkv[:, k, 320:384], hT[:, k, t0:t0 + n], start=(k == 0), stop=(k == 7))
            P.tt("dve", ta[R64, 0:n], pa[0:64, 0:n], COS[R64, t0:t0 + n], ALU.mult)
            P.tt("dve", tb[R64, 0:n], pb[0:64, 0:n], SINS[R64, t0:t0 + n], ALU.mult)
            P.tt("dve", kpe[R64, t0:t0 + n], ta[R64, 0:n], tb[R64, 0:n], ALU.add)
        else:
            P.copy("dve", kpe[R64, t0:t0 + n], pa[0:64, 0:n])
    P.barrier()
    A.reset(m3t)
    wo = [A.b16(f"wo{k}", [D]) for k in range(2)]
    wq = [A.b16(f"wq{k}", [4, 128]) for k in range(2)]
    wr = [A.b16(f"wr{k}", [4, 128]) for k in range(2)]
    wk = [A.b16(f"wk{k}", [2, 128]) for k in range(2)]
    wv = [A.b16(f"wv{k}", [2, 128]) for k in range(2)]
    qn = A.b16("qn", [L]); qr = A.b16("qr", [L]); kn = A.b16("kn", [T]); vh = A.b16("vh", [18, 128])
    attT = A.b16("attT", [L])
    S_sb = [A.f32(f"S_sb{k}", [T]) for k in range(2)]
    Pb = [A.b16(f"Pb{k}", [T]) for k in range(2)]
    PT = [A.b16(f"PT{k}", [18, 128]) for k in range(2)]
    mx = [A.f32(f"mx{k}", [1]) for k in range(2)]
    bmx = [A.f32(f"bmx{k}", [8]) for k in range(2)]
    rsum = [A.f32(f"rsum{k}", [1]) for k in range(2)]
    onb = [A.b16(f"onb{k}", [128]) for k in range(2)]
    qt_ = A.f32("qt_", [512]); ta = A.f32("ta", [512]); tb = A.f32("tb", [512])
    for h in range(8):
        b = h % 2
        P.dma("pool", wq[b], dram(wuqn_d[:, h * 128:(h + 1) * 128].rearrange("(kc p) n -> p kc n", p=128), "wqd"))
        P.dma("pool", wr[b], dram(wuqr_d[:, h * 128:(h + 1) * 128].rearrange("(kc p) n -> p kc n", p=128), "wrd"))
        P.dma("pool", wk[b], dram(wuk_d[:, h * 128:(h + 1) * 128].rearrange("(kc p) n -> p kc n", p=128), "wkd"))
        P.dma("pool", wv[b], dram(wuv_d[:, h * 128:(h + 1) * 128].rearrange("(kc p) n -> p kc n", p=128), "wvd"))
        P.dma("pool", wo[b], dram(wo_d[h * 128:(h + 1) * 128, :], "wod"))
        for bi, (t0, n) in enumerate(LBLK):
            pk = PS[bi % 2]
            for k in range(4):
                P.mm(pk[:, 0:n], wq[b][:, k, :], cqn[:, k, t0:t0 + n], start=(k == 0), stop=(k == 3))
            P.act(qn[:, t0:t0 + n], pk[:, 0:n], AF.Copy, scale=ATTN_SCALE)
            pa = PS[2]; pb = PS[3]
            for k in range(4):
                P.mm(pa[0:64, 0:n], wr[b][:, k, 0:64], cqn[:, k, t0:t0 + n], start=(k == 0), stop=(k == 3))
            for k in range(4):
                P.mm(pb[0:64, 0:n], wr[b][:, k, 64:128], cqn[:, k, t0:t0 + n], start=(k == 0), stop=(k == 3))
            P.tt("dve", ta[R64, 0:n], pa[0:64, 0:n], COS[R64, t0:t0 + n], ALU.mult)
            P.tt("dve", tb[R64, 0:n], pb[0:64, 0:n], SINS[R64, t0:t0 + n], ALU.mult)
            P.tt("dve", qt_[R64, 0:n], ta[R64, 0:n], tb[R64, 0:n], ALU.add)
            P.act(qr[R64, t0:t0 + n], qt_[R64, 0:n], AF.Copy, scale=ATTN_SCALE)
        for bi, (t0, n) in enumerate(BLK):
            pk = PS[4 + bi % 2]
            for k in range(2):
                P.mm(pk[:, 0:n], wk[b][:, k, :], ckvn[:, k, t0:t0 + n], start=(k == 0), stop=(k == 1))
            P.copy("act", kn[:, t0:t0 + n], pk[:, 0:n])
        for kt in range(18):
            pk = PS[6 + kt % 2]
            for k in range(2):
                P.mm(pk[:, 0:128], ckvn[:, k, kt * 128:(kt + 1) * 128], wv[b][:, k, :], start=(k == 0), stop=(k == 1))
            P.copy("dve", vh[:, kt, :], pk[:, 0:128])

        def scores(qt):
            qs = slice(qt * 128, (qt + 1) * 128)
            c = qt % 2
            for bi, (t0, n) in enumerate(BLK):
                pk = PS[bi % 4]
                P.mm(pk[:, 0:n], qn[:, qs], kn[:, t0:t0 + n], start=True, stop=False)
                P.mm(pk[:, 0:n], qr[R64, qs], kpe[R64, t0:t0 + n], start=False, stop=True)
                P.ts("dve", S_sb[c][:, t0:t0 + n], pk[:, 0:n], 1.0, ALU.mult, None, ALU.max, accum_out=bmx[c][:, bi:bi + 1])

        def softmax(qt):
            c = qt % 2
            P.red("dve", mx[c], bmx[c][:, 0:5], ALU.max)
            P.ts("dve", mx[c], mx[c], -1.0, ALU.mult)
            P.act(Pb[c], S_sb[c], AF.Exp, bias=mx[c], accum_out=rsum[c])

        def attend_tile(qt, part):
            qs = slice(qt * 128, (qt + 1) * 128)
            c = qt % 2
            for g0 in range(0, 18, 8):
                if (part == 0) != (g0 < 16):
                    continue
                ng = min(8, 18 - g0)
                pk = PS[4 + (g0 // 8) % 2]
                pkb = TV(pk.ap.bitcast(BF16), pk.buf)
                for kk in range(ng):
                    kt = g0 + kk
                    P.tr(pkb[:, kk * 128:(kk + 1) * 128], Pb[c][:, kt * 128:(kt + 1) * 128], ident)
                P.op("act", lambda e, pkb=pkb, g0=g0, ng=ng, c=c: e.activation(
                    out=PT[c].ap[:, g0:g0 + ng, :], in_=pkb.ap[:, 0:ng * 128].rearrange("p (a b) -> p a b", a=ng), func=AF.Copy),
                    reads=[pkb], writes=[PT[c]])
            if part == 0:
                return
            po = PS[6]
            for kt in range(18):
                P.mm(po[:, 0:128], PT[c][:, kt, :], vh[:, kt, :], start=(kt == 0), stop=(kt == 17))
            P.op("dve", lambda e, r=rsum[c]: e.reciprocal(out=r.ap, in_=r.ap), reads=[rsum[c]], writes=[rsum[c]])
            P.ts("dve", onb[c], po[:, 0:128], rsum[c], ALU.mult)
            pt2 = PS[7]
            pt2b = TV(pt2.ap.bitcast(BF16), pt2.buf)
            P.tr(pt2b[:, 0:128], onb[c], ident)
            P.copy("act", attT[:, qs], pt2b[:, 0:128])

        scores(0)
        for qt in range(17):
            if qt >= 1:
                attend_tile(qt - 1, 0)
            if qt + 1 < 16:
                scores(qt + 1)
            if qt < 16:
                softmax(qt)
            if qt >= 1:
                attend_tile(qt - 1, 1)
        for m in range(8):
            for bi, (t0, n) in enumerate(LBLK):
                pk = PS[(m * 4 + bi) % 4]
                P.mm(pk[:, 0:n], wo[b][:, m * 128:(m + 1) * 128], attT[:, t0:t0 + n])
                P.stt("dve", xTc[m][:, t0:t0 + n], pk[:, 0:n], MOD[:, 1, 16 + m, 0:1], xTc[m][:, t0:t0 + n], ALU.mult, ALU.add)
    P.barrier()
    A.reset(m3)
    if stage == 3:
        return dump_and_finish()

    moe(1, 16)
    if stage == 4:
        return dump_and_finish()

    fgb = A.f32("fgb", [D])
    P.dma("sp", fgb, dram(v1_d[R_FG:R_FG + 1, :].to_broadcast([128, D]), "fgd"))
    xo = [A.f32(f"xo{k}", [D]) for k in range(2)]
    sqo = A.f32("sqo", [D]); ssum = A.f32("ssum", [1])
    for tt in range(16):
        tok = slice(tt * 128, (tt + 1) * 128)
        o = xo[tt % 2]
        for half in range(2):
            pk = PS[(2 * tt + half) % 4]
            for jj in range(4):
                P.mm(pk[:, jj * 128:(jj + 1) * 128], xTc[half * 4 + jj][:, tok], identf)
            P.copy("act", o[:, half * 512:(half + 1) * 512], pk)
        P.act(sqo, o, AF.Square, accum_out=ssum)
        P.ts("dve", ssum, ssum, 1.0 / D, ALU.mult, EPS, ALU.add)
        P.act(ssum, ssum, AF.Sqrt)
        P.op("dve", lambda e: e.reciprocal(out=ssum.ap, in_=ssum.ap), reads=[ssum], writes=[ssum])
        P.stt("dve", o, o, ssum, fgb, ALU.mult, ALU.mult)
        P.dma("sp", dram(out_d[tt * 128:(tt + 1) * 128, :], "outd"), o)
    P.barrier()
    P.emit()
    return nc, st, P


def _prep_inputs(inp, b, stage):
    f = np.float32
    v1 = np.zeros((NV1, D), f)
    v1[R_N1G:R_N1G + 2] = inp["norm1_g"]; v1[R_N2G:R_N2G + 2] = inp["norm2_g"]
    v1[R_PB] = inp["pool_b"][0]; v1[R_PS] = inp["pool_scale"][0]; v1[R_FG] = inp["final_g"]
    v1[R_C] = inp["c"][b]; v1[R_CC] = inp["c_ctx"]
    v1[R_ADAB:R_ADAB + 12] = inp["ada_b"].reshape(12, D)
    v1[R_QG, :512] = inp["q_norm_g"][0]; v1[R_KG, :256] = inp["kv_norm_g"][0]
    v1[R_BD:R_BD + 64] = inp["b_down"].reshape(64, D)
    v2 = np.ascontiguousarray(inp["b_gu"].reshape(128, D))
    m = {"x": np.ascontiguousarray(inp["x"][b]), "ctx": np.ascontiguousarray(inp["ctx"][b]), "v1": v1, "v2": v2,
         "ada_w": inp["ada_w"], "pool_w": np.ascontiguousarray(inp["pool_w"][0])}
    if stage >= 3:
        wuq = inp["w_uq"][0].reshape(512, 8, 192)
        perm = np.array([a * 32 + (1 - hf) * 16 + i for a in range(2) for hf in range(2) for i in range(16)])
        rope = wuq[:, :, 128:]
        wkv = inp["w_ukv"][0].reshape(256, 8, 256)
        dkv = inp["w_dkv"][0]
        m.update({"w_dq": np.ascontiguousarray(inp["w_dq"][0]),
                  "w_uq_n": np.ascontiguousarray(wuq[:, :, :128].reshape(512, 1024)),
                  "w_uq_r": np.ascontiguousarray(np.concatenate([rope, rope[:, :, perm]], axis=2).reshape(512, 1024)),
                  "w_dkv": np.ascontiguousarray(np.concatenate([dkv, dkv[:, 256:][:, perm]], axis=1)),
                  "w_uk": np.ascontiguousarray(wkv[:, :, :128].reshape(256, 1024)),
                  "w_uv": np.ascontiguousarray(wkv[:, :, 128:].reshape(256, 1024)),
                  "w_o": np.ascontiguousarray(inp["w_o"][0])})
    if stage >= 2:
        m.update({"router_w": inp["router_w"], "router_b": inp["router_b"], "w_gu": inp["w_gu"], "w_down": inp["w_down"],
                  "b_down": inp["b_down"]})
    return m


def kernel(**inputs):
    inp = {k: np.asarray(v) for k, v in inputs.items()}
    nc, st, P = build_program(STAGE)
    with st:
        in_maps = [_prep_inputs(inp, b, STAGE) for b in range(NCORES)]
        res = run_bass_kernel_spmd(nc, in_maps, core_ids=list(range(NCORES)))
    if STAGE < 99:
        return res
    return np.stack([r["out"] for r in res.results], axis=0)
```

```python
import numpy as np
from contextlib import ExitStack
import concourse.bass as bass
import concourse.mybir as mybir
from concourse.bass_utils import run_bass_kernel_spmd

F32 = mybir.dt.float32
BF16 = mybir.dt.bfloat16
I32 = mybir.dt.int32
U32 = mybir.dt.uint32
ALU = mybir.AluOpType
AF = mybir.ActivationFunctionType
AX = mybir.AxisListType

ENGINES = ("pe", "act", "dve", "pool", "sp")
EPOCH = 30000

D = 1024
L = 2048
C = 256
T = L + C
NE = 32
CAP = 1024
NSLOT = NE * CAP
EPS = 1e-6
ATTN_SCALE = 192 ** -0.5

STAGE = 99
NCORES = 8
DEBUG_TAGS = False
NO_ACT_DMA = False
CUT = 0
ZERO_YS = False


class SemGrp:
    __slots__ = ("name", "ndma", "sem", "bg")

    def __init__(self, name):
        self.name = name
        self.ndma = 0
        self.sem = None
        self.bg = False


class Buf:
    __slots__ = ("name", "last_w", "readers", "semgrp", "dram", "psum")

    def __init__(self, name, grp=None, dram=False, psum=False):
        self.name = name
        self.dram = dram
        self.psum = psum
        self.last_w = None
        self.readers = []
        self.semgrp = grp if grp is not None else SemGrp(name)


class TV:
    __slots__ = ("ap", "buf")

    def __init__(self, ap, buf):
        self.ap = ap
        self.buf = buf

    def __getitem__(self, k):
        return TV(self.ap[k], self.buf)

    def bitcast(self, dt):
        return TV(self.ap.bitcast(dt), self.buf)

    def rearrange(self, s, **kw):
        return TV(self.ap.rearrange(s, **kw), self.buf)

    def bc(self, shape):
        return TV(self.ap.to_broadcast(list(shape)), self.buf)

    def sub(self, name):
        return TV(self.ap, Buf(name))


class Op:
    __slots__ = ("eng", "fn", "deps", "is_dma", "grp", "signal", "epoch", "sigval", "tag", "cond", "dval")


def _ap(x):
    return x.ap if isinstance(x, TV) else x


class Prog:
    def __init__(self, nc, stack):
        self.nc = nc
        self.stack = stack
        self.ops = {e: [] for e in ENGINES}
        self.grps = {}
        self.n = 0
        self.cur_cond = None
        self.creg = {}
        self.nregion = 0

    def op(self, eng, fn, reads=(), writes=(), dma=None):
        o = Op()
        o.eng = eng
        o.fn = fn
        o.tag = None
        if DEBUG_TAGS:
            import sys as _s
            f = _s._getframe(1)
            while f.f_code.co_name in ("mm", "tr", "act", "tt", "ts", "stt", "copy", "memset", "red", "dma", "op"):
                f = f.f_back
            o.tag = f.f_lineno
        o.is_dma = dma is not None
        o.signal = False
        o.grp = None
        o.cond = self.cur_cond
        o.dval = 0
        deps = []
        seen = set()

        def add(d):
            if d is None or id(d) in seen:
                return
            seen.add(id(d))
            if d.is_dma:
                deps.append(("d", d.grp, 16 * d.grp.ndma))
            else:
                if d.eng == eng and eng == "pe" and not o.is_dma:
                    return
                deps.append(("c", d))

        rb = [r.buf if isinstance(r, TV) else r for r in reads if r is not None and not isinstance(r, (int, float))]
        wb = [w.buf if isinstance(w, TV) else w for w in writes]
        def latest(readers):
            last = {}
            for rd in readers:
                last[("d", id(rd.grp)) if rd.is_dma else ("c", rd.eng)] = rd
            return last.values()

        for r in rb:
            add(r.last_w)
            if r.psum:
                for rd in latest(r.readers):
                    if rd.eng != eng:
                        add(rd)
        for w in wb:
            add(w.last_w)
            for rd in latest(w.readers):
                add(rd)
        o.deps = deps
        if o.is_dma:
            g = (dma.buf if isinstance(dma, TV) else dma).semgrp
            o.dval = 16 * g.ndma
            g.ndma += 1
            o.grp = g
            self.grps[id(g)] = g
        for r in rb:
            r.readers.append(o)
        for w in wb:
            w.last_w = o
            w.readers = []
        self.ops[eng].append(o)
        self.n += 1
        return o

    def barrier(self, skip_bg=True):
        lasts = []
        for e in ENGINES:
            for o in reversed(self.ops[e]):
                if not o.is_dma and o.fn is not None:
                    lasts.append(o)
                    break
        dm = [("d", g, 16 * g.ndma) for g in self.grps.values() if g.ndma > 0 and not (skip_bg and g.bg)]
        for e in ENGINES:
            o = Op()
            o.eng = e
            o.fn = None
            o.is_dma = False
            o.signal = False
            o.grp = None
            o.cond = None
            o.dval = 0
            o.deps = [("c", x) for x in lasts if not (x.eng == e)] + list(dm)
            self.ops[e].append(o)

    def begin_region(self, flag, engines=("pe", "act", "dve", "sp")):
        assert self.cur_cond is None
        for e in engines:
            def ld(eng, e=e):
                if e not in self.creg:
                    self.creg[e] = eng.alloc_register(f"cflag_{e}")
                return eng.reg_load(self.creg[e], flag.ap)
            self.op(e, ld, reads=[flag])
        self.nregion += 1
        self.cur_cond = self.nregion

    def end_region(self):
        self.cur_cond = None

    def mm(self, out, lhsT, rhs, start=True, stop=True):
        return self.op("pe", lambda e: e.matmul(out.ap, lhsT=lhsT.ap, rhs=rhs.ap, start=start, stop=stop),
                       reads=[lhsT, rhs], writes=[out])

    def tr(self, out, in_, ident):
        return self.op("pe", lambda e: e.transpose(out=out.ap, in_=in_.ap, identity=ident.ap),
                       reads=[in_, ident], writes=[out])

    def act(self, out, in_, func, bias=0.0, scale=1.0, accum_out=None, eng="act"):
        kw = {}
        if accum_out is not None:
            kw["accum_out"] = accum_out.ap
        return self.op(eng, lambda e: e.activation(out=out.ap, in_=in_.ap, func=func, bias=_ap(bias), scale=_ap(scale), **kw),
                       reads=[in_, bias, scale], writes=[out] + ([accum_out] if accum_out is not None else []))

    def tt(self, eng, out, in0, in1, op):
        return self.op(eng, lambda e: e.tensor_tensor(out=out.ap, in0=in0.ap, in1=in1.ap, op=op),
                       reads=[in0, in1], writes=[out])

    def ts(self, eng, out, in0, s1, op0, s2=None, op1=None, accum_out=None):
        kw = {}
        if op1 is not None:
            kw["op1"] = op1
        if accum_out is not None:
            kw["accum_out"] = accum_out.ap
        return self.op(eng, lambda e: e.tensor_scalar(out=out.ap, in0=in0.ap, scalar1=_ap(s1), scalar2=_ap(s2), op0=op0, **kw),
                       reads=[in0, s1, s2], writes=[out] + ([accum_out] if accum_out is not None else []))

    def stt(self, eng, out, in0, scalar, in1, op0, op1):
        return self.op(eng, lambda e: e.scalar_tensor_tensor(out=out.ap, in0=in0.ap, scalar=_ap(scalar), in1=in1.ap, op0=op0, op1=op1),
                       reads=[in0, scalar, in1], writes=[out])

    def copy(self, eng, out, in_):
        if eng == "act":
            return self.op(eng, lambda e: e.activation(out=out.ap, in_=in_.ap, func=AF.Copy), reads=[in_], writes=[out])
        return self.op(eng, lambda e: e.tensor_copy(out=out.ap, in_=in_.ap), reads=[in_], writes=[out])

    def memset(self, eng, out, val):
        return self.op(eng, lambda e: e.memset(out.ap, val), writes=[out])

    def red(self, eng, out, in_, op, axis=AX.X):
        return self.op(eng, lambda e: e.tensor_reduce(out=out.ap, in_=in_.ap, axis=axis, op=op), reads=[in_], writes=[out])

    def dma(self, q, out, in_, owner=None, **kw):
        if q == "act" and NO_ACT_DMA:
            q = "sp"
        if owner is None:
            owner = in_ if out.buf.dram else out
        return self.op(q, lambda e: e.dma_start(out=out.ap, in_=in_.ap, **kw), reads=[in_], writes=[out], dma=owner)

    def emit(self):
        nc = self.nc
        for e in ENGINES:
            for o in self.ops[e]:
                for d in o.deps:
                    if d[0] == "c":
                        d[1].signal = True
        self.esems = {}
        for e in ENGINES:
            cnt = 0
            for o in self.ops[e]:
                if not o.is_dma and o.signal:
                    o.epoch = cnt // EPOCH
                    o.sigval = cnt % EPOCH + 1
                    cnt += 1
            nep = max(1, (cnt + EPOCH - 1) // EPOCH)
            self.esems[e] = [self.stack.enter_context(nc.semaphore(f"s_{e}{k}")) for k in range(nep)]
        for g in self.grps.values():
            g.sem = self.stack.enter_context(nc.semaphore(f"d_{g.name}"))
        self.nsem = sum(len(v) for v in self.esems.values()) + len(self.grps)
        block = self.stack.enter_context(nc.Block())
        reg = {"pe": block.tensor, "act": block.scalar, "dve": block.vector, "pool": block.gpsimd, "sp": block.sync}
        nwaits = {e: 0 for e in ENGINES}

        def make(e):
            def body(eng):
                def do_waits(o, waited):
                    for d in o.deps:
                        if d[0] == "d":
                            sem, val, key = d[1].sem, d[2], ("d", id(d[1]))
                        else:
                            x = d[1]
                            sem, val, key = self.esems[x.eng][x.epoch], x.sigval, ("c", x.eng, x.epoch)
                        if waited.get(key, 0) >= val:
                            continue
                        waited[key] = val
                        eng.wait_ge(sem, val)
                        nwaits[e] += 1

                def real(o, waited):
                    do_waits(o, waited)
                    if o.fn is None:
                        assert not o.signal
                        return
                    ins = o.fn(eng)
                    if o.is_dma:
                        ins.then_inc(o.grp.sem, 16)
                    elif o.signal:
                        ins.then_inc(self.esems[e][o.epoch], 1)

                pend = {}

                def flush():
                    for sem, n in pend.values():
                        eng.sem_inc(sem, n)
                    pend.clear()

                def ghost(o, waited, first):
                    if o.is_dma:
                        if o.dval > 0:
                            flush()
                            eng.wait_ge(o.grp.sem, o.dval)
                        k_ = id(o.grp.sem)
                        pend[k_] = (o.grp.sem, pend.get(k_, (None, 0))[1] + 16)
                        flush()
                    elif o.signal:
                        sm = self.esems[e][o.epoch]
                        k_ = id(sm)
                        pend[k_] = (sm, pend.get(k_, (None, 0))[1] + 1)

                waited = {}
                ops = self.ops[e]
                i = 0
                while i < len(ops):
                    o = ops[i]
                    if o.cond is None:
                        real(o, waited)
                        i += 1
                        continue
                    j = i
                    while j < len(ops) and ops[j].cond == o.cond:
                        j += 1
                    w1 = dict(waited)
                    with eng.If_ne(self.creg[e], 0):
                        for k in range(i, j):
                            real(ops[k], w1)
                    w2 = dict(waited)
                    with eng.Else():
                        eng.drain()
                        for k in range(i, j):
                            ghost(ops[k], w2, k == i)
                        flush()
                    i = j
            return body

        for e in ENGINES:
            if self.ops[e]:
                reg[e](make(e))
        self.nwaits = nwaits


def _shape(ap, shape):
    if len(shape) <= 1:
        return ap
    names = "abcdefg"[:len(shape)]
    kw = {names[i]: int(shape[i]) for i in range(len(shape) - 1)}
    return ap.rearrange(f"p ({' '.join(names)}) -> p {' '.join(names)}", **kw)


class Arena:
    def __init__(self, P, cols):
        self.P = P
        self.t = P.stack.enter_context(P.nc.sbuf_tensor("arena", [128, cols], F32))
        self.cols = cols
        self.top = 0
        self.n = 0

    def mark(self):
        return self.top

    def reset(self, m):
        self.top = m

    def f32(self, name, shape):
        n = int(np.prod(shape))
        off = self.top
        self.top += n
        assert self.top <= self.cols, f"arena overflow {name}: {self.top} > {self.cols}"
        ap = _shape(self.t[:, off:off + n], shape)
        self.n += 1
        return TV(ap, Buf(f"{name}{self.n}"))

    def b16(self, name, shape, dt=BF16):
        n = int(np.prod(shape))
        nf = (n + 1) // 2
        off = self.top
        self.top += nf
        assert self.top <= self.cols, f"arena overflow {name}: {self.top} > {self.cols}"
        ap = _shape(self.t[:, off:off + nf].bitcast(dt)[:, 0:n], shape)
        self.n += 1
        return TV(ap, Buf(f"{name}{self.n}"))

    def i32(self, name, shape):
        tv = self.f32(name, shape)
        return TV(tv.ap.bitcast(I32), tv.buf)


R_N1G, R_N2G, R_PB, R_PS, R_FG, R_C, R_CC, R_ADAB, R_QG, R_KG, R_BD = 0, 2, 4, 5, 6, 7, 8, 9, 21, 22, 32
NV1 = 96


def build_program(stage=99):
    nc = bass.Bass("TRN2", target_bir_lowering=False)
    dr = {}

    def din(name, shape, dt=F32):
        dr[name] = nc.dram_tensor(name, list(shape), dt, kind="ExternalInput").ap()
        return dr[name]

    x_d = din("x", [L, D]); ctx_d = din("ctx", [C, D])
    v1_d = din("v1", [NV1, D]); v2_d = din("v2", [128, D])
    adaw_d = din("ada_w", [2, D, 6 * D])
    poolw_d = din("pool_w", [4, 256, 256])
    if stage >= 2:
        rw_d = din("router_w", [2, D, NE]); rb_d = din("router_b", [2, NE])
        wgu_d = din("w_gu", [2, NE, D, 2 * D]); wdn_d = din("w_down", [2, NE, D, D]); bdn_d = din("b_down", [2, NE, D])
    if stage >= 3:
        wdq_d = din("w_dq", [D, 512]); wuqn_d = din("w_uq_n", [512, 1024]); wuqr_d = din("w_uq_r", [512, 1024])
        wdkv_d = din("w_dkv", [D, 384]); wuk_d = din("w_uk", [256, 1024]); wuv_d = din("w_uv", [256, 1024])
        wo_d = din("w_o", [D, D])
    out_d = nc.dram_tensor("out", [L, D], F32, kind="ExternalOutput").ap()
    dbg_d = nc.dram_tensor("dbg", [128, 8, T], F32, kind="ExternalOutput").ap() if stage < 99 else None
    if stage >= 2:
        xs_d = nc.dram_tensor("xs_scr", [NSLOT + 1, D], BF16, kind="Internal").ap()
        ys_d = nc.dram_tensor("ys_scr", [NSLOT + 1, D], F32, kind="Internal").ap()

    st = ExitStack()
    P = Prog(nc, st)
    A = Arena(P, 52736)
    dram = lambda ap, name: TV(ap, Buf(name, dram=True))

    PS = []
    for k in range(8):
        t = st.enter_context(nc.psum_tensor(f"ps{k}", [128, 512], F32))
        PS.append(TV(t[:, :], Buf(f"ps{k}", psum=True)))

    def ps_b16(k):
        return TV(PS[k].ap.bitcast(BF16), PS[k].buf)

    xT = A.f32("xT", [8, T])
    xTc = [TV(xT.ap[:, j, :], Buf(f"xT{j}")) for j in range(8)]
    VT1 = A.f32("VT1", [8, NV1])
    VT2 = A.f32("VT2", [8, 128])
    MOD = A.f32("MOD", [2, 48, 2])
    DER = A.f32("DER", [2, 6, 8, 2])
    identf = A.f32("identf", [128])
    onesf = A.f32("onesf", [128])
    ident = A.b16("ident", [128])
    onesb = A.b16("onesb", [128])
    tri = A.b16("tri", [128])
    zero1 = A.f32("zero1", [1])
    pmark = A.mark()

    P.memset("pool", identf, 0.0)
    P.op("pool", lambda e: e.affine_select(out=identf.ap, in_=identf.ap, pattern=[[-1, 128]], compare_op=ALU.not_equal,
                                           fill=1.0, base=0, channel_multiplier=1), reads=[identf], writes=[identf])
    P.copy("dve", ident, identf)
    P.memset("pool", onesf, 1.0)
    P.memset("pool", onesb, 1.0)
    P.memset("pool", zero1, 0.0)
    P.memset("pool", tri, 1.0)
    P.op("pool", lambda e: e.affine_select(out=tri.ap, in_=tri.ap, pattern=[[1, 128]], compare_op=ALU.is_gt,
                                           fill=0.0, base=0, channel_multiplier=-1), reads=[tri], writes=[tri])

    m0 = A.mark()
    v1s = A.f32("v1s", [D]); v2s = A.f32("v2s", [D])
    P.dma("sp", v1s[0:NV1], dram(v1_d, "v1d"))
    P.dma("sp", v2s, dram(v2_d, "v2d"))
    for (src, dst, nr) in ((v1s, VT1, NV1), (v2s, VT2, 128)):
        for j in range(8):
            pk = PS[j % 2]
            P.mm(pk[:, 0:nr], src[0:nr, j * 128:(j + 1) * 128], identf[0:nr, 0:nr])
            P.copy("dve", dst[:, j, :], pk[:, 0:nr])
    def cut():
        P.dma("sp", dram(dbg_d[:, 0, :], "dbgd"), xTc[0])
        P.barrier()
        P.emit()
        return nc, st, P
    if CUT == 1:
        return cut()
    scv = A.f32("scv", [8, 2])
    P.act(scv[:, :, 0], VT1[:, :, R_C], AF.Silu)
    P.act(scv[:, :, 1], VT1[:, :, R_CC], AF.Silu)
    aw = [A.f32(f"aw{k}", [8, 768]) for k in range(2)]
    q = 0
    for i in range(2):
        for cc in range(8):
            w = aw[q % 2]
            P.dma("sp" if q % 2 == 0 else "act", w, dram(adaw_d[i, :, cc * 768:(cc + 1) * 768].rearrange("(kc p) n -> p kc n", p=128), "adawd"))
            for mloc in range(6):
                mch = cc * 6 + mloc
                pk = PS[2 + (mch % 2)]
                for k in range(8):
                    P.mm(pk[:, 0:2], w[:, k, mloc * 128:(mloc + 1) * 128], scv[:, k, :], start=(k == 0), stop=(k == 7))
                P.ts("dve", MOD[:, i, mch, :], pk[:, 0:2], VT1[:, mch % 8, R_ADAB + i * 6 + mch // 8: R_ADAB + i * 6 + mch // 8 + 1], ALU.add)
            q += 1
    if CUT == 2:
        return cut()
    for i in range(2):
        for s in range(2):
            P.stt("dve", DER[:, i, 0, :, s], MOD[:, i, 8:16, s], 1.0, VT1[:, :, R_N1G + i], ALU.add, ALU.mult)
            P.stt("dve", DER[:, i, 1, :, s], MOD[:, i, 32:40, s], 1.0, VT1[:, :, R_N2G + i], ALU.add, ALU.mult)
        if i == 0:
            for s in range(2):
                P.tt("dve", DER[:, 0, 2, :, s], MOD[:, 0, 16:24, s], VT1[:, :, R_PS], ALU.mult)
                P.tt("dve", DER[:, 0, 3, :, s], DER[:, 0, 2, :, s], VT1[:, :, R_PB], ALU.mult)

    if CUT == 3:
        return cut()
    xin = [A.f32(f"xin{k}", [D]) for k in range(3)]
    for tt in range(18):
        xi = xin[tt % 3]
        src = x_d[tt * 128:(tt + 1) * 128, :] if tt < 16 else ctx_d[(tt - 16) * 128:(tt - 15) * 128, :]
        P.dma("sp" if tt % 2 == 0 else "act", xi, dram(src, "xd"))
        for half in range(2):
            pk = PS[4 + (2 * tt + half) % 4]
            for jj in range(4):
                j = half * 4 + jj
                P.mm(pk[:, jj * 128:(jj + 1) * 128], xi[:, j * 128:(j + 1) * 128], identf)
            dst = TV(xT.ap[:, half * 4:(half + 1) * 4, tt * 128:(tt + 1) * 128], None)
            srcv = pk.rearrange("p (a b) -> p a b", a=4)
            if half == 0:
                P.op("dve", lambda e, d=dst, s_=srcv: e.tensor_copy(out=d.ap, in_=s_.ap), reads=[pk], writes=xTc[0:4])
            else:
                P.op("act", lambda e, d=dst, s_=srcv: e.activation(out=d.ap, in_=s_.ap, func=AF.Copy), reads=[pk], writes=xTc[4:8])
    if stage >= 2:
        ZOFF = 51600
        zt = TV(A.t[:, ZOFF:ZOFF + 1024].bitcast(BF16), Buf("zt"))
        zt.buf.semgrp.bg = True
        xs_t = TV(xs_d, Buf("xs_scr", dram=True))
        ys_t = TV(ys_d, Buf("ys_scr", dram=True))
        P.memset("pool", zt, 0.0)
        for r in range(0, NSLOT, 256):
            P.dma("sp", TV(xs_d[r:r + 256, :].rearrange("(p a) n -> p (a n)", p=128), xs_t.buf), zt, owner=zt)
        ztf = TV(zt.ap.bitcast(F32), zt.buf)
        if ZERO_YS:
            for r in range(0, NSLOT, 128):
                P.dma("sp", TV(ys_d[r:r + 128, :], ys_t.buf), ztf, owner=zt)
        P.dma("sp", TV(xs_d[NSLOT:NSLOT + 1, :], xs_t.buf), zt[0:1, 0:D], owner=zt)
        P.dma("sp", TV(ys_d[NSLOT:NSLOT + 1, :], ys_t.buf), ztf[0:1, 0:D], owner=zt)

    P.barrier()
    A.reset(m0)

    def dump_and_finish():
        for j in range(8):
            P.dma("sp", dram(dbg_d[:, j, :], "dbgd"), xTc[j])
        P.barrier()
        P.emit()
        return nc, st, P

    if stage == 0:
        return dump_and_finish()

    BLK = [(0, 512), (512, 512), (1024, 512), (1536, 512), (2048, 256)]

    def rstd_all(rstd, ntok_blocks, src_chunks, nfeat, sq_pool, pss):
        nch = len(src_chunks)
        for bi, (t0, n) in enumerate(ntok_blocks):
            pk = pss[bi % len(pss)]
            for j in range(nch):
                sq = sq_pool[(bi * nch + j) % len(sq_pool)]
                P.act(sq[:, 0:n], src_chunks[j][:, t0:t0 + n], AF.Square)
                P.mm(pk[:, 0:n], onesf, sq[:, 0:n], start=(j == 0), stop=(j == nch - 1))
            P.ts("dve", rstd[:, t0:t0 + n], pk[:, 0:n], 1.0 / nfeat, ALU.mult, EPS, ALU.add)
            P.act(rstd[:, t0:t0 + n], rstd[:, t0:t0 + n], AF.Sqrt)
            P.op("dve", lambda e, o=rstd[:, t0:t0 + n]: e.reciprocal(out=o.ap, in_=o.ap), reads=[rstd], writes=[rstd])

    m1 = A.mark()
    rstd = A.f32("rstd", [T])
    sqp = [A.f32(f"sq{k}", [512]) for k in range(3)]
    rstd_all(rstd, BLK, xTc, D, sqp, [PS[0], PS[1]])
    WIN = (2, 4, 8, 16)
    PADW = 16
    segs = ((0, L, 0), (L, C, 1))
    hp = [A.f32(f"hp{s}", [ln + 2 * PADW]) for (_, ln, s) in segs]
    sa = [A.f32(f"sa{s}", [ln + 2 * PADW]) for (_, ln, s) in segs]
    sb = [A.f32(f"sb{s}", [ln + 2 * PADW]) for (_, ln, s) in segs]
    icnt = [[A.f32(f"ic{s}_{g}", [ln]) for g in range(4)] for (_, ln, s) in segs]
    dT = A.b16("dT", [2, T])
    uu = A.f32("uu", [T])
    pw = A.b16("pw", [4, 2, 256])
    P.dma("pool", pw, dram(poolw_d.rearrange("g (kc p) n -> p g kc n", p=128), "pwd"))

    def window_sum(si, w, src, dst_final):
        ln = segs[si][1]
        W = ln + 2 * PADW
        cur = src
        bufs = [sa[si], sb[si]]
        step = 1
        k = 0
        ww = 2
        while ww <= w:
            dst = bufs[k % 2]
            if ww == 2:
                P.tt("dve", dst[:, 1:W], cur[:, 0:W - 1], cur[:, 1:W], ALU.add)
            else:
                sh = ww // 4
                P.tt("dve", dst[:, sh:W - sh], cur[:, 0:W - 2 * sh], cur[:, 2 * sh:W], ALU.add)
            cur = dst
            k += 1
            ww *= 2
        return cur

    for si, (t0, ln, s) in enumerate(segs):
        for bufz in (hp[si], sa[si], sb[si]):
            P.memset("pool", bufz, 0.0)
    for si, (t0, ln, s) in enumerate(segs):
        P.memset("pool", hp[si][:, PADW:PADW + ln], 1.0)
        for g in range(4):
            cur = window_sum(si, WIN[g], hp[si], None)
            P.op("dve", lambda e, o=icnt[si][g], c=cur, ln=ln: e.reciprocal(out=o.ap, in_=c.ap[:, PADW:PADW + ln]),
                 reads=[cur], writes=[icnt[si][g]])
    for g in range(4):
        for kc in range(2):
            j = 2 * g + kc
            P.tt("dve", uu, xTc[j], rstd, ALU.mult)
            for si, (t0, ln, s) in enumerate(segs):
                P.act(hp[si][:, PADW:PADW + ln], uu[:, t0:t0 + ln], AF.Identity,
                      bias=MOD[:, 0, 0 + j, s:s + 1], scale=DER[:, 0, 0, j, s:s + 1])
                cur = window_sum(si, WIN[g], hp[si], None)
                P.tt("pool", cur[:, PADW:PADW + ln], cur[:, PADW:PADW + ln], icnt[si][g], ALU.mult)
                P.tt("pool", dT[:, kc, t0:t0 + ln], cur[:, PADW:PADW + ln], hp[si][:, PADW:PADW + ln], ALU.subtract)
        for mc in range(2):
            j = 2 * g + mc
            for bi, (t0, n) in enumerate(BLK):
                s = 0 if t0 < L else 1
                pk = PS[2 + (bi % 4)]
                for kc in range(2):
                    P.mm(pk[:, 0:n], pw[:, g, kc, mc * 128:(mc + 1) * 128], dT[:, kc, t0:t0 + n], start=(kc == 0), stop=(kc == 1))
                P.stt("dve", xTc[j][:, t0:t0 + n], pk[:, 0:n], DER[:, 0, 2, j, s:s + 1], xTc[j][:, t0:t0 + n], ALU.mult, ALU.add)
                P.ts("dve", xTc[j][:, t0:t0 + n], xTc[j][:, t0:t0 + n], DER[:, 0, 3, j, s:s + 1], ALU.add)
    P.barrier()
    A.reset(m1)

    if stage == 1:
        return dump_and_finish()

    REG = {}

    def mkregs(e):
        for nm, val in (("sc", NSLOT - 1), ("ga", NSLOT)):
            r = e.alloc_register(f"bc_{nm}")
            e.reg_mov(r, val)
            REG[nm] = r
        return e.nop()
    P.op("pool", mkregs)
    def moe(layer, ntt):
        ntok = ntt * 128
        blks = [b for b in BLK if b[0] < ntok]
        mm0 = A.mark()
        slot_i = A.i32("slot_i", [18, 4])
        gates = A.f32("gates", [18, 4])
        G = A.f32("G", [18, NE])
        masks = A.b16("masks", [18, NE])
        ebase = A.f32("ebase", [NE])
        ebi = A.i32("ebi", [NE])
        rw = A.f32("rw", [8, NE])
        rb = A.f32("rb", [NE])
        bdT = A.f32("bdT", [D])
        flags_f = A.f32("flags_f", [4, NE]); flags_i = A.i32("flags_i", [4, NE])
        P.op("pool", lambda e: e.iota(ebi.ap, pattern=[[CAP, NE]], base=0, channel_multiplier=0), writes=[ebi])
        P.copy("dve", ebase, ebi)
        P.dma("sp", rw, dram(rw_d[layer].rearrange("(kc p) n -> p kc n", p=128), "rwd"))
        P.dma("sp", rb[0:1, :], dram(rb_d[layer:layer + 1, :], "rbd"))
        P.dma("sp", bdT[0:NE, :], dram(bdn_d[layer], "bdd"))
        ma = A.mark()
        rstd2 = A.f32("rstd2", [T])
        sq2 = [A.f32(f"sq2{k}", [512]) for k in range(3)]
        rstd_all(rstd2, blks, xTc, D, sq2, [PS[0], PS[1]])
        u2 = [A.f32(f"u2{k}", [128]) for k in range(2)]
        h2f = [A.f32(f"h2f{k}", [8, 128]) for k in range(2)]
        h2b = [A.b16(f"h2b{k}", [8, 128]) for k in range(2)]
        h2tok = [A.b16(f"h2tok{k}", [D]) for k in range(2)]
        lg = A.f32("lg", [NE]); top8 = A.f32("top8", [8]); maskf = A.f32("maskf", [NE])
        pos = A.f32("pos", [NE]); t1 = A.f32("t1", [NE]); oh = A.f32("oh", [NE]); sel = A.f32("sel", [NE])
        slotsf = A.f32("slotsf", [4]); negm = A.f32("negm", [1]); ex = A.f32("ex", [4]); sumex = A.f32("sumex", [1])
        for tt in range(ntt):
            s = 0 if tt < 16 else 1
            tok = slice(tt * 128, (tt + 1) * 128)
            hf = h2f[tt % 2]; hb = h2b[tt % 2]
            for j in range(8):
                u = u2[j % 2]
                P.tt("dve", u, xTc[j][:, tok], rstd2[:, tok], ALU.mult)
                P.act(hf[:, j, :], u, AF.Identity, bias=MOD[:, layer, 24 + j, s:s + 1], scale=DER[:, layer, 1, j, s:s + 1])
                P.act(hb[:, j, :], u, AF.Identity, bias=MOD[:, layer, 24 + j, s:s + 1], scale=DER[:, layer, 1, j, s:s + 1])
            pl = PS[2 + tt % 2]
            for j in range(8):
                P.mm(pl[:, 0:NE], hf[:, j, :], rw[:, j, :], start=(j == 0), stop=False)
            P.mm(pl[:, 0:NE], onesf[0:1, :], rb[0:1, :], start=False, stop=True)
            P.copy("dve", lg, pl[:, 0:NE])
            P.op("dve", lambda e: e.max(out=top8.ap, in_=lg.ap), reads=[lg], writes=[top8])
            P.ts("dve", maskf, lg, top8[:, 3:4], ALU.is_ge)
            P.copy("dve", masks[:, tt, :], maskf)
            pp = PS[4 + tt % 2]
            P.mm(pp[:, 0:NE], tri, masks[:, tt, :], start=True, stop=(tt == 0))
            for i in range(tt):
                P.mm(pp[:, 0:NE], onesb, masks[:, i, :], start=False, stop=(i == tt - 1))
            P.copy("dve", pos, pp[:, 0:NE])
            P.ts("dve", t1, pos, float(CAP), ALU.is_ge, 1.0e6, ALU.mult)
            P.tt("dve", pos, pos, ebase, ALU.add)
            P.tt("dve", pos, pos, t1, ALU.add)
            P.ts("dve", pos, pos, float(NSLOT), ALU.min)
            P.ts("dve", negm, top8[:, 0:1], -1.0, ALU.mult)
            P.act(ex, top8[:, 0:4], AF.Exp, bias=negm, accum_out=sumex)
            P.op("dve", lambda e: e.reciprocal(out=sumex.ap, in_=sumex.ap), reads=[sumex], writes=[sumex])
            P.ts("dve", gates[:, tt, :], ex, sumex, ALU.mult)
            for k in range(4):
                P.ts("dve", oh, lg, top8[:, k:k + 1], ALU.is_equal)
                P.tt("dve", sel, oh, pos, ALU.mult)
                P.red("dve", slotsf[:, k:k + 1], sel, ALU.add)
                if k == 0:
                    P.ts("dve", G[:, tt, :], oh, gates[:, tt, 0:1], ALU.mult)
                else:
                    P.stt("dve", G[:, tt, :], oh, gates[:, tt, k:k + 1], G[:, tt, :], ALU.mult, ALU.add)
            P.copy("dve", slot_i[:, tt, :], slotsf)
            pt_ = PS[6 + tt % 2]
            ptb = TV(pt_.ap.bitcast(BF16), pt_.buf)
            for j in range(8):
                P.tr(ptb[:, j * 128:(j + 1) * 128], hb[:, j, :], ident)
            ht = h2tok[tt % 2]
            P.copy("act", ht, ptb)
            for k in range(4):
                P.op("pool", lambda e, ht=ht, tt=tt, k=k: e.indirect_dma_start(
                    out=xs_d, out_offset=bass.IndirectOffsetOnAxis(ap=slot_i.ap[:, tt, k:k + 1], axis=0),
                    in_=ht.ap, in_offset=None, bounds_check=REG["sc"], oob_is_err=False),
                    reads=[ht, slot_i], writes=[xs_t], dma=ht)
        pc = PS[0]
        for tt in range(ntt):
            P.mm(pc[:, 0:NE], onesb, masks[:, tt, :], start=(tt == 0), stop=(tt == ntt - 1))
        for q in range(4):
            P.ts("dve", flags_f[:, q, :], pc[:, 0:NE], float(q * 256), ALU.is_gt)
        P.copy("dve", flags_i, flags_f)
        P.barrier()
        A.reset(ma)
        BW = 256
        NQ = CAP // BW
        NRING = 9
        ring = [A.b16(f"ring{k}", [8, 512]) for k in range(NRING)]
        XeT = [A.b16(f"XeT{k}", [8, BW]) for k in range(2)]
        actT = [A.b16(f"actT{k}", [8, BW]) for k in range(2)]
        xrow = [A.b16(f"xrow{k}", [D]) for k in range(2)]
        yout = [A.f32(f"yout{k}", [D]) for k in range(2)]
        gsb = [A.f32(f"gsb{k}", [BW]) for k in range(2)]
        sgb = [A.f32(f"sgb{k}", [BW]) for k in range(2)]
        usb = [A.f32(f"usb{k}", [BW]) for k in range(2)]
        GORD = (0, 2, 1, 3)
        nq = 0
        cnt_it = [0, 0, 0]
        for e in range(NE):
            wg = {}
            for g in GORD:
                r = ring[nq % NRING]; nq += 1
                P.dma("pool", r, dram(wgu_d[layer, e, :, g * 512:(g + 1) * 512].rearrange("(kc p) n -> p kc n", p=128), "wgud"))
                wg[g] = r
            wd = {}
            for g in range(2):
                r = ring[nq % NRING]; nq += 1
                P.dma("pool", r, dram(wdn_d[layer, e, :, g * 512:(g + 1) * 512].rearrange("(kc p) n -> p kc n", p=128), "wdnd"))
                wd[g] = r
            row = (layer * NE + e) * 2

            def regionT(q, ee=e):
                P.begin_region(flags_i[0:1, q, ee:ee + 1], engines=("pe", "act", "sp"))
                xe = XeT[q % 2]
                for t in range(BW // 128):
                    stl = q * (BW // 128) + t
                    xr = xrow[cnt_it[0] % 2]
                    pk = PS[6 + cnt_it[0] % 2]
                    cnt_it[0] += 1
                    P.dma("sp", xr, TV(xs_d[ee * CAP + stl * 128: ee * CAP + (stl + 1) * 128, :], xs_t.buf), owner=xr)
                    pkb = TV(pk.ap.bitcast(BF16), pk.buf)
                    for j in range(8):
                        P.tr(pkb[:, j * 128:(j + 1) * 128], xr[:, j * 128:(j + 1) * 128], ident)
                    P.op("act", lambda e_, pkb=pkb, t=t, xe=xe: e_.activation(
                        out=xe.ap[:, :, t * 128:(t + 1) * 128], in_=pkb.ap.rearrange("p (a b) -> p a b", a=8), func=AF.Copy),
                        reads=[pkb], writes=[xe])
                P.end_region()

            def regionG(q):
                P.begin_region(flags_i[0:1, q, e:e + 1], engines=("pe", "act", "dve"))
                xe = XeT[q % 2]
                for j in range(8):
                    it = cnt_it[1]; cnt_it[1] += 1
                    pg = PS[(2 * it) % 4]; pu = PS[(2 * it + 1) % 4]
                    cg = (j % 4) * 128
                    for k in range(8):
                        P.mm(pg[:, 0:BW], wg[j // 4][:, k, cg:cg + 128], xe[:, k, :], start=(k == 0), stop=(k == 7))
                    for k in range(8):
                        P.mm(pu[:, 0:BW], wg[2 + j // 4][:, k, cg:cg + 128], xe[:, k, :], start=(k == 0), stop=(k == 7))
                    gs = gsb[it % 2]; sg = sgb[it % 2]; us = usb[it % 2]
                    P.ts("dve", gs, pg[:, 0:BW], VT2[:, j, row:row + 1], ALU.add, 7.0, ALU.min)
                    P.act(sg, gs, AF.Sigmoid, scale=1.702)
                    P.ts("dve", us, pu[:, 0:BW], VT2[:, j, row + 1:row + 2], ALU.add, 7.0, ALU.min)
                    P.ts("dve", us, us, -7.0, ALU.max, 1.0, ALU.add)
                    P.tt("dve", gs, gs, sg, ALU.mult)
                    P.tt("dve", actT[q % 2][:, j, :], gs, us, ALU.mult)
                P.end_region()

            def regionB(q):
                P.begin_region(flags_i[0:1, q, e:e + 1], engines=("pe", "act"))
                for t in range(BW // 128):
                    stl = q * (BW // 128) + t
                    yo = yout[cnt_it[2] % 2]
                    for n2 in range(2):
                        pd = PS[4 + (2 * cnt_it[2] + n2) % 2]
                        for k in range(8):
                            P.mm(pd, actT[q % 2][:, k, t * 128:(t + 1) * 128], wd[n2][:, k, :], start=(k == 0), stop=(k == 7))
                        P.copy("act", yo[:, n2 * 512:(n2 + 1) * 512], pd)
                    cnt_it[2] += 1
                    P.dma("act", TV(ys_d[e * CAP + stl * 128: e * CAP + (stl + 1) * 128, :], ys_t.buf), yo, owner=yo)
                P.end_region()

            if e == 0:
                regionT(0)
            for q in range(NQ):
                if q + 1 < NQ:
                    regionT(q + 1)
                regionG(q)
                if q >= 1:
                    regionB(q - 1)
            if e + 1 < NE:
                regionT(0, e + 1)
            regionB(NQ - 1)
        P.barrier()
        A.reset(ma)
        yk = [A.f32(f"yk{k}", [D]) for k in range(4)]
        acc = [A.f32(f"acc{k}", [D]) for k in range(2)]
        GT = [A.f32(f"GT{k}", [128]) for k in range(2)]
        for tt in range(ntt):
            s = 0 if tt < 16 else 1
            tok = slice(tt * 128, (tt + 1) * 128)
            ac = acc[tt % 2]
            for k in range(4):
                P.op("pool", lambda e, tt=tt, k=k: e.indirect_dma_start(
                    out=yk[k].ap, out_offset=None, in_=ys_d,
                    in_offset=bass.IndirectOffsetOnAxis(ap=slot_i.ap[:, tt, k:k + 1], axis=0),
                    bounds_check=REG["ga"], oob_is_err=False),
                    reads=[ys_t, slot_i], writes=[yk[k]], dma=yk[k])
                if k == 0:
                    P.ts("dve", ac, yk[0], gates[:, tt, 0:1], ALU.mult)
                else:
                    P.stt("dve", ac, yk[k], gates[:, tt, k:k + 1], ac, ALU.mult, ALU.add)
            pgt = PS[tt % 2]
            P.mm(pgt[0:NE, 0:128], G[:, tt, :], identf)
            gt = GT[tt % 2]
            P.copy("act", gt[0:NE, :], pgt[0:NE, 0:128])
            for j in range(8):
                pj = PS[2 + j % 4]
                P.mm(pj[:, 0:128], ac[:, j * 128:(j + 1) * 128], identf, start=True, stop=False)
                P.mm(pj[:, 0:128], bdT[0:NE, j * 128:(j + 1) * 128], gt[0:NE, :], start=False, stop=True)
                P.stt("dve", xTc[j][:, tok], pj[:, 0:128], MOD[:, layer, 40 + j, s:s + 1], xTc[j][:, tok], ALU.mult, ALU.add)
        P.barrier()
        A.reset(mm0)

    P.barrier(skip_bg=False)

    moe(0, 18)
    if stage == 2:
        return dump_and_finish()

    LBLK = BLK[0:4]
    m3 = A.mark()
    cqn = A.b16("cqn", [4, L])
    ckvn = A.b16("ckvn", [2, T])
    kpe = A.b16("kpe", [T])
    COS = A.f32("COS", [L]); SINS = A.f32("SINS", [L])
    m3t = A.mark()
    ti = A.i32("ti", [L]); pi_ = A.i32("pi", [1]); pf = A.f32("pf", [1]); invf = A.f32("invf", [1]); sgn = A.f32("sgn", [1])
    ang = A.f32("ang", [L]); nn = A.i32("nn", [L]); nf = A.f32("nf", [L]); fx = A.f32("fx", [L])
    R64 = slice(0, 64)
    P.op("pool", lambda e: e.iota(ti.ap[R64], pattern=[[1, L]], base=0, channel_multiplier=0), writes=[ti])
    P.op("pool", lambda e: e.iota(pi_.ap[R64], pattern=[[0, 1]], base=0, channel_multiplier=1), writes=[pi_])
    P.op("dve", lambda e: e.tensor_single_scalar(out=ti.ap[0:32], in_=ti.ap[0:32], scalar=6, op=ALU.arith_shift_right), reads=[ti], writes=[ti])
    P.op("dve", lambda e: e.tensor_single_scalar(out=ti.ap[32:64], in_=ti.ap[32:64], scalar=63, op=ALU.bitwise_and), reads=[ti], writes=[ti])
    P.copy("dve", ang[R64], ti[R64])
    P.op("dve", lambda e: e.tensor_single_scalar(out=nn.ap[R64, 0:1], in_=pi_.ap[R64], scalar=15, op=ALU.bitwise_and), reads=[pi_], writes=[nn])
    P.copy("dve", pf[R64], nn[R64, 0:1])
    P.act(invf[R64], pf[R64], AF.Exp, scale=-float(np.log(10000.0)) / 16.0)
    P.op("dve", lambda e: e.tensor_scalar(out=nn.ap[R64, 1:2], in0=pi_.ap[R64], scalar1=4, scalar2=1, op0=ALU.arith_shift_right, op1=ALU.bitwise_and), reads=[pi_], writes=[nn])
    P.copy("dve", sgn[R64], nn[R64, 1:2])
    P.ts("dve", sgn[R64], sgn[R64], 2.0, ALU.mult, -1.0, ALU.add)
    P.ts("dve", ang[R64], ang[R64], invf[R64], ALU.mult)
    TWO_PI = float(2 * np.pi)
    for (dst, shift) in ((SINS, 0.0), (COS, float(np.pi / 2))):
        P.ts("dve", nf[R64], ang[R64], shift, ALU.add, 1.0 / TWO_PI, ALU.mult)
        P.copy("dve", nn[R64], nf[R64])
        P.copy("dve", nf[R64], nn[R64])
        P.stt("dve", fx[R64], nf[R64], -TWO_PI, ang[R64], ALU.mult, ALU.add)
        if shift != 0.0:
            P.ts("dve", fx[R64], fx[R64], shift, ALU.add)
        P.ts("dve", nf[R64], fx[R64], -float(np.pi), ALU.is_lt, TWO_PI, ALU.mult)
        P.tt("dve", fx[R64], fx[R64], nf[R64], ALU.add)
        P.ts("dve", nf[R64], fx[R64], float(np.pi), ALU.is_gt, -TWO_PI, ALU.mult)
        P.tt("dve", fx[R64], fx[R64], nf[R64], ALU.add)
        P.act(dst[R64], fx[R64], AF.Sin)
    P.ts("dve", SINS[R64], SINS[R64], sgn[R64], ALU.mult)
    P.barrier()
    A.reset(m3t)
    hT = A.b16("hT", [8, T])
    wdq = A.b16("wdq", [8, 512]); wdkv = A.b16("wdkv", [8, 384])
    P.dma("pool", wdq, dram(wdq_d.rearrange("(kc p) n -> p kc n", p=128), "wdqd"))
    P.dma("pool", wdkv, dram(wdkv_d.rearrange("(kc p) n -> p kc n", p=128), "wdkvd"))
    m3a = A.mark()
    rstd1 = A.f32("rstd1", [T])
    sq3 = [A.f32(f"sq3{k}", [512]) for k in range(3)]
    rstd_all(rstd1, BLK, xTc, D, sq3, [PS[0], PS[1]])
    u3 = [A.f32(f"u3{k}", [512]) for k in range(2)]
    it3 = 0
    for j in range(8):
        for (t0, n) in BLK:
            s = 0 if t0 < L else 1
            u = u3[it3 % 2]; it3 += 1
            P.tt("dve", u[:, 0:n], xTc[j][:, t0:t0 + n], rstd1[:, t0:t0 + n], ALU.mult)
            P.act(hT[:, j, t0:t0 + n], u[:, 0:n], AF.Identity, bias=MOD[:, 1, 0 + j, s:s + 1], scale=DER[:, 1, 0, j, s:s + 1])
    P.barrier()
    A.reset(m3a)
    sq3 = [A.f32(f"sq3{k}", [512]) for k in range(3)]
    cqf = A.f32("cqf", [4, 512]); ckf = A.f32("ckf", [2, 512]); rq = A.f32("rq", [512]); rk = A.f32("rk", [512])
    ta = A.f32("ta", [512]); tb = A.f32("tb", [512])
    for bi, (t0, n) in enumerate(BLK):
        lat = t0 < L
        if lat:
            for m in range(4):
                pk = PS[2 + m % 2]
                for k in range(8):
                    P.mm(pk[:, 0:n], wdq[:, k, m * 128:(m + 1) * 128], hT[:, k, t0:t0 + n], start=(k == 0), stop=(k == 7))
                P.copy("act", cqf[:, m, 0:n], pk[:, 0:n])
            rstd_all(rq, [(0, n)], [cqf[:, m, :] for m in range(4)], 512, sq3, [PS[0]])
            for m in range(4):
                P.tt("dve", cqf[:, m, 0:n], cqf[:, m, 0:n], rq[:, 0:n], ALU.mult)
                P.act(cqn[:, m, t0:t0 + n], cqf[:, m, 0:n], AF.Copy, scale=VT1[:, m, R_QG:R_QG + 1])
        for m in range(2):
            pk = PS[4 + m % 2]
            for k in range(8):
                P.mm(pk[:, 0:n], wdkv[:, k, m * 128:(m + 1) * 128], hT[:, k, t0:t0 + n], start=(k == 0), stop=(k == 7))
            P.copy("act", ckf[:, m, 0:n], pk[:, 0:n])
        rstd_all(rk, [(0, n)], [ckf[:, m, :] for m in range(2)], 256, sq3, [PS[1]])
        for m in range(2):
            P.tt("dve", ckf[:, m, 0:n], ckf[:, m, 0:n], rk[:, 0:n], ALU.mult)
            P.act(ckvn[:, m, t0:t0 + n], ckf[:, m, 0:n], AF.Copy, scale=VT1[:, m, R_KG:R_KG + 1])
        pa = PS[6]; pb = PS[7]
        for k in range(8):
            P.mm(pa[0:64, 0:n], wdkv[:, k, 256:320], hT[:, k, t0:t0 + n], start=(k == 0), stop=(k == 7))
        if lat:
            for k in range(8):
                P.mm(pb[0:64, 0:n], wdkv[:, k, 320:384], hT[:, k, t0:t0 + n], start=(k == 0), stop=(k == 7))
            P.tt("dve", ta[R64, 0:n], pa[0:64, 0:n], COS[R64, t0:t0 + n], ALU.mult)
            P.tt("dve", tb[R64, 0:n], pb[0:64, 0:n], SINS[R64, t0:t0 + n], ALU.mult)
            P.tt("dve", kpe[R64, t0:t0 + n], ta[R64, 0:n], tb[R64, 0:n], ALU.add)
        else:
            P.copy("dve", kpe[R64, t0:t0 + n], pa[0:64, 0:n])
    P.barrier()
    A.reset(m3t)
    wo = [A.b16(f"wo{k}", [D]) for k in range(2)]
    wq = [A.b16(f"wq{k}", [4, 128]) for k in range(2)]
    wr = [A.b16(f"wr{k}", [4, 128]) for k in range(2)]
    wk = [A.b16(f"wk{k}", [2, 128]) for k in range(2)]
    wv = [A.b16(f"wv{k}", [2, 128]) for k in range(2)]
    qn = A.b16("qn", [L]); qr = A.b16("qr", [L]); kn = A.b16("kn", [T]); vh = A.b16("vh", [18, 128])
    attT = A.b16("attT", [L])
    S_sb = [A.f32(f"S_sb{k}", [T]) for k in range(2)]
    Pb = [A.b16(f"Pb{k}", [T]) for k in range(2)]
    PT = [A.b16(f"PT{k}", [18, 128]) for k in range(2)]
    mx = [A.f32(f"mx{k}", [1]) for k in range(2)]
    bmx = [A.f32(f"bmx{k}", [8]) for k in range(2)]
    rsum = [A.f32(f"rsum{k}", [1]) for k in range(2)]
    onb = [A.b16(f"onb{k}", [128]) for k in range(2)]
    qt_ = A.f32("qt_", [512]); ta = A.f32("ta", [512]); tb = A.f32("tb", [512])
    for h in range(8):
        b = h % 2
        P.dma("pool", wq[b], dram(wuqn_d[:, h * 128:(h + 1) * 128].rearrange("(kc p) n -> p kc n", p=128), "wqd"))
        P.dma("pool", wr[b], dram(wuqr_d[:, h * 128:(h + 1) * 128].rearrange("(kc p) n -> p kc n", p=128), "wrd"))
        P.dma("pool", wk[b], dram(wuk_d[:, h * 128:(h + 1) * 128].rearrange("(kc p) n -> p kc n", p=128), "wkd"))
        P.dma("pool", wv[b], dram(wuv_d[:, h * 128:(h + 1) * 128].rearrange("(kc p) n -> p kc n", p=128), "wvd"))
        P.dma("pool", wo[b], dram(wo_d[h * 128:(h + 1) * 128, :], "wod"))
        for bi, (t0, n) in enumerate(LBLK):
            pk = PS[bi % 2]
            for k in range(4):
                P.mm(pk[:, 0:n], wq[b][:, k, :], cqn[:, k, t0:t0 + n], start=(k == 0), stop=(k == 3))
            P.act(qn[:, t0:t0 + n], pk[:, 0:n], AF.Copy, scale=ATTN_SCALE)
            pa = PS[2]; pb = PS[3]
            for k in range(4):
                P.mm(pa[0:64, 0:n], wr[b][:, k, 0:64], cqn[:, k, t0:t0 + n], start=(k == 0), stop=(k == 3))
            for k in range(4):
                P.mm(pb[0:64, 0:n], wr[b][:, k, 64:128], cqn[:, k, t0:t0 + n], start=(k == 0), stop=(k == 3))
            P.tt("dve", ta[R64, 0:n], pa[0:64, 0:n], COS[R64, t0:t0 + n], ALU.mult)
            P.tt("dve", tb[R64, 0:n], pb[0:64, 0:n], SINS[R64, t0:t0 + n], ALU.mult)
            P.tt("dve", qt_[R64, 0:n], ta[R64, 0:n], tb[R64, 0:n], ALU.add)
            P.act(qr[R64, t0:t0 + n], qt_[R64, 0:n], AF.Copy, scale=ATTN_SCALE)
        for bi, (t0, n) in enumerate(BLK):
            pk = PS[4 + bi % 2]
            for k in range(2):
                P.mm(pk[:, 0:n], wk[b][:, k, :], ckvn[:, k, t0:t0 + n], start=(k == 0), stop=(k == 1))
            P.copy("act", kn[:, t0:t0 + n], pk[:, 0:n])
        for kt in range(18):
            pk = PS[6 + kt % 2]
            for k in range(2):
                P.mm(pk[:, 0:128], ckvn[:, k, kt * 128:(kt + 1) * 128], wv[b][:, k, :], start=(k == 0), stop=(k == 1))
            P.copy("dve", vh[:, kt, :], pk[:, 0:128])

        def scores(qt):
            qs = slice(qt * 128, (qt + 1) * 128)
            c = qt % 2
            for bi, (t0, n) in enumerate(BLK):
                pk = PS[bi % 4]
                P.mm(pk[:, 0:n], qn[:, qs], kn[:, t0:t0 + n], start=True, stop=False)
                P.mm(pk[:, 0:n], qr[R64, qs], kpe[R64, t0:t0 + n], start=False, stop=True)
                if bi % 2 == 0:
                    P.ts("dve", S_sb[c][:, t0:t0 + n], pk[:, 0:n], 1.0, ALU.mult, None, ALU.max, accum_out=bmx[c][:, bi:bi + 1])
                else:
                    P.copy("act", S_sb[c][:, t0:t0 + n], pk[:, 0:n])
                    P.red("dve", bmx[c][:, bi:bi + 1], S_sb[c][:, t0:t0 + n], ALU.max)

        def softmax(qt):
            c = qt % 2
            P.red("dve", mx[c], bmx[c][:, 0:5], ALU.max)
            P.ts("dve", mx[c], mx[c], -1.0, ALU.mult)
            P.act(Pb[c], S_sb[c], AF.Exp, bias=mx[c], accum_out=rsum[c])

        def attend_tile(qt, part):
            qs = slice(qt * 128, (qt + 1) * 128)
            c = qt % 2
            for g0 in range(0, 18, 8):
                if part == 2 or (part == 0) != (g0 < 16):
                    continue
                ng = min(8, 18 - g0)
                pk = PS[4 + (g0 // 8) % 2]
                pkb = TV(pk.ap.bitcast(BF16), pk.buf)
                for kk in range(ng):
                    kt = g0 + kk
                    P.tr(pkb[:, kk * 128:(kk + 1) * 128], Pb[c][:, kt * 128:(kt + 1) * 128], ident)
                P.op("act", lambda e, pkb=pkb, g0=g0, ng=ng, c=c: e.activation(
                    out=PT[c].ap[:, g0:g0 + ng, :], in_=pkb.ap[:, 0:ng * 128].rearrange("p (a b) -> p a b", a=ng), func=AF.Copy),
                    reads=[pkb], writes=[PT[c]])
            if part != 2:
                return
            po = PS[6]
            for kt in range(18):
                P.mm(po[:, 0:128], PT[c][:, kt, :], vh[:, kt, :], start=(kt == 0), stop=(kt == 17))
            P.op("dve", lambda e, r=rsum[c]: e.reciprocal(out=r.ap, in_=r.ap), reads=[rsum[c]], writes=[rsum[c]])
            P.ts("dve", onb[c], po[:, 0:128], rsum[c], ALU.mult)
            pt2 = PS[7]
            pt2b = TV(pt2.ap.bitcast(BF16), pt2.buf)
            P.tr(pt2b[:, 0:128], onb[c], ident)
            P.copy("act", attT[:, qs], pt2b[:, 0:128])

        scores(0)
        for qt in range(17):
            if qt >= 1:
                attend_tile(qt - 1, 0)
            if qt + 1 < 16:
                scores(qt + 1)
            if qt >= 1:
                attend_tile(qt - 1, 1)
            if qt < 16:
                softmax(qt)
            if qt >= 1:
                attend_tile(qt - 1, 2)
        for m in range(8):
            for bi, (t0, n) in enumerate(LBLK):
                pk = PS[(m * 4 + bi) % 4]
                P.mm(pk[:, 0:n], wo[b][:, m * 128:(m + 1) * 128], attT[:, t0:t0 + n])
                P.stt("dve", xTc[m][:, t0:t0 + n], pk[:, 0:n], MOD[:, 1, 16 + m, 0:1], xTc[m][:, t0:t0 + n], ALU.mult, ALU.add)
    P.barrier()
    A.reset(m3)
    if stage == 3:
        return dump_and_finish()

    moe(1, 16)
    if stage == 4:
        return dump_and_finish()

    fgb = A.f32("fgb", [D])
    P.dma("sp", fgb, dram(v1_d[R_FG:R_FG + 1, :].to_broadcast([128, D]), "fgd"))
    xo = [A.f32(f"xo{k}", [D]) for k in range(2)]
    sqo = A.f32("sqo", [D]); ssum = A.f32("ssum", [1])
    for tt in range(16):
        tok = slice(tt * 128, (tt + 1) * 128)
        o = xo[tt % 2]
        for half in range(2):
            pk = PS[(2 * tt + half) % 4]
            for jj in range(4):
                P.mm(pk[:, jj * 128:(jj + 1) * 128], xTc[half * 4 + jj][:, tok], identf)
            P.copy("act", o[:, half * 512:(half + 1) * 512], pk)
        P.act(sqo, o, AF.Square, accum_out=ssum)
        P.ts("dve", ssum, ssum, 1.0 / D, ALU.mult, EPS, ALU.add)
        P.act(ssum, ssum, AF.Sqrt)
        P.op("dve", lambda e: e.reciprocal(out=ssum.ap, in_=ssum.ap), reads=[ssum], writes=[ssum])
        P.stt("dve", o, o, ssum, fgb, ALU.mult, ALU.mult)
        P.dma("sp", dram(out_d[tt * 128:(tt + 1) * 128, :], "outd"), o)
    P.barrier()
    P.emit()
    return nc, st, P


def _prep_inputs(inp, b, stage):
    f = np.float32
    v1 = np.zeros((NV1, D), f)
    v1[R_N1G:R_N1G + 2] = inp["norm1_g"]; v1[R_N2G:R_N2G + 2] = inp["norm2_g"]
    v1[R_PB] = inp["pool_b"][0]; v1[R_PS] = inp["pool_scale"][0]; v1[R_FG] = inp["final_g"]
    v1[R_C] = inp["c"][b]; v1[R_CC] = inp["c_ctx"]
    v1[R_ADAB:R_ADAB + 12] = inp["ada_b"].reshape(12, D)
    v1[R_QG, :512] = inp["q_norm_g"][0]; v1[R_KG, :256] = inp["kv_norm_g"][0]
    v1[R_BD:R_BD + 64] = inp["b_down"].reshape(64, D)
    v2 = np.ascontiguousarray(inp["b_gu"].reshape(128, D))
    m = {"x": np.ascontiguousarray(inp["x"][b]), "ctx": np.ascontiguousarray(inp["ctx"][b]), "v1": v1, "v2": v2,
         "ada_w": inp["ada_w"], "pool_w": np.ascontiguousarray(inp["pool_w"][0])}
    if stage >= 3:
        wuq = inp["w_uq"][0].reshape(512, 8, 192)
        perm = np.array([a * 32 + (1 - hf) * 16 + i for a in range(2) for hf in range(2) for i in range(16)])
        rope = wuq[:, :, 128:]
        wkv = inp["w_ukv"][0].reshape(256, 8, 256)
        dkv = inp["w_dkv"][0]
        m.update({"w_dq": np.ascontiguousarray(inp["w_dq"][0]),
                  "w_uq_n": np.ascontiguousarray(wuq[:, :, :128].reshape(512, 1024)),
                  "w_uq_r": np.ascontiguousarray(np.concatenate([rope, rope[:, :, perm]], axis=2).reshape(512, 1024)),
                  "w_dkv": np.ascontiguousarray(np.concatenate([dkv, dkv[:, 256:][:, perm]], axis=1)),
                  "w_uk": np.ascontiguousarray(wkv[:, :, :128].reshape(256, 1024)),
                  "w_uv": np.ascontiguousarray(wkv[:, :, 128:].reshape(256, 1024)),
                  "w_o": np.ascontiguousarray(inp["w_o"][0])})
    if stage >= 2:
        m.update({"router_w": inp["router_w"], "router_b": inp["router_b"], "w_gu": inp["w_gu"], "w_down": inp["w_down"],
                  "b_down": inp["b_down"]})
    return m


def kernel(**inputs):
    inp = {k: np.asarray(v) for k, v in inputs.items()}
    nc, st, P = build_program(STAGE)
    with st:
        in_maps = [_prep_inputs(inp, b, STAGE) for b in range(NCORES)]
        res = run_bass_kernel_spmd(nc, in_maps, core_ids=list(range(NCORES)))
    if STAGE < 99:
        return res
    return np.stack([r["out"] for r in res.results], axis=0)
```
